# Optimizing a Trainium2 kernel written in Bass

```python
import jax, jax.numpy as jnp
from jax import lax
import numpy as np

D_MODEL = 1024
BATCH = 4
SEQ = 8192
DEPTH = 4

N_META = 16
N_A = DEPTH // 2
N_B = DEPTH - N_A
N_DENSE = (DEPTH + 1) // 2
N_MOE = DEPTH // 2

M_HEADS = 8
M_QK = 64
M_V = D_MODEL // M_HEADS
M_CHUNK = 64
M_IN = 2 * M_HEADS * M_QK + M_HEADS * M_V + D_MODEL + 2 * M_HEADS

A_HEADS = 16
A_KV = 4
A_HD = 64
A_GROUP = A_HEADS // A_KV
WINDOW = 128
ROPE_THETA = 500000.0
ROPE_DIM = A_HD // 4

D_FF = 2816
N_EXP = 8
TOP_K = 2
D_FF_EXP = 3584
MOE_BLOCK = 256

ALPHA = (2.0 * DEPTH) ** 0.25
BETA = (8.0 * DEPTH) ** -0.25
LN_EPS = 1e-5
NEG = -1e30

kernel_name = "hybrid_mlstm_swa_yoco_moe"


def layer_norm(x, g, b):
    xf = x.astype(jnp.float32)
    mu = xf.mean(-1, keepdims=True)
    var = jnp.square(xf - mu).mean(-1, keepdims=True)
    return ((xf - mu) * lax.rsqrt(var + LN_EPS) * g.astype(jnp.float32) + b.astype(jnp.float32)).astype(x.dtype)


def rope_tables(length):
    half = ROPE_DIM // 2
    inv_freq = ROPE_THETA ** (-jnp.arange(half, dtype=jnp.float32) * 2.0 / ROPE_DIM)
    ang = jnp.arange(length, dtype=jnp.float32)[:, None] * inv_freq[None, :]
    return jnp.cos(ang), jnp.sin(ang)


def partial_rope(x, cos, sin):
    half = ROPE_DIM // 2
    xr = x[..., :ROPE_DIM].astype(jnp.float32)
    x1, x2 = xr[..., :half], xr[..., half:]
    c, s = cos[None, :, None, :], sin[None, :, None, :]
    rot = jnp.concatenate([x1 * c - x2 * s, x2 * c + x1 * s], -1).astype(x.dtype)
    return jnp.concatenate([rot, x[..., ROPE_DIM:]], -1)


def mlstm_chunkwise(q, k, v, i_pre, f_pre):
    bsz, length, n_h, dk = q.shape
    dv = v.shape[-1]
    f32 = jnp.float32
    pad = (-length) % M_CHUNK
    q = jnp.pad(q.astype(f32) * dk ** -0.5, ((0, 0), (pad, 0), (0, 0), (0, 0)))
    k = jnp.pad(k.astype(f32), ((0, 0), (pad, 0), (0, 0), (0, 0)))
    v = jnp.pad(v.astype(f32), ((0, 0), (pad, 0), (0, 0), (0, 0)))
    li = jnp.pad(i_pre.astype(f32), ((0, 0), (pad, 0), (0, 0)), constant_values=NEG)
    lf = jnp.pad(jax.nn.log_sigmoid(f_pre.astype(f32)), ((0, 0), (pad, 0), (0, 0)))
    n_c = (length + pad) // M_CHUNK

    def chunks(t):
        return t.reshape(bsz, n_c, M_CHUNK, n_h, -1).transpose(0, 3, 1, 2, 4)

    qc, kc, vc = chunks(q), chunks(k), chunks(v)
    lic = li.reshape(bsz, n_c, M_CHUNK, n_h).transpose(0, 3, 1, 2)
    lfc = lf.reshape(bsz, n_c, M_CHUNK, n_h).transpose(0, 3, 1, 2)
    b = jnp.cumsum(lfc, axis=-1)
    b_last = b[..., -1]

    a = b_last[..., None] - b + lic
    a_max = a.max(-1)
    w = jnp.exp(a - a_max[..., None])
    c_loc = jnp.einsum('bhnc,bhnck,bhncv->bhnkv', w, kc, vc)
    n_loc = jnp.einsum('bhnc,bhnck->bhnk', w, kc)

    def step(carry, inp):
        c_st, n_st, m_st = carry
        cl, nl, ml, g = inp
        m_new = jnp.maximum(g + m_st, ml)
        s_old = jnp.exp(g + m_st - m_new)
        s_new = jnp.exp(ml - m_new)
        c_new = s_old[..., None, None] * c_st + s_new[..., None, None] * cl
        n_new = s_old[..., None] * n_st + s_new[..., None] * nl
        return (c_new, n_new, m_new), (c_st, n_st, m_st)

    init = (jnp.zeros((bsz, n_h, dk, dv), f32), jnp.zeros((bsz, n_h, dk), f32), jnp.zeros((bsz, n_h), f32))
    xs = (jnp.moveaxis(c_loc, 2, 0), jnp.moveaxis(n_loc, 2, 0), jnp.moveaxis(a_max, 2, 0), jnp.moveaxis(b_last, 2, 0))
    _, (c0, n0, m0) = lax.scan(step, init, xs)
    c0, n0, m0 = jnp.moveaxis(c0, 0, 2), jnp.moveaxis(n0, 0, 2), jnp.moveaxis(m0, 0, 2)

    idx = jnp.arange(M_CHUNK)
    causal = idx[:, None] >= idx[None, :]
    dmat = jnp.where(causal, b[..., :, None] - b[..., None, :] + lic[..., None, :], NEG)
    inter = b + m0[..., None]
    m_t = jnp.maximum(dmat.max(-1), inter)
    s = jnp.einsum('bhnik,bhnjk->bhnij', qc, kc) * jnp.exp(dmat - m_t[..., None])
    s_inter = jnp.exp(inter - m_t)
    num = jnp.einsum('bhnij,bhnjv->bhniv', s, vc) + s_inter[..., None] * jnp.einsum('bhnik,bhnkv->bhniv', qc, c0)
    den = s.sum(-1) + s_inter * jnp.einsum('bhnik,bhnk->bhni', qc, n0)
    h = num / jnp.maximum(jnp.abs(den), jnp.exp(-m_t))[..., None]
    h = h.transpose(0, 2, 3, 1, 4).reshape(bsz, n_c * M_CHUNK, n_h, dv)
    return h[:, pad:]


def mlstm_mixer(x, w_in, b_gate, ln_h, w_out):
    bsz, length, _ = x.shape
    proj = x @ w_in
    o1 = M_HEADS * M_QK
    o2 = 2 * o1
    o3 = o2 + M_HEADS * M_V
    o4 = o3 + D_MODEL
    q = proj[..., :o1].reshape(bsz, length, M_HEADS, M_QK)
    k = proj[..., o1:o2].reshape(bsz, length, M_HEADS, M_QK)
    v = proj[..., o2:o3].reshape(bsz, length, M_HEADS, M_V)
    og = proj[..., o3:o4]
    gates = proj[..., o4:] + b_gate
    hm = mlstm_chunkwise(q, k, v, gates[..., :M_HEADS], gates[..., M_HEADS:])
    mu = hm.mean(-1, keepdims=True)
    var = jnp.square(hm - mu).mean(-1, keepdims=True)
    hn = (hm - mu) * lax.rsqrt(var + LN_EPS) * ln_h.astype(jnp.float32).reshape(M_HEADS, M_V)
    out = (hn.reshape(bsz, length, M_HEADS * M_V) * jax.nn.sigmoid(og.astype(jnp.float32))).astype(x.dtype)
    return out @ w_out


def shared_kv(h, w_kv, cos, sin):
    bsz, length, _ = h.shape
    kv = h @ w_kv
    k = kv[..., :A_KV * A_HD].reshape(bsz, length, A_KV, A_HD)
    v = kv[..., A_KV * A_HD:].reshape(bsz, length, A_KV, A_HD)
    return partial_rope(k, cos, sin), v


def swa_mixer(x, k, v, w_q, sinks, w_o, cos, sin):
    bsz, length, _ = x.shape
    n_real = length - N_META
    n_blk = n_real // WINDOW
    f32 = jnp.float32
    scale = A_HD ** -0.5
    q = partial_rope((x @ w_q).reshape(bsz, length, A_HEADS, A_HD), cos, sin)
    q = q.reshape(bsz, length, A_KV, A_GROUP, A_HD)
    sink = sinks.astype(f32).reshape(A_KV, A_GROUP)[:, :, None]
    qm, qr = q[:, :N_META], q[:, N_META:]
    km, kr = k[:, :N_META], k[:, N_META:]
    vm, vr = v[:, :N_META].astype(f32), v[:, N_META:]

    s_mm = jnp.einsum('bqhgd,bkhd->bhgqk', qm, km).astype(f32) * scale
    s_mm = jnp.where(jnp.tril(jnp.ones((N_META, N_META), bool)), s_mm, NEG)
    m_mm = jnp.maximum(s_mm.max(-1), sink)
    p_mm = jnp.exp(s_mm - m_mm[..., None])
    den_mm = p_mm.sum(-1) + jnp.exp(sink - m_mm)
    o_m = jnp.einsum('bhgqk,bkhd->bqhgd', p_mm / den_mm[..., None], vm)

    qb = qr.reshape(bsz, n_blk, WINDOW, A_KV, A_GROUP, A_HD)
    kb = kr.reshape(bsz, n_blk, WINDOW, A_KV, A_HD)
    vb = vr.reshape(bsz, n_blk, WINDOW, A_KV, A_HD)

    def with_prev(t):
        prev = jnp.pad(t, ((0, 0), (1, 0), (0, 0), (0, 0), (0, 0)))[:, :-1]
        return jnp.concatenate([prev, t], axis=2)

    k_band, v_band = with_prev(kb), with_prev(vb).astype(f32)
    s_band = jnp.einsum('bnqhgd,bnkhd->bnhgqk', qb, k_band).astype(f32) * scale
    s_meta = jnp.einsum('bnqhgd,bkhd->bnhgqk', qb, km).astype(f32) * scale
    qi = jnp.arange(WINDOW)[:, None]
    kj = jnp.arange(2 * WINDOW)[None, :]
    rel = qi + WINDOW - kj
    in_band = (rel >= 0) & (rel < WINDOW)
    blk_ok = (jnp.arange(n_blk)[:, None, None] > 0) | (kj >= WINDOW)[None]
    mask = in_band[None] & blk_ok
    s_band = jnp.where(mask[None, :, None, None], s_band, NEG)
    m = jnp.maximum(jnp.maximum(s_band.max(-1), s_meta.max(-1)), sink)
    p_band = jnp.exp(s_band - m[..., None])
    p_meta = jnp.exp(s_meta - m[..., None])
    inv_den = 1.0 / (p_band.sum(-1) + p_meta.sum(-1) + jnp.exp(sink - m))
    o_r = (jnp.einsum('bnhgqk,bnkhd->bnhgqd', p_band, v_band)
           + jnp.einsum('bnhgqk,bkhd->bnhgqd', p_meta, vm)) * inv_den[..., None]
    o_r = o_r.transpose(0, 1, 4, 2, 3, 5).reshape(bsz, n_real, A_HEADS * A_HD)
    o = jnp.concatenate([o_m.reshape(bsz, N_META, A_HEADS * A_HD), o_r], axis=1).astype(x.dtype)
    return o @ w_o


def swiglu(x, w_gu, w_down):
    gu = x @ w_gu
    return (jax.nn.silu(gu[..., :D_FF]) * gu[..., D_FF:]) @ w_down


def moe_swiglu(x, w_router, b_router, w_gu, w_down):
    bsz, length, d = x.shape
    n_tok = bsz * length
    f32 = jnp.float32
    xt = x.reshape(n_tok, d)
    logits = (xt @ w_router).astype(f32) + b_router.astype(f32)
    top_val, top_idx = lax.top_k(logits, TOP_K)
    gate = jax.nn.softmax(top_val, axis=-1)
    n_assign = n_tok * TOP_K
    flat_e = top_idx.reshape(-1)
    flat_tok = jnp.repeat(jnp.arange(n_tok, dtype=jnp.int32), TOP_K)
    flat_gate = gate.reshape(-1)
    order = jnp.argsort(flat_e)
    e_sorted = flat_e[order]
    counts = jnp.bincount(flat_e, length=N_EXP)
    padded = (counts + MOE_BLOCK - 1) // MOE_BLOCK * MOE_BLOCK
    start = jnp.cumsum(counts) - counts
    pad_end = jnp.cumsum(padded)
    pad_start = pad_end - padded
    dest = pad_start[e_sorted] + jnp.arange(n_assign) - start[e_sorted]
    n_rows = -(-n_assign // MOE_BLOCK) * MOE_BLOCK + N_EXP * MOE_BLOCK
    n_blocks = n_rows // MOE_BLOCK
    row_tok = jnp.zeros((n_rows,), jnp.int32).at[dest].set(flat_tok[order])
    row_gate = jnp.zeros((n_rows,), f32).at[dest].set(flat_gate[order])
    blk_exp = jnp.minimum(jnp.searchsorted(pad_end, jnp.arange(n_blocks) * MOE_BLOCK, side='right'), N_EXP - 1)
    xs = xt[row_tok].reshape(n_blocks, MOE_BLOCK, d)

    def expert_block(args):
        xb, e = args
        gu = xb @ w_gu[e]
        return (jax.nn.silu(gu[:, :D_FF_EXP]) * gu[:, D_FF_EXP:]) @ w_down[e]

    ys = lax.map(expert_block, (xs, blk_exp)).reshape(n_rows, d)
    out = jax.ops.segment_sum(ys * row_gate[:, None].astype(ys.dtype), row_tok, num_segments=n_tok)
    return out.reshape(bsz, length, d).astype(x.dtype)


def setup_inputs(seed: int = 0) -> dict:
    key = jax.random.key(seed)
    ks = jax.random.split(key, 24)
    f32 = jnp.float32

    def nrm(k, shape):
        return jax.random.normal(k, shape, f32)

    def dense(k, shape, fan_in, gain=1.0):
        return nrm(k, shape) * (gain * fan_in ** -0.5)

    return {
        "x": nrm(ks[0], (BATCH, SEQ, D_MODEL)),
        "meta": nrm(ks[1], (N_META, D_MODEL)),
        "w_in_a": dense(ks[2], (N_A, D_MODEL, M_IN), D_MODEL),
        "b_gate_a": jnp.concatenate([0.1 * nrm(ks[3], (N_A, M_HEADS)),
                                      3.0 + 0.5 * nrm(ks[4], (N_A, M_HEADS))], axis=-1),
        "ln_h_a": 1.0 + 0.02 * nrm(ks[5], (N_A, M_HEADS * M_V)),
        "w_out_a": dense(ks[6], (N_A, M_HEADS * M_V, D_MODEL), M_HEADS * M_V, BETA),
        "w_kv": dense(ks[7], (D_MODEL, 2 * A_KV * A_HD), D_MODEL),
        "w_q_b": dense(ks[8], (N_B, D_MODEL, A_HEADS * A_HD), D_MODEL),
        "sinks_b": 0.5 * nrm(ks[9], (N_B, A_HEADS)),
        "w_o_b": dense(ks[10], (N_B, A_HEADS * A_HD, D_MODEL), A_HEADS * A_HD, BETA),
        "w_gu_d": dense(ks[11], (N_DENSE, D_MODEL, 2 * D_FF), D_MODEL),
        "w_down_d": dense(ks[12], (N_DENSE, D_FF, D_MODEL), D_FF, BETA),
        "w_router": dense(ks[13], (N_MOE, D_MODEL, N_EXP), D_MODEL),
        "b_router": 0.01 * nrm(ks[14], (N_MOE, N_EXP)),
        "w_gu_e": dense(ks[15], (N_MOE, N_EXP, D_MODEL, 2 * D_FF_EXP), D_MODEL),
        "w_down_e": dense(ks[16], (N_MOE, N_EXP, D_FF_EXP, D_MODEL), D_FF_EXP, BETA),
        "ln_g": 1.0 + 0.02 * nrm(ks[17], (DEPTH, 2, D_MODEL)),
        "ln_b": 0.02 * nrm(ks[18], (DEPTH, 2, D_MODEL)),
    }


def reference(x, meta, w_in_a, b_gate_a, ln_h_a, w_out_a, w_kv, w_q_b, sinks_b, w_o_b,
              w_gu_d, w_down_d, w_router, b_router, w_gu_e, w_down_e, ln_g, ln_b):
    bsz = x.shape[0]
    h = jnp.concatenate([jnp.broadcast_to(meta.astype(x.dtype)[None], (bsz, N_META, D_MODEL)), x], axis=1)
    length = h.shape[1]
    cos, sin = rope_tables(length)
    k_sh, v_sh = None, None
    for layer in range(DEPTH):
        if layer < N_A:
            mix = mlstm_mixer(h, w_in_a[layer], b_gate_a[layer], ln_h_a[layer], w_out_a[layer])
        else:
            if layer == N_A:
                k_sh, v_sh = shared_kv(h, w_kv, cos, sin)
            j = layer - N_A
            mix = swa_mixer(h, k_sh, v_sh, w_q_b[j], sinks_b[j], w_o_b[j], cos, sin)
        h = layer_norm(ALPHA * h + mix, ln_g[layer, 0], ln_b[layer, 0])
        if layer % 2 == 0:
            ffn = swiglu(h, w_gu_d[layer // 2], w_down_d[layer // 2])
        else:
            e = layer // 2
            ffn = moe_swiglu(h, w_router[e], b_router[e], w_gu_e[e], w_down_e[e])
        h = layer_norm(ALPHA * h + ffn, ln_g[layer, 1], ln_b[layer, 1])
    return h[:, N_META:]
```

```python
from concourse.bass_utils import run_bass_kernel_spmd
import numpy as np
from contextlib import ExitStack
import concourse.bass as bass
import concourse.mybir as mybir

F32 = mybir.dt.float32
BF16 = mybir.dt.bfloat16
I32 = mybir.dt.int32
AF = mybir.ActivationFunctionType
ALU = mybir.AluOpType
AX = mybir.AxisListType

SEM_LIMIT = 30000


class T:
    __slots__ = ("h", "name", "w", "r", "dsem", "dkey", "dcnt", "psum", "sems")

    def __init__(self, h, name, psum=False):
        self.h = h
        self.name = name
        self.psum = psum
        self.w = None
        self.r = {}
        self.dsem = None
        self.dkey = None
        self.dcnt = 0
        self.sems = {}

    def __getitem__(self, idx):
        return self.h[idx]


class Prog:
    ENGS = ("pe", "act", "dve", "pool", "sp")

    def __init__(self, nc, stack):
        self.nc = nc
        self.stack = stack
        self.eng = {"pe": nc.tensor, "act": nc.scalar, "dve": nc.vector,
                    "pool": nc.gpsimd, "sp": nc.sync}
        self.nsem = 0
        self.nkey = 0
        self.esem = {}
        self.ekey = {}
        self.ecnt = {}
        self.seen = {e: {} for e in self.ENGS}
        self.all_tok = {}
        for e in self.ENGS:
            self._new_esem(e)
        self.nops = {e: 0 for e in self.ENGS}
        self.last_rowgrp = None
        self.sem_pool = {"sw": [], "hw": []}
        self.dma_tiles = []

    def _sem(self, name):
        self.nsem += 1
        return self.stack.enter_context(self.nc.semaphore(f"{name}_{self.nsem}"))

    def _new_esem(self, e):
        self.esem[e] = self._sem("e" + e)
        self.ekey[e] = ("e", e, self.nsem)
        self.ecnt[e] = 0

    def sb(self, name, shape, dt, stack=None):
        st = stack or self.stack
        self.nkey += 1
        name = f"{name}_{self.nkey}"
        return T(st.enter_context(self.nc.sbuf_tensor(name, list(shape), dt)), name)

    def ps(self, name, shape, dt, stack=None):
        st = stack or self.stack
        self.nkey += 1
        name = f"{name}_{self.nkey}"
        return T(st.enter_context(self.nc.psum_tensor(name, list(shape), dt)), name, psum=True)

    def dram(self, name, shape, dt, kind=None):
        if kind is None:
            return T(self.nc.dram_tensor(name, list(shape), dt), name)
        return T(self.nc.dram_tensor(name, list(shape), dt, kind=kind), name)

    def _waits(self, e, reads, writes):
        need = {}

        def add(tok):
            if tok is None:
                return
            key, h, v = tok
            if self.seen[e].get(key, 0) >= v:
                return
            if key not in need or need[key][1] < v:
                need[key] = (h, v)

        for t in reads:
            add(t.w)
        for t in writes:
            add(t.w)
            for tok in t.r.values():
                add(tok)
        out = []
        for key, (h, v) in need.items():
            if e == "pe" and key == self.ekey["pe"]:
                continue
            self.seen[e][key] = v
            out.append((h, v))
        return out

    def _commit(self, tok, reads, writes):
        key = tok[0]
        for t in writes:
            t.w = tok
            t.r = {}
        for t in reads:
            if t in writes:
                continue
            t.r[key] = tok
        old = self.all_tok.get(key)
        if old is None or old[1] < tok[2]:
            self.all_tok[key] = (tok[1], tok[2])

    def op(self, e, fn, reads=(), writes=(), signal=True, rowgrp=None):
        E = self.eng[e]
        pr = [t for t in reads if t.psum]
        if pr:
            reads = [t for t in reads if not t.psum]
            writes = list(writes) + [t for t in pr if t not in writes]
        if e == "pe":
            if rowgrp != self.last_rowgrp:
                if self.ecnt["pe"] > 0:
                    E.wait_ge(self.esem["pe"], self.ecnt["pe"])
                self.last_rowgrp = rowgrp
            if rowgrp is not None:
                signal = True
        for (h, v) in self._waits(e, reads, writes):
            E.wait_ge(h, v)
        ins = fn(E)
        self.nops[e] += 1
        if signal:
            self.ecnt[e] += 1
            ins.then_inc(self.esem[e], 1)
            tok = (self.ekey[e], self.esem[e], self.ecnt[e])
            self._commit(tok, reads, writes)
            if self.ecnt[e] >= SEM_LIMIT:
                self._new_esem(e)
        else:
            tok = (self.ekey[e], self.esem[e], self.ecnt[e] + 1)
            self._commit(tok, reads, writes)
        return ins

    def dma(self, q, out, in_, sbt, reads=(), writes=(), **kw):
        E = self.eng[q]
        for (h, v) in self._waits(q, reads, writes):
            E.wait_ge(h, v)
        kind = "sw" if q == "pool" else "hw"
        ent = sbt.sems.get(kind)
        if ent is None or ent[2] + 16 > SEM_LIMIT:
            got = None
            pool_ = self.sem_pool[kind]
            while pool_:
                h_, c_ = pool_.pop()
                if c_ + 4096 < SEM_LIMIT:
                    got = (h_, c_)
                    break
            if got is None:
                got = (self._sem("d" + kind), 0)
            self.nkey += 1
            ent = [got[0], ("d", sbt.name, self.nkey), got[1]]
            sbt.sems[kind] = ent
            self.dma_tiles.append((sbt, kind))
        ent[2] += 16
        ins = E.dma_start(out=out, in_=in_, **kw)
        ins.then_inc(ent[0], 16)
        self.nops[q] += 1
        tok = (ent[1], ent[0], ent[2])
        self._commit(tok, reads, writes)
        return ins

    def _unused(self):
        sbt = None
        tok = (sbt.dkey, sbt.dsem, sbt.dcnt)
        self._commit(tok, reads, writes)
        return ins

    def dma_sem(self, sbt):
        got = None
        while self.sem_pool:
            h_, c_ = self.sem_pool.pop()
            if c_ + 4096 < SEM_LIMIT:
                got = (h_, c_)
                break
        if got is None:
            got = (self._sem("d"), 0)
        sbt.dsem, sbt.dcnt = got
        self.nkey += 1
        sbt.dkey = ("d", sbt.name, self.nkey)
        self.dma_tiles.append(sbt)

    def barrier(self, engs=None):
        for e in (engs or self.ENGS):
            E = self.eng[e]
            for key, (h, v) in self.all_tok.items():
                if e == "pe" and key == self.ekey["pe"]:
                    continue
                if self.seen[e].get(key, 0) >= v:
                    continue
                self.seen[e][key] = v
                E.wait_ge(h, v)

    def end_phase(self):
        self.barrier()
        for (t, kind) in self.dma_tiles:
            ent = t.sems.pop(kind, None)
            if ent is not None:
                self.sem_pool[kind].append((ent[0], ent[2]))
        self.dma_tiles = []

    def collective_allgather(self, src_ap, dst_ap, groups):
        self.barrier()
        if not hasattr(self, "ccsem"):
            self.ccsem = self._sem("cc")
            self.cccnt = 0
        E = self.eng["pool"]
        E.collective_compute("AllGather", ALU.bypass, replica_groups=groups, ins=[src_ap], outs=[dst_ap]).then_inc(self.ccsem)
        self.cccnt += 1
        key = ("cc", "cc", 0)
        self.all_tok[key] = (self.ccsem, self.cccnt)
        self.barrier()

    def finish(self):
        self.barrier()


class View:
    def __init__(self, ap, name="view"):
        self.h = ap
        self.name = name
        self.w = None
        self.r = {}
        self.dsem = None
        self.dkey = None
        self.dcnt = 0
        self.psum = False
        self.sems = {}

    def __getitem__(self, idx):
        return self.h


D = 1024
KC = 8
ALPHA = (2.0 * 4) ** 0.25
LN_EPS = 1e-5


class Act:
    def __init__(self, P, name, ntok, kind=None):
        self.ntok = ntok
        self.h = P.dram(name, [KC, 128, ntok], F32, kind=kind).h
        self.tiles = [T(self.h, f"{name}_t{i}") for i in range((ntok + 127) // 128)]
        self.name = name

    def ap(self, t0, n):
        return self.h[:, :, t0:t0 + n].rearrange("k p t -> p k t")

    def trk(self, t0, n):
        return self.tiles[t0 // 128:(t0 + n + 127) // 128]


class Consts:
    pass


def load_consts(P, C, cin):
    C.ones_f = P.sb("ones_f", [128, 128], F32)
    P.dma("sp", C.ones_f[:], cin["ones"][:], C.ones_f, reads=[cin["ones"]], writes=[C.ones_f])
    C.ident_f = P.sb("ident_f", [128, 128], F32)
    P.dma("sp", C.ident_f[:], cin["ident"][:], C.ident_f, reads=[cin["ident"]], writes=[C.ident_f])
    C.cin = cin
    C.eps_col = P.sb("eps_col", [128, 1], F32)
    P.op("dve", lambda e: e.memset(C.eps_col[:], LN_EPS), writes=[C.eps_col])
    C.ident_b = P.sb("ident_b", [128, 128], BF16)
    P.dma("pool", C.ident_b[:], cin["ident"][:], C.ident_b, reads=[cin["ident"]], writes=[C.ident_b])


def ln_params(P, name, g_ap, b_ap, stack):
    g = P.sb(name + "_g", [128, KC], F32, stack)
    b = P.sb(name + "_b", [128, KC], F32, stack)
    with P.nc.allow_non_contiguous_dma(reason="tiny ln param load"):
        P.dma("sp", g[:], g_ap.rearrange("(k p) -> p k", p=128), g, writes=[g])
        P.dma("sp", b[:], b_ap.rearrange("(k p) -> p k", p=128), b, writes=[b])
    return g, b


class LNBufs:
    def __init__(self, P, stack, W):
        self.W = W
        self.sq = [P.sb(f"ln_sq{i}", [128, W], F32, stack) for i in range(2)]
        self.ps_s = P.ps("ln_ps_s", [128, W], F32, stack)
        self.ps_q = P.ps("ln_ps_q", [128, W], F32, stack)
        self.mean = P.sb("ln_mean", [128, W], F32, stack)
        self.rstd = P.sb("ln_rstd", [128, W], F32, stack)
        self.tmp = [P.sb(f"ln_tmp{i}", [128, W], F32, stack) for i in range(2)]
        self.i = 0


def layernorm_fm(P, C, L, yk, n, g, b, out_fn, after=None):
    for k in range(KC):
        yt, ya = yk(k)
        P.op("pe", lambda e, k=k, ya=ya: e.matmul(L.ps_s[:, :n], C.ones_f[:], ya, start=(k == 0), stop=(k == KC - 1)),
             reads=[C.ones_f, yt], writes=[L.ps_s], signal=(k == KC - 1))
    for k in range(KC):
        yt, ya = yk(k)
        sq = L.sq[k % 2]
        P.op("act", lambda e, ya=ya, sq=sq: e.activation(out=sq[:, :n], in_=ya, func=AF.Square),
             reads=[yt], writes=[sq])
        P.op("pe", lambda e, k=k, sq=sq: e.matmul(L.ps_q[:, :n], C.ones_f[:], sq[:, :n], start=(k == 0), stop=(k == KC - 1)),
             reads=[C.ones_f, sq], writes=[L.ps_q], signal=True)
    P.op("dve", lambda e: e.tensor_scalar(L.mean[:, :n], L.ps_s[:, :n], 1.0 / D, None, ALU.mult),
         reads=[L.ps_s], writes=[L.mean])
    t0 = L.tmp[0]
    P.op("dve", lambda e: e.tensor_tensor(t0[:, :n], L.mean[:, :n], L.mean[:, :n], ALU.mult),
         reads=[L.mean], writes=[t0])
    P.op("dve", lambda e: e.scalar_tensor_tensor(L.rstd[:, :n], L.ps_q[:, :n], 1.0 / D, t0[:, :n], ALU.mult, ALU.subtract),
         reads=[L.ps_q, t0], writes=[L.rstd])
    P.op("act", lambda e: e.activation(out=L.rstd[:, :n], in_=L.rstd[:, :n], func=AF.Sqrt, bias=C.eps_col[:, 0:1]),
         reads=[L.rstd, C.eps_col], writes=[L.rstd])
    P.op("dve", lambda e: e.reciprocal(L.rstd[:, :n], L.rstd[:, :n]), reads=[L.rstd], writes=[L.rstd])
    for k in range(KC):
        yt, ya = yk(k)
        tm = L.tmp[k % 2]
        P.op("dve", lambda e, ya=ya, tm=tm: e.tensor_tensor(tm[:, :n], ya, L.mean[:, :n], ALU.subtract),
             reads=[yt, L.mean], writes=[tm])
        P.op("dve", lambda e, tm=tm: e.tensor_tensor(tm[:, :n], tm[:, :n], L.rstd[:, :n], ALU.mult),
             reads=[tm, L.rstd], writes=[tm])
        dt, dap = out_fn(k)
        P.op("act", lambda e, k=k, tm=tm, dap=dap: e.activation(out=dap, in_=tm[:, :n], func=AF.Identity,
                                                               bias=b[:, k:k + 1], scale=g[:, k:k + 1]),
             reads=[tm, g, b], writes=[dt])
        if after is not None:
            after(k, dt)


def blocks_of(n, w=512):
    out = []
    c = 0
    while c < n:
        out.append((c, min(w, n - c)))
        c += w
    return out


FB = 14
TGM = 1152


def ffn_phase(P, C, hin, hout, tok0, ntok, F, g_ap, b_ap, wgu_ap=None, wd_ap=None, moe=None, out_tok0=None):
    nc = P.nc
    if out_tok0 is None:
        out_tok0 = tok0
    NF = F // 128
    with ExitStack() as st:
        hTs = [P.sb(f"f_hT{i}", [128, KC, TGM], BF16, st) for i in range(2)]
        cur = {}
        bg = {"gen": None}

        def tick():
            if bg["gen"] is not None:
                try:
                    next(bg["gen"])
                except StopIteration:
                    bg["gen"] = None

        def flush():
            while bg["gen"] is not None:
                tick()
        yacc = [P.sb(f"f_yacc{k}", [128, TGM], F32, st) for k in range(KC)]
        act = [P.sb(f"f_act{c}", [128, TGM], BF16, st) for c in range(FB)]
        wgs = [P.sb(f"f_wg{i}", [128, KC, 512], BF16, st) for i in range(3)]
        wds = [P.sb(f"f_wd{i}", [128, FB, 256], BF16, st) for i in range(3)]
        stmp = [P.sb(f"f_st{i}", [128, 512], F32, st) for i in range(2)]
        psg = [P.ps(f"f_psg{i}", [128, 512], F32, st) for i in range(2)]
        psu = [P.ps(f"f_psu{i}", [128, 512], F32, st) for i in range(2)]
        psd = [P.ps(f"f_psd{i}", [128, 512], F32, st) for i in range(2)]
        L = LNBufs(P, st, 512)
        ostg = [P.sb(f"f_ostg{i}", [128, 512], F32, st) for i in range(3)]
        lng, lnb = ln_params(P, "f_ln", g_ap, b_ap, st)
        if moe is not None:
            wr = P.sb("f_wr", [128, KC, 8], F32, st)
            with nc.allow_non_contiguous_dma(reason="small router weight"):
                P.dma("sp", wr[:], moe["wr"].rearrange("(k p) e -> p k e", p=128), wr, writes=[wr])
            brb = P.sb("f_brb", [128, 8], F32, st)
            with nc.allow_non_contiguous_dma(reason="bias bcast"):
                P.dma("sp", brb[:], moe["br"].partition_broadcast(128), brb, writes=[brb])
            sel = P.sb("f_sel", [8, 8, 128], F32, st)
            P.dma("sp", sel[:], C.cin["sel"][:], sel, reads=[C.cin["sel"]], writes=[sel])
            gT = P.sb("f_gT", [8, TGM], F32, st)
            gbc = [P.sb(f"f_gbc{i}", [128, TGM], F32, st) for i in range(2)]
            gtmp = [P.sb(f"f_gtmp{i}", [128, 512], F32, st) for i in range(2)]
            rt = {n_: P.sb("f_rt_" + n_, [128, 8], F32, st) for n_ in ("lg", "v", "mk", "ex", "gt")}
            rden = P.sb("f_rden", [128, 1], F32, st)
            negv = P.sb("f_negv", [128, 1], F32, st)
        cnt = {"wg": 0, "wd": 0, "pg": 0, "pd": 0, "st": 0, "gt": 0, "os": 0, "gb": 0}

        groups = []
        t = 0
        first = ntok % 1024 if (ntok % 1024) else 0
        while t < ntok:
            n = min(1024 + (first if t == 0 else 0), ntok - t)
            groups.append((t, n))
            t += n

        def expert(wgu, wd, first_acc, gate_e):
            wgu_v = wgu.rearrange("(k p) c -> p k c", p=128)
            wd_v = wd.rearrange("(c p) d -> p c d", p=128)
            hT = cur["hT"]
            blks = cur["blks"]
            gb = None
            gbox = [None]

            def make_gb():
                gb_ = gbc[cnt["gb"] % 2]
                cnt["gb"] += 1
                for (c0, nb) in blks:
                    ps = L.ps_s
                    P.op("pe", lambda e, c0=c0, nb=nb: e.matmul(ps[:, :nb], sel[:, gate_e, :], gT[:, c0:c0 + nb], start=True, stop=True),
                         reads=[sel, gT], writes=[ps])
                    P.op("act", lambda e, c0=c0, nb=nb: e.copy(gb_[:, c0:c0 + nb], ps[:, :nb]), reads=[ps], writes=[gb_])
                return gb_
            for fb0 in range(0, NF, FB):
                nfb = min(FB, NF - fb0)
                for s0 in range(0, nfb, 2):
                    ns = min(2, nfb - s0)
                    ws = wgs[cnt["wg"] % 3]
                    cnt["wg"] += 1
                    cg = (fb0 + s0) * 128
                    P.dma("pool", ws[:, :, 0:ns * 128], wgu_v[:, :, cg:cg + ns * 128], ws, writes=[ws])
                    P.dma("pool", ws[:, :, 256:256 + ns * 128], wgu_v[:, :, F + cg:F + cg + ns * 128], ws, writes=[ws])
                    for ci in range(ns):
                        a = act[s0 + ci]
                        for (c0, nb) in blks:
                            pg = psg[cnt["pg"] % 2]
                            pu = psu[cnt["pg"] % 2]
                            cnt["pg"] += 1
                            for k in range(KC):
                                P.op("pe", lambda e, k=k, pg=pg, c0=c0, nb=nb, ci=ci, ws=ws: e.matmul(
                                    pg[:, :nb], ws[:, k, ci * 128:(ci + 1) * 128], hT[:, k, c0:c0 + nb],
                                    start=(k == 0), stop=(k == KC - 1)),
                                    reads=[ws, hT], writes=[pg], signal=(k == KC - 1))
                            for k in range(KC):
                                P.op("pe", lambda e, k=k, pu=pu, c0=c0, nb=nb, ci=ci, ws=ws: e.matmul(
                                    pu[:, :nb], ws[:, k, 256 + ci * 128:256 + (ci + 1) * 128], hT[:, k, c0:c0 + nb],
                                    start=(k == 0), stop=(k == KC - 1)),
                                    reads=[ws, hT], writes=[pu], signal=(k == KC - 1))
                            sm = stmp[cnt["st"] % 2]
                            cnt["st"] += 1
                            P.op("act", lambda e, pg=pg, sm=sm, nb=nb: e.activation(out=sm[:, :nb], in_=pg[:, :nb], func=AF.Silu),
                                 reads=[pg], writes=[sm])
                            P.op("dve", lambda e, pu=pu, sm=sm, a=a, c0=c0, nb=nb: e.tensor_tensor(
                                a[:, c0:c0 + nb], sm[:, :nb], pu[:, :nb], ALU.mult),
                                reads=[sm, pu], writes=[a])
                            tick()
                flush()
                if gate_e is not None and gbox[0] is None:
                    gbox[0] = make_gb()
                gb = gbox[0]
                for dp in range(4):
                    wdt = wds[cnt["wd"] % 3]
                    cnt["wd"] += 1
                    P.dma("pool", wdt[:, :nfb, :], wd_v[:, fb0:fb0 + nfb, dp * 256:(dp + 1) * 256], wdt, writes=[wdt])
                    for dc in range(2):
                        kk = dp * 2 + dc
                        for (c0, nb) in blks:
                            pd = psd[cnt["pd"] % 2]
                            cnt["pd"] += 1
                            for c in range(nfb):
                                P.op("pe", lambda e, c=c, pd=pd, c0=c0, nb=nb, dc=dc, wdt=wdt: e.matmul(
                                    pd[:, :nb], wdt[:, c, dc * 128:(dc + 1) * 128], act[c][:, c0:c0 + nb],
                                    start=(c == 0), stop=(c == nfb - 1)),
                                    reads=[wdt, act[c]], writes=[pd], signal=(c == nfb - 1))
                            y = yacc[kk]
                            if gb is None:
                                P.op("dve", lambda e, pd=pd, y=y, c0=c0, nb=nb: e.tensor_tensor(
                                    y[:, c0:c0 + nb], pd[:, :nb], y[:, c0:c0 + nb], ALU.add),
                                    reads=[pd, y], writes=[y])
                            else:
                                gt = gtmp[cnt["gt"] % 2]
                                cnt["gt"] += 1
                                P.op("dve", lambda e, pd=pd, gt=gt, c0=c0, nb=nb: e.tensor_tensor(
                                    gt[:, :nb], pd[:, :nb], gb[:, c0:c0 + nb], ALU.mult),
                                    reads=[pd, gb], writes=[gt])
                                P.op("dve", lambda e, gt=gt, y=y, c0=c0, nb=nb: e.tensor_tensor(
                                    y[:, c0:c0 + nb], gt[:, :nb], y[:, c0:c0 + nb], ALU.add),
                                    reads=[gt, y], writes=[y])

        def prologue(g0, n):
            strk = hin.trk(tok0 + g0, n)
            for k in range(KC):
                P.dma("sp", yacc[k][:, :n], hin.h[k, :, tok0 + g0:tok0 + g0 + n], yacc[k], reads=strk, writes=[yacc[k]])
            yield
            yield
            yield
            if moe is not None:
                for t0 in range(0, n, 128):
                    ps = L.ps_q
                    for k in range(KC):
                        P.op("pe", lambda e, k=k, t0=t0: e.matmul(ps[:, :8], yacc[k][:, t0:t0 + 128], wr[:, k, :],
                                                                 start=(k == 0), stop=(k == KC - 1)),
                             reads=[yacc[k], wr], writes=[ps], signal=(k == KC - 1))
                    lg, v, mk, ex, gt_ = rt["lg"], rt["v"], rt["mk"], rt["ex"], rt["gt"]
                    P.op("dve", lambda e: e.tensor_tensor(lg[:], ps[:, :8], brb[:], ALU.add), reads=[ps, brb], writes=[lg])
                    P.op("dve", lambda e: e.max(v[:], lg[:]), reads=[lg], writes=[v])
                    P.op("dve", lambda e: e.tensor_scalar(mk[:], lg[:], v[:, 1:2], None, ALU.is_ge), reads=[lg, v], writes=[mk])
                    P.op("dve", lambda e: e.tensor_scalar(negv[:], v[:, 0:1], -1.0, None, ALU.mult), reads=[v], writes=[negv])
                    P.op("act", lambda e: e.activation(out=ex[:], in_=lg[:], func=AF.Exp, bias=negv[:, 0:1]),
                         reads=[lg, negv], writes=[ex])
                    P.op("dve", lambda e: e.tensor_tensor(ex[:], ex[:], mk[:], ALU.mult), reads=[ex, mk], writes=[ex])
                    P.op("dve", lambda e: e.reduce_sum(rden[:], ex[:], AX.X), reads=[ex], writes=[rden])
                    P.op("dve", lambda e: e.reciprocal(rden[:], rden[:]), reads=[rden], writes=[rden])
                    P.op("dve", lambda e: e.tensor_scalar(gt_[:], ex[:], rden[:, 0:1], None, ALU.mult), reads=[ex, rden], writes=[gt_])
                    yield
                    pst = L.ps_s
                    P.op("pe", lambda e: e.matmul(pst[:8, :128], gt_[:], C.ident_f[:], start=True, stop=True),
                         reads=[gt_, C.ident_f], writes=[pst])
                    P.op("act", lambda e, t0=t0: e.copy(gT[:, t0:t0 + 128], pst[:8, :128]), reads=[pst], writes=[gT])
                    yield
            for k in range(KC):
                P.op("act", lambda e, k=k: e.mul(yacc[k][:, :n], yacc[k][:, :n], ALPHA),
                     reads=[yacc[k]], writes=[yacc[k]])
            yield

        def epilogue(g0, n):
            for (c0, nb) in blocks_of(n):
                def yk(k, c0=c0, nb=nb):
                    return yacc[k], yacc[k][:, c0:c0 + nb]

                def out_fn(k, nb=nb):
                    o = ostg[cnt["os"] % 3]
                    cnt["os"] += 1
                    return o, o[:, :nb]

                def after(k, o, c0=c0, nb=nb):
                    d0 = out_tok0 + g0 + c0
                    P.dma("sp", hout.h[k, :, d0:d0 + nb], o[:, :nb], o, reads=[o], writes=hout.trk(d0, nb))
                layernorm_fm(P, C, L, yk, nb, lng, lnb, out_fn, after)
                yield

        def chain(*gens):
            for g_ in gens:
                if g_ is not None:
                    yield from g_

        prev_ep = None
        for gi, (g0, n) in enumerate(groups):
            hT = hTs[gi % 2]
            cur["hT"] = hT
            cur["blks"] = blocks_of(n)
            P.dma("pool", hT[:, :, :n], hin.ap(tok0 + g0, n), hT, reads=hin.trk(tok0 + g0, n), writes=[hT])
            bg["gen"] = chain(prev_ep, prologue(g0, n))
            if moe is None:
                expert(wgu_ap, wd_ap, True, None)
            else:
                for ei in range(8):
                    expert(moe["wgu"][ei], moe["wd"][ei], ei == 0, ei)
            prev_ep = epilogue(g0, n)
        bg["gen"] = prev_ep
        flush()
        P.end_phase()


M_OQ, M_OK, M_OV, M_OG, M_OGT = 0, 512, 1024, 2048, 3072
M_IN = 3088


def mlstm_consts(P, C, st):
    ci = C.cin
    C.tri = P.sb("m_tri", [128, 128], F32, st)
    P.dma("sp", C.tri[:], ci["tri"][:], C.tri, reads=[ci["tri"]], writes=[C.tri])
    C.maskT = P.sb("m_maskT", [128, 64], F32, st)
    P.dma("sp", C.maskT[:], ci["maskT"][:], C.maskT, reads=[ci["maskT"]], writes=[C.maskT])
    C.ones_b = P.sb("m_ones_b", [128, 1], BF16, st)
    P.op("dve", lambda e: e.memset(C.ones_b[:], 1.0), writes=[C.ones_b])


def mlstm_phase(P, C, hin, hout, ntok, w_in, b_gate, ln_h, w_out, g_ap, b_ap, rowmask, st_in, st_out, prepass, par=None):
    nc = P.nc
    NT = ntok // 128
    with ExitStack() as st:
        mlstm_consts(P, C, st)
        w_in_v = w_in.rearrange("(k p) c -> p k c", p=128)
        ncol = (512 + 1024 + 16) if prepass else (512 + 1024 + 1024 + 16)
        win = P.sb("m_win", [128, KC, ncol], BF16, st)
        P.dma("pool", win[:, :, 0:512], w_in_v[:, :, M_OK:M_OK + 512], win, writes=[win])
        P.dma("pool", win[:, :, 512:1024], w_in_v[:, :, M_OV:M_OV + 512], win, writes=[win])
        P.dma("pool", win[:, :, 1024:1536], w_in_v[:, :, M_OV + 512:M_OV + 1024], win, writes=[win])
        if prepass:
            GOFF = 1536
        else:
            P.dma("pool", win[:, :, 1536:2048], w_in_v[:, :, M_OG:M_OG + 512], win, writes=[win])
            P.dma("pool", win[:, :, 2048:2560], w_in_v[:, :, M_OG + 512:M_OG + 1024], win, writes=[win])
            GOFF = 2560
        P.dma("pool", win[:, :, GOFF:GOFF + 16], w_in_v[:, :, M_OGT:M_OGT + 16], win, writes=[win])
        if not prepass:
            wq2 = P.sb("m_wq2", [128, KC, 8, 128], BF16, st)
            wk2 = P.sb("m_wk2", [128, KC, 8, 128], BF16, st)
            with nc.allow_non_contiguous_dma(reason="one-time dup weight layout"):
                for (wt, off) in ((wq2, M_OQ), (wk2, M_OK)):
                    for k in range(KC):
                        src = w_in_v[:, k, off:off + 512].rearrange("p (h d) -> p h d", h=8)
                        P.dma("pool", wt[:, k, :, 0:64], src, wt, writes=[wt])
                        P.dma("pool", wt[:, k, :, 64:128], src, wt, writes=[wt])
        bgb = P.sb("m_bgb", [128, 16], F32, st)
        with nc.allow_non_contiguous_dma(reason="bias bcast"):
            P.dma("sp", bgb[:], b_gate.partition_broadcast(128), bgb, writes=[bgb])
        rmask = P.sb("m_rmask", [128, 2], F32, st)
        P.dma("sp", rmask[:], rowmask[:], rmask, reads=[rowmask], writes=[rmask])
        Cst = P.sb("m_C", [128, 8, 128], F32, st)
        nst = P.sb("m_n", [128, 8], F32, st)
        mst = P.sb("m_m", [8, 1], F32, st)
        P.dma("sp", Cst[:], st_in["C"][:], Cst, reads=[st_in["C"]], writes=[Cst])
        P.dma("sp", nst[:], st_in["n"][:], nst, reads=[st_in["n"]], writes=[nst])
        with nc.allow_non_contiguous_dma(reason="8-element state vector"):
            P.dma("sp", mst[:], st_in["m"][:], mst, reads=[st_in["m"]], writes=[mst])
        if par is not None:
            parc = P.sb("m_par", [128, 1], F32, st)
            P.dma("sp", parc[:], par[:], parc, reads=[par], writes=[parc])
            P.op("dve", lambda e: e.tensor_scalar(Cst[:], Cst[:], parc[:, 0:1], None, ALU.mult), reads=[Cst, parc], writes=[Cst])
            P.op("dve", lambda e: e.tensor_scalar(nst[:], nst[:], parc[:, 0:1], None, ALU.mult), reads=[nst, parc], writes=[nst])
            P.op("dve", lambda e: e.tensor_scalar(mst[:], mst[:], parc[:8, 0:1], None, ALU.mult), reads=[mst, parc], writes=[mst])
        Cr = P.sb("m_Cr", [128, 8, 128], F32, st)
        Crb = P.sb("m_Crb", [128, 8, 128], BF16, st)
        nr = P.sb("m_nr", [128, 8], F32, st)
        nrb = P.sb("m_nrb", [128, 8], BF16, st)
        hT = [P.sb(f"m_hT{i}", [128, KC, 128], BF16, st) for i in range(2)]
        ke2 = P.sb("m_ke2", [128, 8, 128], BF16, st)
        vb = P.sb("m_vb", [128, 8, 128], BF16, st)
        sm = {n_: P.sb("m_s_" + n_, [128, 8], F32, st) for n_ in ("li", "lf", "b", "u", "d", "e", "lb", "t")}
        gsb = P.sb("m_gsb", [128, 16], F32, st)
        uT = P.sb("m_uT", [8, 128], F32, st)
        bT = P.sb("m_bT", [8, 128], F32, st)
        umax = P.sb("m_umax", [8, 2], F32, st)
        cc = P.sb("m_cc", [8, 2], F32, st)
        rr = P.sb("m_rr", [8, 2], F32, st)
        cw = P.sb("m_cw", [8, 128], F32, st)
        rw = P.sb("m_rw", [8, 2, 128], F32, st)
        zero8 = P.sb("m_zero8", [8, 128], F32, st)
        P.op("dve", lambda e: e.memset(zero8[:], 0.0), writes=[zero8])
        rbc = P.sb("m_rbc", [128, 2, 8], F32, st)
        pb = [P.ps(f"m_pb{i}", [128, 512], F32, st) for i in range(7)]
        ptb = P.ps("m_ptb", [128, 1024], BF16, st)
        import os
        STOP = os.environ.get("MSTOP", "Z")
        if not prepass:
            wout = P.sb("m_wout", [128, KC, 1024], BF16, st)
            w_out_v = w_out.rearrange("(k p) c -> p k c", p=128)
            for c0 in range(0, 1024, 512):
                P.dma("pool", wout[:, :, c0:c0 + 512], w_out_v[:, :, c0:c0 + 512], wout, writes=[wout])
            lnh = P.sb("m_lnh", [128, 1024], F32, st)
            with nc.allow_non_contiguous_dma(reason="ln_h bcast"):
                P.dma("sp", lnh[:], ln_h.partition_broadcast(128), lnh, writes=[lnh])
            lng, lnb = ln_params(P, "m_ln", g_ap, b_ap, st)
            qT2 = P.sb("m_qT2", [128, 8, 128], BF16, st)
            kT2 = P.sb("m_kT2", [128, 8, 128], BF16, st)
            sg2 = [P.sb(f"m_sg{i}", [128, 1024], F32, st) for i in range(2)]
            PT = P.sb("m_PT", [128, 8, 64], BF16, st)
            stmp = P.sb("m_stmp", [128, 8, 64], F32, st)
            hm = P.sb("m_hm", [128, 8, 128], F32, st)
            hc = P.sb("m_hc", [128, 8, 128], F32, st)
            gated2 = [P.sb(f"m_gated{i}", [128, 1024], BF16, st) for i in range(2)]
            gT = P.sb("m_gT", [128, KC, 128], BF16, st)
            hres = P.sb("m_hres", [128, KC, 128], F32, st)
            ypre = [P.sb(f"m_y{k}", [128, 128], F32, st) for k in range(KC)]
            hs = {n_: P.sb("m_h_" + n_, [128, 8], F32, st) for n_ in ("den", "ri", "mu", "var")}
            L = LNBufs.__new__(LNBufs)
            L.W = 128
            L.sq = [P.sb(f"mln_sq{i}", [128, 128], F32, st) for i in range(2)]
            L.ps_s, L.ps_q = pb[2], pb[3]
            L.mean = P.sb("mln_mean", [128, 128], F32, st)
            L.rstd = P.sb("mln_rstd", [128, 128], F32, st)
            L.tmp = [P.sb(f"mln_tmp{i}", [128, 128], F32, st) for i in range(2)]
            ostg = [P.sb(f"m_ostg{i}", [128, 128], F32, st) for i in range(3)]
        ocnt = [0]

        def proj_tok(ps, ht, c0, n_):
            for k in range(KC):
                P.op("pe", lambda e, k=k: e.matmul(ps[:, :n_], ht[:, k, :], win[:, k, c0:c0 + n_],
                                                   start=(k == 0), stop=(k == KC - 1)),
                     reads=[ht, win], writes=[ps], signal=(k == KC - 1))

        def proj_fm2(ps2, ht, wt, hook=None):
            for h in range(8):
                ps = ps2[h // 4]
                for k in range(KC):
                    P.op("pe", lambda e, k=k, h=h, ps=ps: e.matmul(ps[:, (h % 4) * 128:(h % 4 + 1) * 128], wt[:, k, h, :], ht[:, k, :],
                                                                  start=(k == 0), stop=(k == KC - 1)),
                         reads=[ht, wt], writes=[ps], signal=(k == KC - 1 and h % 4 == 3))
                if hook is not None:
                    hook()

        pending = [None]

        def post(t, gated):
            for k in range(KC):
                P.op("pe", lambda e, k=k: e.transpose(ptb[:, k * 128:(k + 1) * 128], gated[:, k * 128:(k + 1) * 128], C.ident_b[:]),
                     reads=[gated, C.ident_b], writes=[ptb], signal=(k == KC - 1))
            P.op("act", lambda e: e.copy(gT[:], ptb[:].rearrange("p (k t) -> p k t", k=KC)), reads=[ptb], writes=[gT])
            P.dma("sp", hres[:], hin.ap(t * 128, 128), hres, reads=hin.trk(t * 128, 128), writes=[hres])
            yield
            pM = (pb[4], pb[5])
            for dk in range(KC):
                pm = pM[dk // 4]
                for k in range(KC):
                    P.op("pe", lambda e, k=k, dk=dk, pm=pm: e.matmul(pm[:, (dk % 4) * 128:(dk % 4 + 1) * 128], wout[:, k, dk * 128:(dk + 1) * 128],
                                                                    gT[:, k, :], start=(k == 0), stop=(k == KC - 1)),
                         reads=[wout, gT], writes=[pm], signal=(k == KC - 1))
                P.op("dve", lambda e, dk=dk, pm=pm: e.scalar_tensor_tensor(ypre[dk][:], hres[:, dk, :], ALPHA,
                                                                          pm[:, (dk % 4) * 128:(dk % 4 + 1) * 128], ALU.mult, ALU.add),
                     reads=[hres, pm], writes=[ypre[dk]])
                if dk == 3:
                    yield

            def yk(k):
                return ypre[k], ypre[k][:]

            def out_fn(k):
                o = ostg[ocnt[0] % 3]
                ocnt[0] += 1
                return o, o[:]

            def after(k, o, t=t):
                P.dma("sp", hout.h[k, :, t * 128:(t + 1) * 128], o[:], o, reads=[o], writes=hout.trk(t * 128, 128))
            yield
            layernorm_fm(P, C, L, yk, 128, lng, lnb, out_fn, after)

        def outp(t, sg, gated):
            mu, var = hs["mu"], hs["var"]
            P.op("dve", lambda e: e.tensor_reduce(mu[:], hm[:], AX.X, ALU.add), reads=[hm], writes=[mu])
            yield
            P.op("dve", lambda e: e.tensor_scalar(mu[:], mu[:], 1.0 / 128, None, ALU.mult), reads=[mu], writes=[mu])
            yield
            P.op("dve", lambda e: e.tensor_tensor(hc[:], hm[:], mu[:].unsqueeze(2).to_broadcast([128, 8, 128]), ALU.subtract),
                 reads=[hm, mu], writes=[hc])
            yield
            P.op("act", lambda e: e.activation(out=hm[:], in_=hc[:], func=AF.Square), reads=[hc], writes=[hm])
            yield
            P.op("dve", lambda e: e.tensor_reduce(var[:], hm[:], AX.X, ALU.add), reads=[hm], writes=[var])
            yield
            P.op("dve", lambda e: e.tensor_scalar(var[:], var[:], 1.0 / 128, LN_EPS, ALU.mult, ALU.add), reads=[var], writes=[var])
            yield
            P.op("act", lambda e: e.activation(out=var[:], in_=var[:], func=AF.Sqrt), reads=[var], writes=[var])
            yield
            P.op("dve", lambda e: e.reciprocal(var[:], var[:]), reads=[var], writes=[var])
            yield
            P.op("dve", lambda e: e.tensor_tensor(hc[:], hc[:], var[:].unsqueeze(2).to_broadcast([128, 8, 128]), ALU.mult),
                 reads=[hc, var], writes=[hc])
            yield
            hcf = hc[:].rearrange("p h d -> p (h d)")
            P.op("dve", lambda e: e.tensor_tensor(hcf, hcf, lnh[:], ALU.mult), reads=[hc, lnh], writes=[hc])
            yield
            P.op("dve", lambda e: e.tensor_tensor(gated[:], hcf, sg[:], ALU.mult), reads=[hc, sg], writes=[gated])
            yield

        def chain2(a, b):
            yield from a
            yield from b

        def adv(nsteps=2):
            for _ in range(nsteps):
                if pending[0] is not None:
                    try:
                        next(pending[0])
                    except StopIteration:
                        pending[0] = None

        def adv_all():
            while pending[0] is not None:
                adv()

        pg = pb[6]
        pD = pb[6]
        eP = [sm["e"], P.sb("m_s_e1", [128, 8], F32, st)]
        lbP = [sm["lb"], P.sb("m_s_lb1", [128, 8], F32, st)]
        rbcP = [rbc, P.sb("m_rbc1", [128, 2, 8], F32, st)]
        gG = [None]

        def tickG():
            if gG[0] is not None:
                try:
                    next(gG[0])
                except StopIteration:
                    gG[0] = None

        def flushG():
            while gG[0] is not None:
                tickG()

        def G(t):
            ht = hT[t % 2]
            P.dma("pool", ht[:], hin.ap(t * 128, 128), ht, reads=hin.trk(t * 128, 128), writes=[ht])
            yield
            proj_tok(pg, ht, GOFF, 16)
            yield
            P.op("dve", lambda e: e.tensor_tensor(gsb[:], pg[:, :16], bgb[:], ALU.add), reads=[pg, bgb], writes=[gsb])
            yield
            li, lf, b_, u_, d_, tt = (sm[n_] for n_ in ("li", "lf", "b", "u", "d", "t"))
            e_, lb, rbc = eP[t % 2], lbP[t % 2], rbcP[t % 2]
            P.op("act", lambda e: e.activation(out=tt[:], in_=gsb[:, 8:16], func=AF.Exp, scale=-1.0), reads=[gsb], writes=[tt])
            yield
            P.op("act", lambda e: e.activation(out=tt[:], in_=tt[:], func=AF.Ln, bias=1.0), reads=[tt], writes=[tt])
            yield
            if t == 0:
                P.op("dve", lambda e: e.tensor_scalar(lf[:], tt[:], -1.0, rmask[:, 0:1], ALU.mult, ALU.mult), reads=[tt, rmask], writes=[lf])
                yield
                P.op("dve", lambda e: e.tensor_scalar(li[:], gsb[:, 0:8], rmask[:, 0:1], rmask[:, 1:2], ALU.mult, ALU.add),
                     reads=[gsb, rmask], writes=[li])
                yield
            else:
                P.op("dve", lambda e: e.tensor_scalar(lf[:], tt[:], -1.0, None, ALU.mult), reads=[tt], writes=[lf])
                yield
                P.op("dve", lambda e: e.tensor_copy(li[:], gsb[:, 0:8]), reads=[gsb], writes=[li])
                yield
            P.op("pe", lambda e: e.matmul(pg[:, 16:24], C.tri[:], lf[:], start=True, stop=True), reads=[C.tri, lf], writes=[pg])
            yield
            P.op("dve", lambda e: e.tensor_copy(b_[:], pg[:, 16:24]), reads=[pg], writes=[b_])
            yield
            P.op("dve", lambda e: e.tensor_tensor(u_[:], li[:], b_[:], ALU.subtract), reads=[li, b_], writes=[u_])
            yield
            P.op("pe", lambda e: e.matmul(pg[:8, 32:160], u_[:], C.ident_f[:], start=True, stop=True), reads=[u_, C.ident_f], writes=[pg])
            yield
            P.op("act", lambda e: e.copy(uT[:], pg[:8, 32:160]), reads=[pg], writes=[uT])
            yield
            P.op("pe", lambda e: e.matmul(pg[:8, 160:288], b_[:], C.ident_f[:], start=True, stop=True), reads=[b_, C.ident_f], writes=[pg])
            yield
            P.op("act", lambda e: e.copy(bT[:], pg[:8, 160:288]), reads=[pg], writes=[bT])
            yield
            P.op("dve", lambda e: e.tensor_reduce(umax[:], uT[:].rearrange("p (c i) -> p c i", c=2), AX.X, ALU.max),
                 reads=[uT], writes=[umax])
            yield
            for ch in range(2):
                P.op("dve", lambda e, ch=ch: e.tensor_tensor(cc[:, ch:ch + 1], mst[:], umax[:, ch:ch + 1], ALU.max),
                     reads=[mst, umax], writes=[cc])
                yield
                P.op("dve", lambda e, ch=ch: e.tensor_tensor(rr[:, ch:ch + 1], mst[:], cc[:, ch:ch + 1], ALU.subtract),
                     reads=[mst, cc], writes=[rr])
                yield
                P.op("dve", lambda e, ch=ch: e.tensor_tensor(mst[:], cc[:, ch:ch + 1], bT[:, ch * 64 + 63:ch * 64 + 64], ALU.add),
                     reads=[cc, bT], writes=[mst])
                yield
            P.op("act", lambda e: e.activation(out=rr[:], in_=rr[:], func=AF.Exp), reads=[rr], writes=[rr])
            yield
            for ch in range(2):
                P.op("dve", lambda e, ch=ch: e.tensor_scalar(cw[:, ch * 64:(ch + 1) * 64], zero8[:, :64], cc[:, ch:ch + 1], None, ALU.add),
                     reads=[zero8, cc], writes=[cw])
                yield
                P.op("dve", lambda e, ch=ch: e.tensor_scalar(rw[:, ch, :], zero8[:], rr[:, ch:ch + 1], None, ALU.add),
                     reads=[zero8, rr], writes=[rw])
                yield
            P.op("pe", lambda e: e.matmul(pg[:, 24:32], cw[:], C.ident_f[:8, :8], start=True, stop=True), reads=[cw, C.ident_f], writes=[pg])
            yield
            P.op("dve", lambda e: e.tensor_tensor(d_[:], u_[:], pg[:, 24:32], ALU.subtract), reads=[u_, pg], writes=[d_])
            yield
            P.op("act", lambda e: e.activation(out=e_[:], in_=d_[:], func=AF.Exp), reads=[d_], writes=[e_])
            yield
            P.op("dve", lambda e: e.scalar_tensor_tensor(tt[:], b_[:], -1.0, pg[:, 24:32], ALU.mult, ALU.subtract), reads=[b_, pg], writes=[tt])
            yield
            P.op("act", lambda e: e.activation(out=lb[:], in_=tt[:], func=AF.Exp), reads=[tt], writes=[lb])
            yield
            for ch in range(2):
                P.op("pe", lambda e, ch=ch: e.matmul(pg[:, 288 + ch * 8:296 + ch * 8], rw[:, ch, :], C.ident_f[:8, :8], start=True, stop=True),
                     reads=[rw, C.ident_f], writes=[pg])
                yield
            P.op("dve", lambda e: e.tensor_copy(rbc[:].rearrange("p c h -> p (c h)"), pg[:, 288:304]), reads=[pg], writes=[rbc])
            yield


        gG[0] = G(0)
        flushG()
        for t in range(NT):
            ht = hT[t % 2]
            e_, lb, rbc = eP[t % 2], lbP[t % 2], rbcP[t % 2]
            adv()
            pk = pb[4]
            proj_tok(pk, ht, 0, 512)
            for dup in range(2):
                P.op("dve", lambda e, dup=dup: e.tensor_tensor(ke2[:, :, dup * 64:(dup + 1) * 64], pk[:].rearrange("p (h d) -> p h d", h=8),
                                                              e_[:].unsqueeze(2).to_broadcast([128, 8, 64]), ALU.mult),
                     reads=[pk, e_], writes=[ke2])
            for half in range(2):
                pv = pb[5] if half == 0 else pb[4]
                proj_tok(pv, ht, 512 + half * 512, 512)
                P.op("act", lambda e, half=half, pv=pv: e.copy(vb[:, half * 4:(half + 1) * 4, :], pv[:].rearrange("p (h d) -> p h d", h=4)),
                     reads=[pv], writes=[vb])
                adv()
            adv()
            if not prepass:
                proj_fm2((pb[0], pb[1]), ht, wq2, hook=adv)
                for i_ in range(2):
                    P.op("act", lambda e, i_=i_: e.mul(qT2[:, i_ * 4:(i_ + 1) * 4, :], pb[i_][:].rearrange("p (c t) -> p c t", c=4), 0.125),
                         reads=[pb[i_]], writes=[qT2])
                adv()
                proj_fm2((pb[2], pb[3]), ht, wk2)
                for i_ in range(2):
                    P.op("dve", lambda e, i_=i_: e.tensor_copy(kT2[:, i_ * 4:(i_ + 1) * 4, :], pb[2 + i_][:].rearrange("p (c t) -> p c t", c=4)),
                         reads=[pb[2 + i_]], writes=[kT2])
                if STOP == "A0":
                    continue
                pS = pb[5]
                for ch in range(2):
                    rs = slice(ch * 64, (ch + 1) * 64)
                    for h in range(8):
                        P.op("pe", lambda e, rs=rs, h=h: e.matmul(pS[rs, h * 64:(h + 1) * 64], kT2[rs, h, rs], qT2[rs, h, rs],
                                                                 start=True, stop=True),
                             reads=[kT2, qT2], writes=[pS], rowgrp=ch)
                if STOP == "A1":
                    continue
                P.op("dve", lambda e: e.tensor_tensor(stmp[:], pS[:].rearrange("p (h i) -> p h i", h=8),
                                                      e_[:].unsqueeze(2).to_broadcast([128, 8, 64]), ALU.mult),
                     reads=[pS, e_], writes=[stmp])
                P.op("dve", lambda e: e.tensor_tensor(PT[:], stmp[:], C.maskT[:].unsqueeze(1).to_broadcast([128, 8, 64]), ALU.mult),
                     reads=[stmp, C.maskT], writes=[PT])
                if STOP == "A":
                    continue
                sg = sg2[t % 2]
                for half in range(2):
                    po = pb[4] if half == 0 else pb[5]
                    proj_tok(po, ht, 1536 + half * 512, 512)
                    P.op("act", lambda e, half=half, po=po: e.activation(out=sg[:, half * 512:(half + 1) * 512], in_=po[:], func=AF.Sigmoid),
                         reads=[po], writes=[sg])
                    adv()
            adv_all()
            if t + 1 < NT:
                gG[0] = G(t + 1)
            pA = (pb[0], pb[1])
            pC = (pb[2], pb[3])
            for ch in range(2):
                rs = slice(ch * 64, (ch + 1) * 64)
                P.op("dve", lambda e, ch=ch: e.tensor_tensor(Cr[:], Cst[:], rbc[:, ch, :].unsqueeze(2).to_broadcast([128, 8, 128]), ALU.mult),
                     reads=[Cst, rbc], writes=[Cr])
                P.op("dve", lambda e, ch=ch: e.tensor_tensor(nr[:], nst[:], rbc[:, ch, :], ALU.mult), reads=[nst, rbc], writes=[nr])
                tickG()
                if not prepass:
                    P.op("act", lambda e: e.copy(Crb[:], Cr[:]), reads=[Cr], writes=[Crb])
                    P.op("act", lambda e: e.copy(nrb[:], nr[:]), reads=[nr], writes=[nrb])
                    tickG()
                    for h in range(8):
                        pa = pA[h // 4]
                        hh = h % 4
                        P.op("pe", lambda e, h=h, pa=pa, hh=hh, rs=rs: e.matmul(pa[rs, hh * 128:(hh + 1) * 128], PT[rs, h, :], vb[rs, h, :],
                                                                               start=True, stop=False),
                             reads=[PT, vb], writes=[pa], rowgrp=ch)
                        P.op("pe", lambda e, h=h, pa=pa, hh=hh, rs=rs: e.matmul(pa[rs, hh * 128:(hh + 1) * 128], qT2[rs, h, rs], Crb[rs, h, :],
                                                                               start=False, stop=True),
                             reads=[qT2, Crb], writes=[pa], rowgrp=ch)
                        tickG()
                    for h in range(8):
                        P.op("pe", lambda e, h=h, rs=rs: e.matmul(pD[rs, 320 + h:321 + h], PT[rs, h, :], C.ones_b[rs, :], start=True, stop=False),
                             reads=[PT, C.ones_b], writes=[pD], rowgrp=ch)
                        P.op("pe", lambda e, h=h, rs=rs: e.matmul(pD[rs, 320 + h:321 + h], qT2[rs, h, rs], nrb[rs, h:h + 1], start=False, stop=True),
                             reads=[qT2, nrb], writes=[pD], rowgrp=ch)
                        tickG()
                for h in range(8):
                    pc = pC[h // 4]
                    hh = h % 4
                    P.op("pe", lambda e, h=h, pc=pc, hh=hh, rs=rs: e.matmul(pc[:, hh * 128:(hh + 1) * 128], ke2[rs, h, :], vb[rs, h, :],
                                                                           start=True, stop=True),
                         reads=[ke2, vb], writes=[pc], rowgrp=ch)
                    P.op("pe", lambda e, h=h, rs=rs: e.matmul(pD[:, 336 + ch * 8 + h:337 + ch * 8 + h], ke2[rs, h, :], C.ones_b[rs, :],
                                                             start=True, stop=True),
                         reads=[ke2, C.ones_b], writes=[pD], rowgrp=ch)
                    tickG()
                for i_ in range(2):
                    P.op("dve", lambda e, i_=i_: e.tensor_tensor(Cst[:, i_ * 4:(i_ + 1) * 4, :], Cr[:, i_ * 4:(i_ + 1) * 4, :],
                                                                pC[i_][:].rearrange("p (a d) -> p a d", a=4), ALU.add),
                         reads=[Cr, pC[i_]], writes=[Cst])
                P.op("dve", lambda e, ch=ch: e.tensor_tensor(nst[:], nr[:], pD[:, 336 + ch * 8:344 + ch * 8], ALU.add),
                     reads=[nr, pD], writes=[nst])
            if prepass:
                flushG()
                continue
            den, ri, mu, var = hs["den"], hs["ri"], hs["mu"], hs["var"]
            P.op("act", lambda e: e.activation(out=den[:], in_=pD[:, 320:328], func=AF.Abs), reads=[pD], writes=[den])
            P.op("dve", lambda e: e.tensor_tensor(den[:], den[:], lb[:], ALU.max), reads=[den, lb], writes=[den])
            P.op("dve", lambda e: e.reciprocal(ri[:], den[:]), reads=[den], writes=[ri])
            for half in range(2):
                pa = pA[half]
                P.op("dve", lambda e, half=half, pa=pa: e.tensor_tensor(
                    hm[:, half * 4:(half + 1) * 4, :], pa[:].rearrange("p (h d) -> p h d", h=4),
                    ri[:, half * 4:(half + 1) * 4].unsqueeze(2).to_broadcast([128, 4, 128]), ALU.mult),
                    reads=[pa, ri], writes=[hm])
            pending[0] = chain2(outp(t, sg2[t % 2], gated2[t % 2]), post(t, gated2[t % 2]))
            flushG()
        adv_all()
        P.dma("sp", st_out["C"][:], Cst[:], Cst, reads=[Cst], writes=[st_out["C"]])
        P.dma("sp", st_out["n"][:], nst[:], nst, reads=[nst], writes=[st_out["n"]])
        with nc.allow_non_contiguous_dma(reason="8-element state vector"):
            P.dma("sp", st_out["m"][:], mst[:], mst, reads=[mst], writes=[st_out["m"]])
        P.end_phase()


def rope_inplace(P, x3, cs, sn, tmp, nh):
    xt, xa = x3
    c = cs[:].unsqueeze(1).to_broadcast([128, nh, 8])
    s = sn[:].unsqueeze(1).to_broadcast([128, nh, 8])
    x1 = xa[:, :, 0:8]
    x2 = xa[:, :, 8:16]
    t1, t2, t3, t4 = (tmp[:, i, :nh, :] for i in range(4))
    P.op("dve", lambda e: e.tensor_tensor(t1, x1, c, ALU.mult), reads=[xt, cs], writes=[tmp])
    P.op("dve", lambda e: e.tensor_tensor(t2, x2, s, ALU.mult), reads=[xt, sn], writes=[tmp])
    P.op("dve", lambda e: e.tensor_tensor(t3, x2, c, ALU.mult), reads=[xt, cs], writes=[tmp])
    P.op("dve", lambda e: e.tensor_tensor(t4, x1, s, ALU.mult), reads=[xt, sn], writes=[tmp])
    P.op("dve", lambda e: e.tensor_tensor(x1, t1, t2, ALU.subtract), reads=[tmp], writes=[xt])
    P.op("dve", lambda e: e.tensor_tensor(x2, t3, t4, ALU.add), reads=[tmp], writes=[xt])


def kv_phase(P, C, hin, ntok, w_kv, cos_d, sin_d, kt_out, v_out):
    NT = ntok // 128
    with ExitStack() as st:
        wkv = P.sb("k_wkv", [128, KC, 512], BF16, st)
        P.dma("pool", wkv[:], w_kv.rearrange("(k p) c -> p k c", p=128), wkv, writes=[wkv])
        hT = [P.sb(f"k_hT{i}", [128, KC, 128], BF16, st) for i in range(2)]
        cs = [P.sb(f"k_cs{i}", [128, 8], F32, st) for i in range(2)]
        sn = [P.sb(f"k_sn{i}", [128, 8], F32, st) for i in range(2)]
        kf = P.sb("k_kf", [128, 4, 64], F32, st)
        kb = P.sb("k_kb", [128, 256], BF16, st)
        vbt = [P.sb(f"k_vb{i}", [128, 256], BF16, st) for i in range(2)]
        ktb = [P.sb(f"k_ktb{i}", [64, 4, 128], BF16, st) for i in range(2)]
        tmp = P.sb("k_tmp", [128, 4, 16, 8], F32, st)
        ps = [P.ps(f"k_ps{i}", [128, 512], F32, st) for i in range(2)]
        pt = [P.ps(f"k_pt{i}", [128, 1024], BF16, st) for i in range(2)]
        for t in range(NT):
            ht, c_, s_ = hT[t % 2], cs[t % 2], sn[t % 2]
            P.dma("pool", ht[:], hin.ap(t * 128, 128), ht, reads=hin.trk(t * 128, 128), writes=[ht])
            P.dma("sp", c_[:], cos_d[t * 128:(t + 1) * 128, :], c_, reads=[cos_d], writes=[c_])
            P.dma("sp", s_[:], sin_d[t * 128:(t + 1) * 128, :], s_, reads=[sin_d], writes=[s_])
            p_ = ps[t % 2]
            for k in range(KC):
                P.op("pe", lambda e, k=k: e.matmul(p_[:], ht[:, k, :], wkv[:, k, :], start=(k == 0), stop=(k == KC - 1)),
                     reads=[ht, wkv], writes=[p_], signal=(k == KC - 1))
            vt = vbt[t % 2]
            P.op("act", lambda e: e.copy(vt[:], p_[:, 256:512]), reads=[p_], writes=[vt])
            P.op("act", lambda e: e.copy(kf[:], p_[:, 0:256].rearrange("p (h d) -> p h d", h=4)), reads=[p_], writes=[kf])
            rope_inplace(P, (kf, kf[:]), c_, s_, tmp, 4)
            P.op("act", lambda e: e.copy(kb[:], kf[:].rearrange("p h d -> p (h d)")), reads=[kf], writes=[kb])
            ptt = pt[t % 2]
            for g in range(4):
                P.op("pe", lambda e, g=g: e.transpose(ptt[:64, g * 128:(g + 1) * 128], kb[:, g * 64:(g + 1) * 64], C.ident_b[:]),
                     reads=[kb, C.ident_b], writes=[ptt], signal=(g == 3))
            kt = ktb[t % 2]
            P.op("dve", lambda e: e.tensor_copy(kt[:], ptt[:64, 0:512].rearrange("p (g t) -> p g t", g=4)), reads=[ptt], writes=[kt])
            P.dma("sp", kt_out.h[:, :, t * 128:(t + 1) * 128], kt[:], kt, reads=[kt], writes=[kt_out])
            P.dma("sp", v_out.h[t * 128:(t + 1) * 128, :], vt[:], vt, reads=[vt], writes=[v_out])
        P.end_phase()


def swa_phase(P, C, hin, hout, nblk, w_q, sinks, w_o, g_ap, b_ap, cos_d, sin_d, kt_d, v_d, halo, mask_d, mask0_d,
              in_tok0=128, out_tok0=128):
    nc = P.nc
    SCALE = 0.125
    with ExitStack() as st:
        wq = P.sb("a_wq", [128, KC, 1024], BF16, st)
        wo = P.sb("a_wo", [128, KC, 1024], BF16, st)
        for (wt, wsrc) in ((wq, w_q), (wo, w_o)):
            wv = wsrc.rearrange("(k p) c -> p k c", p=128)
            for c0 in range(0, 1024, 512):
                P.dma("pool", wt[:, :, c0:c0 + 512], wv[:, :, c0:c0 + 512], wt, writes=[wt])
        lng, lnb = ln_params(P, "a_ln", g_ap, b_ap, st)
        snk = P.sb("a_snk", [128, 16], F32, st)
        with nc.allow_non_contiguous_dma(reason="sink bcast"):
            P.dma("sp", snk[:], sinks.partition_broadcast(128), snk, writes=[snk])
        mask = P.sb("a_mask", [128, 272], F32, st)
        mask0 = P.sb("a_mask0", [128, 272], F32, st)
        P.dma("sp", mask[:], mask_d[:], mask, reads=[mask_d], writes=[mask])
        P.dma("sp", mask0[:], mask0_d[:], mask0, reads=[mask0_d], writes=[mask0])
        ktm = P.sb("a_ktm", [64, 4, 16], BF16, st)
        vm = P.sb("a_vm", [16, 256], BF16, st)
        P.dma("sp", ktm[:], halo["kt_meta"][:], ktm, reads=[halo["kt_meta"]], writes=[ktm])
        P.dma("sp", vm[:], halo["v_meta"][:], vm, reads=[halo["v_meta"]], writes=[vm])
        ktw = [P.sb(f"a_ktw{i}", [64, 4, 128], BF16, st) for i in range(3)]
        vw = [P.sb(f"a_vw{i}", [128, 256], BF16, st) for i in range(3)]
        P.dma("sp", ktw[0][:], halo["kt_prev"][:], ktw[0], reads=[halo["kt_prev"]], writes=[ktw[0]])
        P.dma("sp", vw[0][:], halo["v_prev"][:], vw[0], reads=[halo["v_prev"]], writes=[vw[0]])
        hT = [P.sb(f"a_hT{i}", [128, KC, 128], BF16, st) for i in range(2)]
        cs = [P.sb(f"a_cs{i}", [128, 8], F32, st) for i in range(2)]
        sn = [P.sb(f"a_sn{i}", [128, 8], F32, st) for i in range(2)]
        qf = P.sb("a_qf", [128, 16, 64], F32, st)
        qb = P.sb("a_qb", [128, 1024], BF16, st)
        qT = P.sb("a_qT", [64, 16, 128], BF16, st)
        tmp = P.sb("a_tmp", [128, 4, 16, 8], F32, st)
        smx = [P.sb(f"a_sm{i}", [128, 272], F32, st) for i in range(2)]
        pb_ = [P.sb(f"a_p{i}", [128, 272], BF16, st) for i in range(2)]
        pT = [P.sb(f"a_pT{i}", [128, 3, 128], BF16, st) for i in range(2)]
        sc = {n_: [P.sb(f"a_c_{n_}{i}", [128, 1], F32, st) for i in range(2)] for n_ in ("mx", "ng", "rs", "ex", "ri")}
        otok = P.sb("a_otok", [128, 1024], BF16, st)
        gT = P.sb("a_gT", [128, KC, 128], BF16, st)
        hres = P.sb("a_hres", [128, KC, 128], F32, st)
        ypre = [P.sb(f"a_y{k}", [128, 128], F32, st) for k in range(KC)]
        ostg = [P.sb(f"a_ostg{i}", [128, 128], F32, st) for i in range(3)]
        pq = [P.ps(f"a_pq{i}", [128, 512], F32, st) for i in range(2)]
        pS = [P.ps(f"a_pS{i}", [128, 512], F32, st) for i in range(2)]
        pV = [P.ps(f"a_pV{i}", [128, 512], F32, st) for i in range(2)]
        ptA = P.ps("a_ptA", [128, 1024], BF16, st)
        ptB = P.ps("a_ptB", [128, 1024], BF16, st)
        L = LNBufs.__new__(LNBufs)
        L.W = 128
        L.sq = [P.sb(f"aln_sq{i}", [128, 128], F32, st) for i in range(2)]
        L.ps_s, L.ps_q = pS[0], pS[1]
        L.mean = P.sb("aln_mean", [128, 128], F32, st)
        L.rstd = P.sb("aln_rstd", [128, 128], F32, st)
        L.tmp = [P.sb(f"aln_tmp{i}", [128, 128], F32, st) for i in range(2)]
        ocnt = [0]
        qf2 = [qf, P.sb("a_qf1", [128, 16, 64], F32, st)]
        qb2 = [qb, P.sb("a_qb1", [128, 1024], BF16, st)]
        qT2 = [qT, P.sb("a_qT1", [64, 16, 128], BF16, st)]
        smx.append(P.sb("a_sm2", [128, 272], F32, st))
        pb_.append(P.sb("a_p2", [128, 272], BF16, st))
        pT.append(P.sb("a_pT2", [128, 3, 128], BF16, st))
        for n_ in sc:
            for i_ in range(2, 8):
                sc[n_].append(P.sb(f"a_c_{n_}{i_}", [128, 1], F32, st))
        otok2 = [otok, P.sb("a_otok1", [128, 1024], BF16, st)]
        gT2 = [gT, P.sb("a_gT1", [128, KC, 128], BF16, st)]
        hres2 = [hres, P.sb("a_hres1", [128, KC, 128], F32, st)]
        ypre2 = [ypre, [P.sb(f"a_y1{k}", [128, 128], F32, st) for k in range(KC)]]
        L.ps_s, L.ps_q = pq[0], pq[1]

        def pre(n):
            t_in = in_tok0 + n * 128
            ht, c_, s_ = hT[n % 2], cs[n % 2], sn[n % 2]
            qf_, qb_, qT_ = qf2[n % 2], qb2[n % 2], qT2[n % 2]
            P.dma("pool", ht[:], hin.ap(t_in, 128), ht, reads=hin.trk(t_in, 128), writes=[ht])
            P.dma("sp", c_[:], cos_d[t_in:t_in + 128, :], c_, reads=[cos_d], writes=[c_])
            P.dma("sp", s_[:], sin_d[t_in:t_in + 128, :], s_, reads=[sin_d], writes=[s_])
            kown, vown = ktw[(n + 1) % 3], vw[(n + 1) % 3]
            P.dma("sp", kown[:], kt_d.h[:, :, t_in:t_in + 128], kown, reads=[kt_d], writes=[kown])
            P.dma("sp", vown[:], v_d.h[t_in:t_in + 128, :], vown, reads=[v_d], writes=[vown])
            for half in range(2):
                for k in range(KC):
                    P.op("pe", lambda e, k=k, half=half: e.matmul(pq[half][:], ht[:, k, :], wq[:, k, half * 512:(half + 1) * 512],
                                                                 start=(k == 0), stop=(k == KC - 1)),
                         reads=[ht, wq], writes=[pq[half]], signal=(k == KC - 1))
                P.op("act", lambda e, half=half: e.copy(qf_[:, half * 8:(half + 1) * 8, :], pq[half][:].rearrange("p (h d) -> p h d", h=8)),
                     reads=[pq[half]], writes=[qf_])
            rope_inplace(P, (qf_, qf_[:]), c_, s_, tmp, 16)
            P.op("act", lambda e: e.copy(qb_[:], qf_[:].rearrange("p h d -> p (h d)")), reads=[qf_], writes=[qb_])
            for half, ptx in enumerate((ptA, ptB)):
                for hh in range(8):
                    h = half * 8 + hh
                    P.op("pe", lambda e, h=h, hh=hh, ptx=ptx: e.transpose(ptx[:64, hh * 128:(hh + 1) * 128], qb_[:, h * 64:(h + 1) * 64], C.ident_b[:]),
                         reads=[qb_, C.ident_b], writes=[ptx], signal=(hh == 7))
                P.op("dve", lambda e, half=half, ptx=ptx: e.tensor_copy(qT_[:, half * 8:(half + 1) * 8, :], ptx[:64, :].rearrange("p (h t) -> p h t", h=8)),
                     reads=[ptx], writes=[qT_])

        def stage_fns(n):
            HB = n * 16
            kprev, vprev = ktw[n % 3], vw[n % 3]
            kown, vown = ktw[(n + 1) % 3], vw[(n + 1) % 3]
            qT_ = qT2[n % 2]
            otok_ = otok2[n % 2]
            mk = mask0 if n == 0 else mask

            def s1(h):
                g = h // 4
                ps_ = pS[h % 2]
                P.op("pe", lambda e: e.matmul(ps_[:, 0:128], qT_[:, h, :], kprev[:, g, :], start=True, stop=True),
                     reads=[qT_, kprev], writes=[ps_], signal=False)
                P.op("pe", lambda e: e.matmul(ps_[:, 128:256], qT_[:, h, :], kown[:, g, :], start=True, stop=True),
                     reads=[qT_, kown], writes=[ps_], signal=False)
                P.op("pe", lambda e: e.matmul(ps_[:, 256:272], qT_[:, h, :], ktm[:, g, :], start=True, stop=True),
                     reads=[qT_, ktm], writes=[ps_], signal=True)

            def s2a(h):
                i3 = (HB + h) % 3
                ps_, sm_ = pS[h % 2], smx[i3]
                mx, ng = sc["mx"][(HB + h) % 8], sc["ng"][(HB + h) % 8]
                P.op("dve", lambda e: e.scalar_tensor_tensor(sm_[:], ps_[:, 0:272], SCALE, mk[:], ALU.mult, ALU.add),
                     reads=[ps_, mk], writes=[sm_])
                P.op("dve", lambda e: e.reduce_max(mx[:], sm_[:], AX.X), reads=[sm_], writes=[mx])
                P.op("dve", lambda e: e.tensor_scalar(ng[:], mx[:], snk[:, h:h + 1], -1.0, ALU.max, ALU.mult),
                     reads=[mx, snk], writes=[ng])

            def s2b(h):
                i3 = (HB + h) % 3
                sm_, p_ = smx[i3], pb_[i3]
                ng, rs_, ex = (sc[n_][(HB + h) % 8] for n_ in ("ng", "rs", "ex"))
                P.op("act", lambda e: e.activation(out=p_[:], in_=sm_[:], func=AF.Exp, bias=ng[:, 0:1], accum_out=rs_[:]),
                     reads=[sm_, ng], writes=[p_, rs_])
                P.op("act", lambda e: e.activation(out=ex[:], in_=snk[:, h:h + 1], func=AF.Exp, bias=ng[:, 0:1]),
                     reads=[snk, ng], writes=[ex])

            def s2c(h):
                i3 = (HB + h) % 3
                rs_, ex, ri = (sc[n_][(HB + h) % 8] for n_ in ("rs", "ex", "ri"))
                P.op("dve", lambda e: e.tensor_tensor(ri[:], rs_[:], ex[:], ALU.add), reads=[rs_, ex], writes=[ri])
                P.op("dve", lambda e: e.reciprocal(ri[:], ri[:]), reads=[ri], writes=[ri])

            def s3a(h):
                p_ = pb_[(HB + h) % 3]
                ptx = ptA if h % 2 == 0 else ptB
                for j in range(2):
                    P.op("pe", lambda e, j=j: e.transpose(ptx[:, j * 128:(j + 1) * 128], p_[:, j * 128:(j + 1) * 128], C.ident_b[:]),
                         reads=[p_, C.ident_b], writes=[ptx], signal=False)
                P.op("pe", lambda e: e.transpose(ptx[:16, 256:384], p_[:, 256:272], C.ident_b[:]),
                     reads=[p_, C.ident_b], writes=[ptx], signal=True)

                pT_ = pT[(HB + h) % 3]
                P.op("act", lambda e: e.copy(pT_[:, 0:2, :], ptx[:, 0:256].rearrange("p (j t) -> p j t", j=2)),
                     reads=[ptx], writes=[pT_])
                P.op("act", lambda e: e.copy(pT_[:16, 2, :], ptx[:16, 256:384]), reads=[ptx], writes=[pT_])

            def s4a(h):
                g = h // 4
                pT_ = pT[(HB + h) % 3]
                pv = pV[h % 2]
                oc = (h // 2) * 64
                P.op("pe", lambda e: e.matmul(pv[:, oc:oc + 64], pT_[:, 0, :], vprev[:, g * 64:(g + 1) * 64], start=True, stop=False),
                     reads=[pT_, vprev], writes=[pv], signal=False)
                P.op("pe", lambda e: e.matmul(pv[:, oc:oc + 64], pT_[:, 1, :], vown[:, g * 64:(g + 1) * 64], start=False, stop=False),
                     reads=[pT_, vown], writes=[pv], signal=False)
                P.op("pe", lambda e: e.matmul(pv[:, oc:oc + 64], pT_[:16, 2, :], vm[:, g * 64:(g + 1) * 64], start=False, stop=True),
                     reads=[pT_, vm], writes=[pv], signal=True)

            def s4b(h):
                ri = sc["ri"][(HB + h) % 8]
                pv = pV[h % 2]
                oc = (h // 2) * 64
                P.op("dve", lambda e: e.tensor_scalar(otok_[:, h * 64:(h + 1) * 64], pv[:, oc:oc + 64], ri[:, 0:1], None, ALU.mult),
                     reads=[pv, ri], writes=[otok_])

            return (s1, s2a, s2b, s2c, s3a, s4a, s4b)

        def post(n):
            t_in = in_tok0 + n * 128
            otok_, gT_, hres_, ypre_ = otok2[n % 2], gT2[n % 2], hres2[n % 2], ypre2[n % 2]
            for k in range(KC):
                P.op("pe", lambda e, k=k: e.transpose(ptA[:, k * 128:(k + 1) * 128], otok_[:, k * 128:(k + 1) * 128], C.ident_b[:]),
                     reads=[otok_, C.ident_b], writes=[ptA], signal=(k == KC - 1))
            P.op("act", lambda e: e.copy(gT_[:], ptA[:].rearrange("p (k t) -> p k t", k=KC)), reads=[ptA], writes=[gT_])
            P.dma("sp", hres_[:], hin.ap(t_in, 128), hres_, reads=hin.trk(t_in, 128), writes=[hres_])
            yield
            for dk in range(KC):
                pm = pq[dk // 4]
                for k in range(KC):
                    P.op("pe", lambda e, k=k, dk=dk, pm=pm: e.matmul(pm[:, (dk % 4) * 128:(dk % 4 + 1) * 128], wo[:, k, dk * 128:(dk + 1) * 128],
                                                                    gT_[:, k, :], start=(k == 0), stop=(k == KC - 1)),
                         reads=[wo, gT_], writes=[pm], signal=(k == KC - 1))
                P.op("dve", lambda e, dk=dk, pm=pm: e.scalar_tensor_tensor(ypre_[dk][:], hres_[:, dk, :], ALPHA,
                                                                          pm[:, (dk % 4) * 128:(dk % 4 + 1) * 128], ALU.mult, ALU.add),
                     reads=[hres_, pm], writes=[ypre_[dk]])
                if dk == 3:
                    yield

            def yk(k):
                return ypre_[k], ypre_[k][:]

            def out_fn(k):
                o = ostg[ocnt[0] % 3]
                ocnt[0] += 1
                return o, o[:]

            def after(k, o, n=n):
                d0 = out_tok0 + n * 128
                P.dma("sp", hout.h[k, :, d0:d0 + 128], o[:], o, reads=[o], writes=hout.trk(d0, 128))
            yield
            layernorm_fm(P, C, L, yk, 128, lng, lnb, out_fn, after)

        NST = 7
        fns = {}
        pre(0)
        posts = []
        total = nblk * 16
        for step in range(total + NST - 1):
            for j in range(NST):
                H = step - j
                if 0 <= H < total:
                    n, h = divmod(H, 16)
                    if n not in fns:
                        fns[n] = stage_fns(n)
                    fns[n][j](h)
                    if j == NST - 1 and h == 15:
                        posts.append(post(n))
                        fns.pop(n - 1, None)
            n_s1, h_s1 = divmod(min(step, total - 1), 16)
            if step < total and h_s1 == 6 and n_s1 + 1 < nblk:
                pre(n_s1 + 1)
            if posts:
                try:
                    next(posts[0])
                except StopIteration:
                    posts.pop(0)
        for g_ in posts:
            for _ in g_:
                pass
        P.end_phase()


NBLK = 16
BLK = 1024
NSLOT = NBLK * BLK
BIGI = 1 << 28


class MoeScratch:
    def __init__(self, P, ntok):
        self.ntok = ntok
        self.htok = P.dram("ms_htok", [ntok, 1024], F32)
        self.xs = P.dram("ms_xs", [NSLOT, 1024], BF16)
        self.aux = P.dram("ms_aux", [NSLOT, 16], F32)
        self.y = [P.dram(f"ms_y{r}", [ntok, 1024], F32) for r in range(2)]
        self.inited = False


def moe_sparse_phase(P, C, S, hin, hout, tok0, ntok, F, g_ap, b_ap, moe, out_tok0=None):
    nc = P.nc
    g = nc.gpsimd
    if out_tok0 is None:
        out_tok0 = tok0
    NT = ntok // 128
    NF = F // 128
    with ExitStack() as st:
        pb = [P.ps(f"x_pb{i}", [128, 512], F32, st) for i in range(7)]
        ptb = P.ps("x_ptb", [128, 1024], BF16, st)
        lng, lnb = ln_params(P, "x_ln", g_ap, b_ap, st)
        wr = P.sb("x_wr", [128, KC, 8], F32, st)
        with nc.allow_non_contiguous_dma(reason="small router weight"):
            P.dma("sp", wr[:], moe["wr"].rearrange("(k p) e -> p k e", p=128), wr, writes=[wr])
        brb = P.sb("x_brb", [128, 8], F32, st)
        with nc.allow_non_contiguous_dma(reason="bias bcast"):
            P.dma("sp", brb[:], moe["br"].partition_broadcast(128), brb, writes=[brb])
        tris = P.sb("x_tris", [128, 128], F32, st)
        P.dma("sp", tris[:], C.cin["tris"][:], tris, reads=[C.cin["tris"]], writes=[tris])
        iot = P.sb("x_iota", [128, 1], F32, st)
        P.dma("sp", iot[:], C.cin["iota"][:], iot, reads=[C.cin["iota"]], writes=[iot])
        M1 = P.sb("x_M1", [128, NT, 8], F32, st)
        M2 = P.sb("x_M2", [128, NT, 8], F32, st)
        POS = P.sb("x_POS", [128, NT, 8], F32, st)
        GG = P.sb("x_GG", [128, NT, 2], F32, st)
        CNT = P.sb("x_CNT", [128, 8], F32, st)
        P.op("dve", lambda e: e.memset(CNT[:], 0.0), writes=[CNT])
        auxinit = P.sb("x_auxinit", [128, 16, 16], F32, st)
        auxinit_i = auxinit[:].bitcast(I32)
        P.op("dve", lambda e: e.memset(auxinit[:], 0.0), writes=[auxinit])
        P.op("dve", lambda e: e.memset(auxinit_i[:, :, 1:3], BIGI), reads=[auxinit], writes=[auxinit])
        aux_v = S.aux.h[:, :].rearrange("(a p s) c -> a p s c", p=128, s=16)
        for a in range(NSLOT // (128 * 16)):
            P.dma("sp", aux_v[a], auxinit[:], auxinit, reads=[auxinit], writes=[S.aux])
        if not S.inited:
            zrow = P.sb("x_zrow", [128, 4, 1024], BF16, st)
            P.op("dve", lambda e: e.memset(zrow[:], 0.0), writes=[zrow])
            xs_v = S.xs.h[:, :].rearrange("(a p s) c -> a p s c", p=128, s=4)
            for a in range(NSLOT // 512):
                P.dma("sp", xs_v[a], zrow[:], zrow, reads=[zrow], writes=[S.xs])
            S.inited = True
        hT = P.sb("x_hT", [128, KC, BLK], BF16, st)
        xrow = P.sb("x_xrow", [128, 8, 1024], BF16, st)
        auxb = P.sb("x_auxb", [128, 8, 16], F32, st)
        act = [P.sb(f"x_act{c}", [128, BLK], BF16, st) for c in range(FB)]
        ytok = [P.sb(f"x_ytok{s}", [128, 1024], F32, st) for s in range(8)]
        wgs = [P.sb(f"x_wg{i}", [128, KC, 512], BF16, st) for i in range(3)]
        wds = [P.sb(f"x_wd{i}", [128, FB, 1024], BF16, st) for i in range(2)]
        stmp = [P.sb(f"x_st{i}", [128, 512], F32, st) for i in range(2)]
        class KV_:
            def __init__(self, t):
                self.t = t
        hfm = [ytok[0], ytok[1]]
        htk = [ytok[2], ytok[3]]
        rt = {n_: P.sb("x_rt_" + n_, [128, 8], F32, st) for n_ in ("lg", "v", "mk")}
        c1 = {n_: P.sb("x_c_" + n_, [128, 1], F32, st) for n_ in ("d", "ex", "g1")}
        for t in range(NT):
            hf, hk = hfm[t % 2], htk[t % 2]
            hfv = hf[:].rearrange("p (k t) -> p k t", k=KC)
            P.dma("sp", hfv, hin.ap(tok0 + t * 128, 128), hf, reads=hin.trk(tok0 + t * 128, 128), writes=[hf])
            ps = pb[0]
            for k in range(KC):
                P.op("pe", lambda e, k=k: e.matmul(ps[:, :8], hfv[:, k, :], wr[:, k, :], start=(k == 0), stop=(k == KC - 1)),
                     reads=[hf, wr], writes=[ps], signal=(k == KC - 1))
            lg, v, mk = rt["lg"], rt["v"], rt["mk"]
            P.op("dve", lambda e: e.tensor_tensor(lg[:], ps[:, :8], brb[:], ALU.add), reads=[ps, brb], writes=[lg])
            P.op("dve", lambda e: e.max(v[:], lg[:]), reads=[lg], writes=[v])
            P.op("dve", lambda e, t=t: e.tensor_scalar(M1[:, t, :], lg[:], v[:, 0:1], None, ALU.is_equal), reads=[lg, v], writes=[M1])
            P.op("dve", lambda e, t=t: e.tensor_scalar(M2[:, t, :], lg[:], v[:, 1:2], None, ALU.is_equal), reads=[lg, v], writes=[M2])
            P.op("dve", lambda e, t=t: e.tensor_tensor(mk[:], M1[:, t, :], M2[:, t, :], ALU.add), reads=[M1, M2], writes=[mk])
            d_, ex, g1 = c1["d"], c1["ex"], c1["g1"]
            P.op("dve", lambda e: e.tensor_tensor(d_[:], v[:, 1:2], v[:, 0:1], ALU.subtract), reads=[v], writes=[d_])
            P.op("act", lambda e: e.activation(out=ex[:], in_=d_[:], func=AF.Exp), reads=[d_], writes=[ex])
            P.op("dve", lambda e: e.tensor_scalar(g1[:], ex[:], 1.0, None, ALU.add), reads=[ex], writes=[g1])
            P.op("dve", lambda e, t=t: e.reciprocal(GG[:, t, 0:1], g1[:]), reads=[g1], writes=[GG])
            P.op("dve", lambda e, t=t: e.tensor_tensor(GG[:, t, 1:2], GG[:, t, 0:1], ex[:], ALU.mult), reads=[GG, ex], writes=[GG])
            P.op("pe", lambda e: e.matmul(ps[:, 8:16], tris[:], mk[:], start=True, stop=True), reads=[tris, mk], writes=[ps])
            P.op("pe", lambda e: e.matmul(ps[:, 16:24], C.ones_f[:], mk[:], start=True, stop=True), reads=[C.ones_f, mk], writes=[ps])
            P.op("dve", lambda e, t=t: e.tensor_tensor(POS[:, t, :], ps[:, 8:16], CNT[:], ALU.add), reads=[ps, CNT], writes=[POS])
            P.op("dve", lambda e: e.tensor_tensor(CNT[:], CNT[:], ps[:, 16:24], ALU.add), reads=[ps, CNT], writes=[CNT])
            for half in range(2):
                pt = pb[1 + half]
                for kk in range(4):
                    k = half * 4 + kk
                    P.op("pe", lambda e, k=k, kk=kk, pt=pt: e.matmul(pt[:, kk * 128:(kk + 1) * 128], hfv[:, k, :], C.ident_f[:], start=True, stop=True),
                         reads=[hf, C.ident_f], writes=[pt], signal=(kk == 3))
                P.op("act", lambda e, half=half, pt=pt: e.copy(hk[:, half * 512:(half + 1) * 512], pt[:]), reads=[pt], writes=[hk])
            P.dma("sp", S.htok.h[tok0 + t * 128:tok0 + (t + 1) * 128, :], hk[:], hk, reads=[hk], writes=[S.htok])
        NB = P.sb("x_NB", [128, 8], F32, st)
        tmp8 = P.sb("x_tmp8", [128, 8], F32, st)
        P.op("dve", lambda e: e.tensor_scalar(NB[:], CNT[:], 0.0, None, ALU.is_gt), reads=[CNT], writes=[NB])
        for kq in range(1, 5):
            P.op("dve", lambda e, kq=kq: e.tensor_scalar(tmp8[:], CNT[:], float(kq * BLK), None, ALU.is_gt), reads=[CNT], writes=[tmp8])
            P.op("dve", lambda e: e.tensor_tensor(NB[:], NB[:], tmp8[:], ALU.add), reads=[NB, tmp8], writes=[NB])
        PST = P.sb("x_PST", [128, 8], F32, st)
        PEND = P.sb("x_PEND", [128, 8], F32, st)
        P.op("dve", lambda e: e.memset(PST[:], 0.0), writes=[PST])
        for e_ in range(8):
            P.op("dve", lambda e, e_=e_: e.scalar_tensor_tensor(PEND[:, e_:e_ + 1], NB[:, e_:e_ + 1], float(BLK), PST[:, e_:e_ + 1], ALU.mult, ALU.add),
                 reads=[NB, PST], writes=[PEND])
            if e_ < 7:
                P.op("dve", lambda e, e_=e_: e.tensor_copy(PST[:, e_ + 1:e_ + 2], PEND[:, e_:e_ + 1]), reads=[PEND], writes=[PST])
        BE = P.sb("x_BE", [128, NBLK], F32, st)
        BEI = P.sb("x_BEI", [128, NBLK], I32, st)
        for b in range(NBLK):
            P.op("dve", lambda e, b=b: e.tensor_scalar(tmp8[:], PEND[:], float(b * BLK), None, ALU.is_le), reads=[PEND], writes=[tmp8])
            P.op("dve", lambda e, b=b: e.reduce_sum(BE[:, b:b + 1], tmp8[:], AX.X), reads=[tmp8], writes=[BE])
        P.op("dve", lambda e: e.tensor_scalar(BE[:], BE[:], 7.0, None, ALU.min), reads=[BE], writes=[BE])
        P.op("dve", lambda e: e.tensor_copy(BEI[:], BE[:]), reads=[BE], writes=[BEI])
        xb = [act[0], act[1]]
        dd = [P.sb(f"x_dd{i}", [128, 2], F32, st) for i in range(2)]
        ddi = [P.sb(f"x_ddi{i}", [128, 2], I32, st) for i in range(2)]
        axr = [P.sb(f"x_axr{i}", [128, 2, 16], F32, st) for i in range(2)]
        tidf = P.sb("x_tidf", [128, 1], F32, st)
        tidi = P.sb("x_tidi", [128, 1], I32, st)

        def ind_scatter(dst_h, idx_ap, src_ap, src_t, idx_t, dst_t, bound):
            for (h, v) in P._waits("pool", [src_t, idx_t], [dst_t]):
                g.wait_ge(h, v)
            if src_t.dsem is None or src_t.dcnt + 16 > SEM_LIMIT:
                P.dma_sem(src_t)
            src_t.dcnt += 16
            g.indirect_dma_start(out=dst_h, out_offset=bass.IndirectOffsetOnAxis(ap=idx_ap, axis=0), in_=src_ap, in_offset=None,
                                 bounds_check=bound, oob_is_err=False).then_inc(src_t.dsem, 16)
            tok = (src_t.dkey, src_t.dsem, src_t.dcnt)
            P._commit(tok, [src_t, idx_t], [dst_t])

        for t in range(NT):
            x_, d2, d2i, ax = xb[t % 2], dd[t % 2], ddi[t % 2], axr[t % 2]
            P.dma("pool", x_[:], S.htok.h[tok0 + t * 128:tok0 + (t + 1) * 128, :], x_, reads=[S.htok], writes=[x_])
            P.op("dve", lambda e, t=t: e.tensor_tensor(tmp8[:], POS[:, t, :], PST[:], ALU.add), reads=[POS, PST], writes=[tmp8])
            for r, Mr in enumerate((M1, M2)):
                P.op("dve", lambda e, t=t, Mr=Mr: e.tensor_tensor(rt["lg"][:], tmp8[:], Mr[:, t, :], ALU.mult), reads=[tmp8, Mr], writes=[rt["lg"]])
                P.op("dve", lambda e, r=r: e.reduce_sum(d2[:, r:r + 1], rt["lg"][:], AX.X), reads=[rt["lg"]], writes=[d2])
            P.op("dve", lambda e: e.tensor_copy(d2i[:], d2[:]), reads=[d2], writes=[d2i])
            P.op("dve", lambda e, t=t: e.tensor_scalar(tidf[:], iot[:], float(tok0 + t * 128), None, ALU.add), reads=[iot], writes=[tidf])
            P.op("dve", lambda e: e.tensor_copy(tidi[:], tidf[:]), reads=[tidf], writes=[tidi])
            axi = ax[:].bitcast(I32)
            P.op("dve", lambda e: e.memset(ax[:], 0.0), writes=[ax])
            P.op("dve", lambda e: e.memset(axi[:, :, 1:3], BIGI), reads=[ax], writes=[ax])
            for r in range(2):
                P.op("dve", lambda e, r=r, t=t: e.tensor_copy(ax[:, r, 0:1], GG[:, t, r:r + 1]), reads=[GG, ax], writes=[ax])
                P.op("dve", lambda e, r=r: e.tensor_copy(axi[:, r, 1 + r:2 + r], tidi[:]), reads=[tidi, ax], writes=[ax])
            for r in range(2):
                ind_scatter(S.xs.h[:, :], d2i[:, r:r + 1], x_[:, :], x_, d2i, S.xs, NSLOT - 1)
                ind_scatter(S.aux.h[:, :], d2i[:, r:r + 1], ax[:, r, :], ax, d2i, S.aux, NSLOT - 1)
        psg = (pb[0], pb[1])
        psu = (pb[2], pb[3])
        psd = (pb[4], pb[5])
        cnt = {"wg": 0, "wd": 0, "pg": 0, "pd": 0, "st": 0}
        wgu0 = moe["wgu"][0].rearrange("(k p) c -> p k c", p=128)
        wd0 = moe["wd"][0].rearrange("(c p) d -> p c d", p=128)
        ESTR_GU = 1024 * 2 * F
        ESTR_D = F * 1024

        def dyn_rows(out2d, tensor, pattern, ebreg, ebase, mult, add, big, col, tile):
            P.nkey += 1
            with g.register(f"x_eb{P.nkey}") as eb, g.register(f"x_ad{P.nkey}") as ad:
                g.reg_load(eb, ebreg)
                g.reg_add(ad, eb, ebase)
                g.reg_mul(ad, ad, mult)
                g.reg_add(ad, ad, add)
                g.reg_mul(ad, ad, big)
                g.reg_add(ad, ad, col)
                v = g.snap(ad, donate=True, min_val=0, max_val=(1 << 28))
                P.dma("pool", out2d, bass.AP(tensor, v, pattern), tile, writes=[tile])

        def tens_off(x):
            return (x.tensor, x.offset) if hasattr(x, "tensor") else (x, 0)
        wgu_t, off_gu = tens_off(moe["wgu"])
        wd_t, off_d = tens_off(moe["wd"])
        eb_gu = off_gu // ESTR_GU
        eb_d = off_d // ESTR_D
        assert off_gu % ESTR_GU == 0 and off_d % ESTR_D == 0

        for b in range(NBLK):
            for (h, v) in P._waits("pool", [BEI], []):
                g.wait_ge(h, v)
            ereg = BEI[0:1, b:b + 1]
            P.dma("sp", xrow[:], S.xs.h[b * BLK:(b + 1) * BLK, :].rearrange("(s p) c -> p s c", p=128), xrow, reads=[S.xs], writes=[xrow])
            P.dma("sp", auxb[:], S.aux.h[b * BLK:(b + 1) * BLK, :].rearrange("(s p) c -> p s c", p=128), auxb, reads=[S.aux], writes=[auxb])
            for k in range(KC):
                for s_ in range(8):
                    P.op("pe", lambda e, k=k, s_=s_: e.transpose(ptb[:, s_ * 128:(s_ + 1) * 128], xrow[:, s_, k * 128:(k + 1) * 128], C.ident_b[:]),
                         reads=[xrow, C.ident_b], writes=[ptb], signal=(s_ == 7))
                eng = "act" if k % 2 == 0 else "dve"
                if eng == "act":
                    P.op("act", lambda e, k=k: e.copy(hT[:, k, :], ptb[:]), reads=[ptb], writes=[hT])
                else:
                    P.op("dve", lambda e, k=k: e.tensor_copy(hT[:, k, :], ptb[:]), reads=[ptb], writes=[hT])
            for fb0 in range(0, NF, FB):
                nfb = min(FB, NF - fb0)
                for s0 in range(0, nfb, 2):
                    ns = min(2, nfb - s0)
                    ws = wgs[cnt["wg"] % 3]
                    cnt["wg"] += 1
                    cg = (fb0 + s0) * 128
                    for k in range(KC):
                        dyn_rows(ws[:, k, 0:ns * 128], wgu_t, [[2 * F, 128], [1, ns * 128]], ereg, eb_gu, KC, k, 128 * 2 * F, cg, ws)
                        dyn_rows(ws[:, k, 256:256 + ns * 128], wgu_t, [[2 * F, 128], [1, ns * 128]], ereg, eb_gu, KC, k, 128 * 2 * F, F + cg, ws)
                    for ci in range(ns):
                        a = act[s0 + ci]
                        for c0 in (0, 512):
                            pg = psg[cnt["pg"] % 2]
                            pu = psu[cnt["pg"] % 2]
                            cnt["pg"] += 1
                            for k in range(KC):
                                P.op("pe", lambda e, k=k, pg=pg, c0=c0, ci=ci, ws=ws: e.matmul(
                                    pg[:], ws[:, k, ci * 128:(ci + 1) * 128], hT[:, k, c0:c0 + 512], start=(k == 0), stop=(k == KC - 1)),
                                    reads=[ws, hT], writes=[pg], signal=(k == KC - 1))
                            for k in range(KC):
                                P.op("pe", lambda e, k=k, pu=pu, c0=c0, ci=ci, ws=ws: e.matmul(
                                    pu[:], ws[:, k, 256 + ci * 128:256 + (ci + 1) * 128], hT[:, k, c0:c0 + 512], start=(k == 0), stop=(k == KC - 1)),
                                    reads=[ws, hT], writes=[pu], signal=(k == KC - 1))
                            sm = stmp[cnt["st"] % 2]
                            cnt["st"] += 1
                            P.op("act", lambda e, pg=pg, sm=sm: e.activation(out=sm[:], in_=pg[:], func=AF.Silu), reads=[pg], writes=[sm])
                            P.op("dve", lambda e, pu=pu, sm=sm, a=a, c0=c0: e.tensor_tensor(a[:, c0:c0 + 512], sm[:], pu[:], ALU.mult),
                                 reads=[sm, pu], writes=[a])
                wdt = wds[cnt["wd"] % 2]
                cnt["wd"] += 1
                for dh in range(2):
                    for c in range(nfb):
                        dyn_rows(wdt[:, c, dh * 512:(dh + 1) * 512], wd_t, [[1024, 128], [1, 512]], ereg, eb_d, NF, fb0 + c, 128 * 1024, dh * 512, wdt)
                for s_ in range(8):
                    for dh in range(2):
                        pd = psd[cnt["pd"] % 2]
                        cnt["pd"] += 1
                        for c in range(nfb):
                            P.op("pe", lambda e, c=c, pd=pd, s_=s_, dh=dh, wdt=wdt: e.matmul(
                                pd[:], act[c][:, s_ * 128:(s_ + 1) * 128], wdt[:, c, dh * 512:(dh + 1) * 512], start=(c == 0), stop=(c == nfb - 1)),
                                reads=[wdt, act[c]], writes=[pd], signal=(c == nfb - 1))
                        y = ytok[s_]
                        if fb0 == 0:
                            P.op("dve", lambda e, pd=pd, y=y, s_=s_, dh=dh: e.tensor_scalar(y[:, dh * 512:(dh + 1) * 512], pd[:], auxb[:, s_, 0:1], None, ALU.mult),
                                 reads=[pd, auxb], writes=[y])
                        else:
                            P.op("dve", lambda e, pd=pd, y=y, s_=s_, dh=dh: e.scalar_tensor_tensor(
                                y[:, dh * 512:(dh + 1) * 512], pd[:], auxb[:, s_, 0:1], y[:, dh * 512:(dh + 1) * 512], ALU.mult, ALU.add),
                                reads=[pd, auxb, y], writes=[y])
            auxbi = auxb[:].bitcast(I32)
            for s_ in range(8):
                for r in range(2):
                    ind_scatter(S.y[r].h[:, :], auxbi[:, s_, 1 + r:2 + r], ytok[s_][:, :], ytok[s_], auxb, S.y[r], S.ntok - 1)
        ya = [ytok[0], ytok[1]]
        yb = [ytok[2], ytok[3]]
        hh = [ytok[4], ytok[5]]
        sq = ytok[6]
        e1 = {n_: P.sb("x_e_" + n_, [128, 1], F32, st) for n_ in ("s", "q", "mu", "m2", "rs")}
        ostg = [ytok[7], P.sb("x_ostg1", [128, 1024], F32, st)]
        for t in range(NT):
            a_, b_, h_ = ya[t % 2], yb[t % 2], hh[t % 2]
            r0 = tok0 + t * 128
            P.dma("sp", a_[:], S.y[0].h[r0:r0 + 128, :], a_, reads=[S.y[0]], writes=[a_])
            P.dma("sp", b_[:], S.y[1].h[r0:r0 + 128, :], b_, reads=[S.y[1]], writes=[b_])
            P.dma("sp", h_[:], S.htok.h[r0:r0 + 128, :], h_, reads=[S.htok], writes=[h_])
            P.op("dve", lambda e: e.tensor_tensor(a_[:], a_[:], b_[:], ALU.add), reads=[a_, b_], writes=[a_])
            P.op("dve", lambda e: e.scalar_tensor_tensor(a_[:], h_[:], ALPHA, a_[:], ALU.mult, ALU.add), reads=[h_, a_], writes=[a_])
            s_, q_, mu, m2, rs = (e1[n_] for n_ in ("s", "q", "mu", "m2", "rs"))
            P.op("dve", lambda e: e.reduce_sum(s_[:], a_[:], AX.X), reads=[a_], writes=[s_])
            P.op("act", lambda e: e.activation(out=sq[:], in_=a_[:], func=AF.Square, accum_out=q_[:]), reads=[a_], writes=[sq, q_])
            P.op("dve", lambda e: e.tensor_scalar(mu[:], s_[:], 1.0 / D, None, ALU.mult), reads=[s_], writes=[mu])
            P.op("dve", lambda e: e.tensor_tensor(m2[:], mu[:], mu[:], ALU.mult), reads=[mu], writes=[m2])
            P.op("dve", lambda e: e.scalar_tensor_tensor(rs[:], q_[:], 1.0 / D, m2[:], ALU.mult, ALU.subtract), reads=[q_, m2], writes=[rs])
            P.op("act", lambda e: e.activation(out=rs[:], in_=rs[:], func=AF.Sqrt, bias=C.eps_col[:, 0:1]), reads=[rs, C.eps_col], writes=[rs])
            P.op("dve", lambda e: e.reciprocal(rs[:], rs[:]), reads=[rs], writes=[rs])
            P.op("dve", lambda e: e.tensor_scalar(b_[:], a_[:], mu[:, 0:1], rs[:, 0:1], ALU.subtract, ALU.mult), reads=[a_, mu, rs], writes=[b_])
            o = ostg[t % 2]
            ov = o[:].rearrange("p (k t) -> p k t", k=KC)
            for half in range(2):
                pt = pb[half]
                for kk in range(4):
                    k = half * 4 + kk
                    P.op("pe", lambda e, k=k, kk=kk, pt=pt: e.matmul(pt[:, kk * 128:(kk + 1) * 128], b_[:, k * 128:(k + 1) * 128], C.ident_f[:], start=True, stop=True),
                         reads=[b_, C.ident_f], writes=[pt], signal=(kk == 3))
                for kk in range(4):
                    k = half * 4 + kk
                    P.op("act", lambda e, k=k, kk=kk, pt=pt: e.activation(out=ov[:, k, :], in_=pt[:, kk * 128:(kk + 1) * 128], func=AF.Identity,
                                                                         bias=lnb[:, k:k + 1], scale=lng[:, k:k + 1]),
                         reads=[pt, lng, lnb], writes=[o])
            d0 = out_tok0 + t * 128
            P.dma("sp", hout.ap(d0, 128), ov, o, reads=[o], writes=hout.trk(d0, 128))
        P.end_phase()


import ml_dtypes

N_CORES = 8
SEQ = 8192
HALF = 4096
NTOK_A = 128 + HALF
NEGM = -30000.0
D_FF = 2816
D_FF_EXP = 3584


def host_consts():
    sel = np.zeros((8, 8, 128), np.float32)
    for e in range(8):
        sel[e, e, :] = 1
    j = np.arange(128)[:, None]
    i = np.arange(128)[None, :]
    tri = ((j // 64 == i // 64) & (j <= i)).astype(np.float32)
    maskT = ((np.arange(128)[:, None] % 64) <= np.arange(64)[None, :]).astype(np.float32)
    qi = np.arange(128)[:, None]
    kj = np.arange(256)[None, :]
    rel = qi + 128 - kj
    band = (rel >= 0) & (rel < 128)
    mask = np.zeros((128, 272), np.float32)
    mask[:, :256] = np.where(band, 0.0, NEGM)
    tris = (j < i).astype(np.float32)
    iota = np.arange(128, dtype=np.float32).reshape(128, 1)
    return {"ones": np.ones((128, 128), np.float32), "ident": np.eye(128, dtype=np.float32), "sel": sel,
            "tri": tri, "maskT": maskT, "mask": mask, "tris": tris, "iota": iota}


CONST_SHAPES = {"ones": [128, 128], "ident": [128, 128], "sel": [8, 8, 128], "tri": [128, 128],
                "maskT": [128, 64], "mask": [128, 272], "tris": [128, 128], "iota": [128, 1]}


def per_core_layout(x, meta):
    cores = []
    half_idx = np.arange(8, dtype=np.float32)
    inv_freq = (np.float32(500000.0) ** (-half_idx * np.float32(2.0) / np.float32(16))).astype(np.float32)
    base_mask = host_consts()["mask"]
    for c in range(N_CORES):
        b, hf = c // 2, c % 2
        tok = np.zeros((NTOK_A, 1024), np.float32)
        pos = np.zeros((NTOK_A,), np.float32)
        rm = np.zeros((128, 2), np.float32)
        if hf == 0:
            tok[112:128] = meta
            pos[112:128] = np.arange(16, dtype=np.float32)
            rm[112:128, 0] = 1.0
        rm[:, 1] = (1.0 - rm[:, 0]) * np.float32(-1e30)
        tok[128:] = x[b, hf * HALF:(hf + 1) * HALF]
        pos[128:] = 16 + hf * HALF + np.arange(HALF, dtype=np.float32)
        ang = (pos[:, None] * inv_freq[None, :]).astype(np.float32)
        m0 = base_mask.copy()
        if hf == 0:
            m0[:, :128] = NEGM
        cores.append({
            "xin": np.ascontiguousarray(tok.T.reshape(8, 128, NTOK_A)),
            "rowmask": rm, "cos": np.cos(ang).astype(np.float32), "sin": np.sin(ang).astype(np.float32),
            "mask0": m0,
        })
    return cores


class Builder:
    def __init__(self):
        self.nc = bass.Bass("TRN2", target_bir_lowering=False)
        self.stack = ExitStack()
        self.P = Prog(self.nc, self.stack)
        self.C = Consts()
        self.ins = {}
        cin = {k: self.inp(k, s) for k, s in CONST_SHAPES.items()}
        load_consts(self.P, self.C, cin)

    def inp(self, name, shape, dt=F32):
        t = self.P.dram(name, shape, dt, kind="ExternalInput")
        self.ins[name] = t
        return t

    def out(self, name, shape, dt=F32):
        return self.P.dram(name, shape, dt, kind="ExternalOutput")

    def act_in(self, name, ntok):
        a = Act(self.P, name, ntok, kind="ExternalInput")
        return a

    def act_out(self, name, ntok):
        return Act(self.P, name, ntok, kind="ExternalOutput")

    def act_tmp(self, name, ntok):
        return Act(self.P, name, ntok)

    def finish(self):
        self.P.finish()
        self.stack.close()
        return self.nc


def state_io(B, prefix, out):
    mk = B.out if out else B.inp
    return {"C": mk(prefix + "C", [128, 8, 128]), "n": mk(prefix + "n", [128, 8]), "m": mk(prefix + "m", [8, 1])}


def run(nc, in_maps):
    res = run_bass_kernel_spmd(nc, in_maps, core_ids=list(range(N_CORES)))
    return res.results


STW = 1040


def state_views(ap2d):
    return {"C": View(ap2d[:, 0:1024].rearrange("p (h d) -> p h d", h=8)),
            "n": View(ap2d[:, 1024:1032]),
            "m": View(ap2d[0:8, 1032:1033])}


def build_fused():
    B = Builder()
    P, C = B.P, B.C
    nc = B.nc
    PAIRS = [[0, 1], [2, 3], [4, 5], [6, 7]]
    xin = B.act_in("xin", NTOK_A)
    rmk = B.inp("rowmask", [128, 2])
    par = B.inp("par", [128, 1])
    cos_d = B.inp("cos", [NTOK_A, 8])
    sin_d = B.inp("sin_t", [NTOK_A, 8])
    m0 = B.inp("mask0", [128, 272])
    zst = B.inp("zstate", [128, STW])
    w_in_a = B.inp("w_in_a", [2, 1024, 3088])
    b_gate_a = B.inp("b_gate_a", [2, 16])
    ln_h_a = B.inp("ln_h_a", [2, 1024])
    w_out_a = B.inp("w_out_a", [2, 1024, 1024])
    w_kv = B.inp("w_kv", [1024, 512])
    w_q_b = B.inp("w_q_b", [2, 1024, 1024])
    sinks_b = B.inp("sinks_b", [2, 16])
    w_o_b = B.inp("w_o_b", [2, 1024, 1024])
    w_gu_d = B.inp("w_gu_d", [2, 1024, 2 * D_FF])
    w_down_d = B.inp("w_down_d", [2, D_FF, 1024])
    w_router = B.inp("w_router", [2, 1024, 8])
    b_router = B.inp("b_router", [2, 8])
    w_gu_e = B.inp("w_gu_e", [2, 8, 1024, 2 * D_FF_EXP])
    w_down_e = B.inp("w_down_e", [2, 8, D_FF_EXP, 1024])
    ln_g = B.inp("ln_g", [4, 2, 1024])
    ln_b = B.inp("ln_b", [4, 2, 1024])
    out = B.act_out("out", HALF)
    hA = B.act_tmp("hA", NTOK_A)
    hB = B.act_tmp("hB", NTOK_A)
    hmid = B.act_tmp("hmid", NTOK_A)
    st_loc = P.dram("st_loc", [128, STW], F32)
    st_all = P.dram("st_all", [256, STW], F32)
    st_dump = P.dram("st_dump", [128, STW], F32)
    kt_d = P.dram("kt_d", [64, 4, NTOK_A], BF16)
    v_d = P.dram("v_d", [NTOK_A, 256], BF16)
    HW = 64 * 4 * 144 + 144 * 256
    halo_loc = P.dram("halo_loc", [1, HW], BF16)
    halo_all = P.dram("halo_all", [2, HW], BF16)

    def ffn(layer, hin, hout, tok0, ntok, out_tok0=None):
        i = layer // 2
        if layer % 2 == 0:
            ffn_phase(P, C, hin, hout, tok0, ntok, D_FF, ln_g.h[layer, 1], ln_b.h[layer, 1],
                      wgu_ap=w_gu_d.h[i], wd_ap=w_down_d.h[i], out_tok0=out_tok0)
        else:
            ffn_phase(P, C, hin, hout, tok0, ntok, D_FF_EXP, ln_g.h[layer, 1], ln_b.h[layer, 1],
                      moe=dict(wr=w_router.h[i], br=b_router.h[i], wgu=w_gu_e.h[i], wd=w_down_e.h[i]), out_tok0=out_tok0)

    h_cur, h_nxt = xin, hA
    for layer in range(2):
        mlstm_phase(P, C, h_cur, None, NTOK_A, w_in_a.h[layer], b_gate_a.h[layer], None, None, None, None, rmk,
                    state_views(zst.h[:, :]), state_views(st_loc.h[:, :]), True)
        P.collective_allgather(st_loc.h.ap().opt(), st_all.h.ap().opt(), PAIRS)
        mlstm_phase(P, C, h_cur, hmid, NTOK_A, w_in_a.h[layer], b_gate_a.h[layer], ln_h_a.h[layer], w_out_a.h[layer],
                    ln_g.h[layer, 0], ln_b.h[layer, 0], rmk, state_views(st_all.h[0:128, :]), state_views(st_dump.h[:, :]),
                    False, par=par)
        ffn(layer, hmid, h_nxt, 0, NTOK_A)
        h_cur, h_nxt = h_nxt, (hB if h_nxt is hA else hA)
    kv_phase(P, C, h_cur, NTOK_A, w_kv[:], cos_d, sin_d, kt_d, v_d)
    hk = halo_loc.h[0, 0:64 * 4 * 144].rearrange("(d g t) -> d g t", d=64, g=4)
    hv = halo_loc.h[0, 64 * 4 * 144:HW].rearrange("(t c) -> t c", c=256)
    with ExitStack() as st:
        kbuf = P.sb("hx_k", [64, 4, 144], BF16, st)
        vbuf = P.sb("hx_v", [128, 2, 256], BF16, st)
        P.dma("sp", kbuf[:, :, 0:16], kt_d.h[:, :, 112:128], kbuf, writes=[kbuf])
        P.dma("sp", kbuf[:, :, 16:144], kt_d.h[:, :, NTOK_A - 128:NTOK_A], kbuf, writes=[kbuf])
        P.dma("sp", hk, kbuf[:], kbuf, reads=[kbuf], writes=[halo_loc])
        P.dma("sp", vbuf[:16, 0, :], v_d.h[112:128, :], vbuf, writes=[vbuf])
        P.dma("sp", vbuf[:, 1, :], v_d.h[NTOK_A - 128:NTOK_A, :], vbuf, writes=[vbuf])
        P.dma("sp", hv[0:16, :], vbuf[:16, 0, :], vbuf, reads=[vbuf], writes=[halo_loc])
        P.dma("sp", hv[16:144, :], vbuf[:, 1, :], vbuf, reads=[vbuf], writes=[halo_loc])
        P.end_phase()
    P.collective_allgather(halo_loc.h.ap().opt(), halo_all.h.ap().opt(), PAIRS)
    rk = halo_all.h[0, 0:64 * 4 * 144].rearrange("(d g t) -> d g t", d=64, g=4)
    rv = halo_all.h[0, 64 * 4 * 144:HW].rearrange("(t c) -> t c", c=256)
    halo = {"kt_meta": View(rk[:, :, 0:16]), "v_meta": View(rv[0:16, :]),
            "kt_prev": View(rk[:, :, 16:144]), "v_prev": View(rv[16:144, :])}
    for layer in range(2, 4):
        j = layer - 2
        swa_phase(P, C, h_cur, hmid, HALF // 128, w_q_b.h[j], sinks_b.h[j], w_o_b.h[j], ln_g.h[layer, 0], ln_b.h[layer, 0],
                  cos_d, sin_d, kt_d, v_d, halo, C.cin["mask"], m0)
        if layer == 3:
            ffn(layer, hmid, out, 128, HALF, out_tok0=0)
        else:
            ffn(layer, hmid, h_nxt, 128, HALF)
            h_cur, h_nxt = h_nxt, (hB if h_nxt is hA else hA)
    return B.finish()


def kernel(x, meta, w_in_a, b_gate_a, ln_h_a, w_out_a, w_kv, w_q_b, sinks_b, w_o_b,
           w_gu_d, w_down_d, w_router, b_router, w_gu_e, w_down_e, ln_g, ln_b):
    f = lambda a: np.ascontiguousarray(np.asarray(a, dtype=np.float32))
    x, meta = f(x), f(meta)
    consts = host_consts()
    lay = per_core_layout(x, meta)
    shared = {"w_in_a": f(w_in_a), "b_gate_a": f(b_gate_a), "ln_h_a": f(ln_h_a), "w_out_a": f(w_out_a), "w_kv": f(w_kv),
              "w_q_b": f(w_q_b), "sinks_b": f(sinks_b), "w_o_b": f(w_o_b), "w_gu_d": f(w_gu_d), "w_down_d": f(w_down_d),
              "w_router": f(w_router), "b_router": f(b_router), "w_gu_e": f(w_gu_e), "w_down_e": f(w_down_e),
              "ln_g": f(ln_g), "ln_b": f(ln_b), "zstate": np.zeros((128, STW), np.float32)}
    nc = build_fused()
    in_maps = []
    for c in range(N_CORES):
        m = dict(consts)
        m.update(shared)
        m.update({"xin": lay[c]["xin"], "rowmask": lay[c]["rowmask"], "cos": lay[c]["cos"], "sin_t": lay[c]["sin"],
                  "mask0": lay[c]["mask0"], "par": np.full((128, 1), float(c % 2), np.float32)})
        in_maps.append(m)
    res = run_bass_kernel_spmd(nc, in_maps, core_ids=list(range(N_CORES))).results
    out = np.empty((4, SEQ, 1024), np.float32)
    for c in range(N_CORES):
        b, hf = c // 2, c % 2
        out[b, hf * HALF:(hf + 1) * HALF] = res[c]["out"].reshape(1024, HALF).T
    return out
```

```python
from concourse.bass_utils import run_bass_kernel_spmd
import numpy as np
from contextlib import ExitStack
import concourse.bass as bass
import concourse.mybir as mybir

F32 = mybir.dt.float32
BF16 = mybir.dt.bfloat16
I32 = mybir.dt.int32
AF = mybir.ActivationFunctionType
ALU = mybir.AluOpType
AX = mybir.AxisListType

SEM_LIMIT = 30000


class T:
    __slots__ = ("h", "name", "w", "r", "dsem", "dkey", "dcnt", "psum", "sems")

    def __init__(self, h, name, psum=False):
        self.h = h
        self.name = name
        self.psum = psum
        self.w = None
        self.r = {}
        self.dsem = None
        self.dkey = None
        self.dcnt = 0
        self.sems = {}

    def __getitem__(self, idx):
        return self.h[idx]


class Prog:
    ENGS = ("pe", "act", "dve", "pool", "sp")

    def __init__(self, nc, stack):
        self.nc = nc
        self.stack = stack
        self.eng = {"pe": nc.tensor, "act": nc.scalar, "dve": nc.vector,
                    "pool": nc.gpsimd, "sp": nc.sync}
        self.nsem = 0
        self.nkey = 0
        self.esem = {}
        self.ekey = {}
        self.ecnt = {}
        self.seen = {e: {} for e in self.ENGS}
        self.all_tok = {}
        for e in self.ENGS:
            self._new_esem(e)
        self.nops = {e: 0 for e in self.ENGS}
        self.last_rowgrp = None
        self.sem_pool = {"sw": [], "hw": []}
        self.dma_tiles = []

    def _sem(self, name):
        self.nsem += 1
        return self.stack.enter_context(self.nc.semaphore(f"{name}_{self.nsem}"))

    def _new_esem(self, e):
        self.esem[e] = self._sem("e" + e)
        self.ekey[e] = ("e", e, self.nsem)
        self.ecnt[e] = 0

    def sb(self, name, shape, dt, stack=None):
        st = stack or self.stack
        self.nkey += 1
        name = f"{name}_{self.nkey}"
        return T(st.enter_context(self.nc.sbuf_tensor(name, list(shape), dt)), name)

    def ps(self, name, shape, dt, stack=None):
        st = stack or self.stack
        self.nkey += 1
        name = f"{name}_{self.nkey}"
        return T(st.enter_context(self.nc.psum_tensor(name, list(shape), dt)), name, psum=True)

    def dram(self, name, shape, dt, kind=None):
        if kind is None:
            return T(self.nc.dram_tensor(name, list(shape), dt), name)
        return T(self.nc.dram_tensor(name, list(shape), dt, kind=kind), name)

    def _waits(self, e, reads, writes):
        need = {}

        def add(tok):
            if tok is None:
                return
            key, h, v = tok
            if self.seen[e].get(key, 0) >= v:
                return
            if key not in need or need[key][1] < v:
                need[key] = (h, v)

        for t in reads:
            add(t.w)
        for t in writes:
            add(t.w)
            for tok in t.r.values():
                add(tok)
        out = []
        for key, (h, v) in need.items():
            if e == "pe" and key == self.ekey["pe"]:
                continue
            self.seen[e][key] = v
            out.append((h, v))
        return out

    def _commit(self, tok, reads, writes):
        key = tok[0]
        for t in writes:
            t.w = tok
            t.r = {}
        for t in reads:
            if t in writes:
                continue
            t.r[key] = tok
        old = self.all_tok.get(key)
        if old is None or old[1] < tok[2]:
            self.all_tok[key] = (tok[1], tok[2])

    def op(self, e, fn, reads=(), writes=(), signal=True, rowgrp=None):
        E = self.eng[e]
        pr = [t for t in reads if t.psum]
        if pr:
            reads = [t for t in reads if not t.psum]
            writes = list(writes) + [t for t in pr if t not in writes]
        if e == "pe":
            if rowgrp != self.last_rowgrp:
                if self.ecnt["pe"] > 0:
                    E.wait_ge(self.esem["pe"], self.ecnt["pe"])
                self.last_rowgrp = rowgrp
            if rowgrp is not None:
                signal = True
        for (h, v) in self._waits(e, reads, writes):
            E.wait_ge(h, v)
        ins = fn(E)
        self.nops[e] += 1
        if signal:
            self.ecnt[e] += 1
            ins.then_inc(self.esem[e], 1)
            tok = (self.ekey[e], self.esem[e], self.ecnt[e])
            self._commit(tok, reads, writes)
            if self.ecnt[e] >= SEM_LIMIT:
                self._new_esem(e)
        else:
            tok = (self.ekey[e], self.esem[e], self.ecnt[e] + 1)
            self._commit(tok, reads, writes)
        return ins

    def dma(self, q, out, in_, sbt, reads=(), writes=(), **kw):
        E = self.eng[q]
        for (h, v) in self._waits(q, reads, writes):
            E.wait_ge(h, v)
        kind = "sw" if q == "pool" else "hw"
        ent = sbt.sems.get(kind)
        if ent is None or ent[2] + 16 > SEM_LIMIT:
            got = None
            pool_ = self.sem_pool[kind]
            while pool_:
                h_, c_ = pool_.pop()
                if c_ + 4096 < SEM_LIMIT:
                    got = (h_, c_)
                    break
            if got is None:
                got = (self._sem("d" + kind), 0)
            self.nkey += 1
            ent = [got[0], ("d", sbt.name, self.nkey), got[1]]
            sbt.sems[kind] = ent
            self.dma_tiles.append((sbt, kind))
        ent[2] += 16
        ins = E.dma_start(out=out, in_=in_, **kw)
        ins.then_inc(ent[0], 16)
        self.nops[q] += 1
        tok = (ent[1], ent[0], ent[2])
        self._commit(tok, reads, writes)
        return ins

    def _unused(self):
        sbt = None
        tok = (sbt.dkey, sbt.dsem, sbt.dcnt)
        self._commit(tok, reads, writes)
        return ins

    def dma_sem(self, sbt):
        got = None
        while self.sem_pool:
            h_, c_ = self.sem_pool.pop()
            if c_ + 4096 < SEM_LIMIT:
                got = (h_, c_)
                break
        if got is None:
            got = (self._sem("d"), 0)
        sbt.dsem, sbt.dcnt = got
        self.nkey += 1
        sbt.dkey = ("d", sbt.name, self.nkey)
        self.dma_tiles.append(sbt)

    def barrier(self, engs=None):
        for e in (engs or self.ENGS):
            E = self.eng[e]
            for key, (h, v) in self.all_tok.items():
                if e == "pe" and key == self.ekey["pe"]:
                    continue
                if self.seen[e].get(key, 0) >= v:
                    continue
                self.seen[e][key] = v
                E.wait_ge(h, v)

    def end_phase(self):
        self.barrier()
        for (t, kind) in self.dma_tiles:
            ent = t.sems.pop(kind, None)
            if ent is not None:
                self.sem_pool[kind].append((ent[0], ent[2]))
        self.dma_tiles = []

    def collective_allgather(self, src_ap, dst_ap, groups):
        self.barrier()
        if not hasattr(self, "ccsem"):
            self.ccsem = self._sem("cc")
            self.cccnt = 0
        E = self.eng["pool"]
        E.collective_compute("AllGather", ALU.bypass, replica_groups=groups, ins=[src_ap], outs=[dst_ap]).then_inc(self.ccsem)
        self.cccnt += 1
        key = ("cc", "cc", 0)
        self.all_tok[key] = (self.ccsem, self.cccnt)
        self.barrier()

    def finish(self):
        self.barrier()


class View:
    def __init__(self, ap, name="view"):
        self.h = ap
        self.name = name
        self.w = None
        self.r = {}
        self.dsem = None
        self.dkey = None
        self.dcnt = 0
        self.psum = False
        self.sems = {}

    def __getitem__(self, idx):
        return self.h


D = 1024
KC = 8
ALPHA = (2.0 * 4) ** 0.25
LN_EPS = 1e-5


class Act:
    def __init__(self, P, name, ntok, kind=None):
        self.ntok = ntok
        self.h = P.dram(name, [KC, 128, ntok], F32, kind=kind).h
        self.tiles = [T(self.h, f"{name}_t{i}") for i in range((ntok + 127) // 128)]
        self.name = name

    def ap(self, t0, n):
        return self.h[:, :, t0:t0 + n].rearrange("k p t -> p k t")

    def trk(self, t0, n):
        return self.tiles[t0 // 128:(t0 + n + 127) // 128]


class Consts:
    pass


def load_consts(P, C, cin):
    C.ones_f = P.sb("ones_f", [128, 128], F32)
    P.dma("sp", C.ones_f[:], cin["ones"][:], C.ones_f, reads=[cin["ones"]], writes=[C.ones_f])
    C.ident_f = P.sb("ident_f", [128, 128], F32)
    P.dma("sp", C.ident_f[:], cin["ident"][:], C.ident_f, reads=[cin["ident"]], writes=[C.ident_f])
    C.cin = cin
    C.eps_col = P.sb("eps_col", [128, 1], F32)
    P.op("dve", lambda e: e.memset(C.eps_col[:], LN_EPS), writes=[C.eps_col])
    C.ident_b = P.sb("ident_b", [128, 128], BF16)
    P.dma("pool", C.ident_b[:], cin["ident"][:], C.ident_b, reads=[cin["ident"]], writes=[C.ident_b])


def ln_params(P, name, g_ap, b_ap, stack):
    g = P.sb(name + "_g", [128, KC], F32, stack)
    b = P.sb(name + "_b", [128, KC], F32, stack)
    with P.nc.allow_non_contiguous_dma(reason="tiny ln param load"):
        P.dma("sp", g[:], g_ap.rearrange("(k p) -> p k", p=128), g, writes=[g])
        P.dma("sp", b[:], b_ap.rearrange("(k p) -> p k", p=128), b, writes=[b])
    return g, b


class LNBufs:
    def __init__(self, P, stack, W):
        self.W = W
        self.sq = [P.sb(f"ln_sq{i}", [128, W], F32, stack) for i in range(2)]
        self.ps_s = P.ps("ln_ps_s", [128, W], F32, stack)
        self.ps_q = P.ps("ln_ps_q", [128, W], F32, stack)
        self.mean = P.sb("ln_mean", [128, W], F32, stack)
        self.rstd = P.sb("ln_rstd", [128, W], F32, stack)
        self.tmp = [P.sb(f"ln_tmp{i}", [128, W], F32, stack) for i in range(2)]
        self.i = 0


def layernorm_fm(P, C, L, yk, n, g, b, out_fn, after=None):
    for k in range(KC):
        yt, ya = yk(k)
        P.op("pe", lambda e, k=k, ya=ya: e.matmul(L.ps_s[:, :n], C.ones_f[:], ya, start=(k == 0), stop=(k == KC - 1)),
             reads=[C.ones_f, yt], writes=[L.ps_s], signal=(k == KC - 1))
    for k in range(KC):
        yt, ya = yk(k)
        sq = L.sq[k % 2]
        P.op("act", lambda e, ya=ya, sq=sq: e.activation(out=sq[:, :n], in_=ya, func=AF.Square),
             reads=[yt], writes=[sq])
        P.op("pe", lambda e, k=k, sq=sq: e.matmul(L.ps_q[:, :n], C.ones_f[:], sq[:, :n], start=(k == 0), stop=(k == KC - 1)),
             reads=[C.ones_f, sq], writes=[L.ps_q], signal=True)
    P.op("dve", lambda e: e.tensor_scalar(L.mean[:, :n], L.ps_s[:, :n], 1.0 / D, None, ALU.mult),
         reads=[L.ps_s], writes=[L.mean])
    t0 = L.tmp[0]
    P.op("dve", lambda e: e.tensor_tensor(t0[:, :n], L.mean[:, :n], L.mean[:, :n], ALU.mult),
         reads=[L.mean], writes=[t0])
    P.op("dve", lambda e: e.scalar_tensor_tensor(L.rstd[:, :n], L.ps_q[:, :n], 1.0 / D, t0[:, :n], ALU.mult, ALU.subtract),
         reads=[L.ps_q, t0], writes=[L.rstd])
    P.op("act", lambda e: e.activation(out=L.rstd[:, :n], in_=L.rstd[:, :n], func=AF.Sqrt, bias=C.eps_col[:, 0:1]),
         reads=[L.rstd, C.eps_col], writes=[L.rstd])
    P.op("dve", lambda e: e.reciprocal(L.rstd[:, :n], L.rstd[:, :n]), reads=[L.rstd], writes=[L.rstd])
    for k in range(KC):
        yt, ya = yk(k)
        tm = L.tmp[k % 2]
        P.op("dve", lambda e, ya=ya, tm=tm: e.tensor_tensor(tm[:, :n], ya, L.mean[:, :n], ALU.subtract),
             reads=[yt, L.mean], writes=[tm])
        P.op("dve", lambda e, tm=tm: e.tensor_tensor(tm[:, :n], tm[:, :n], L.rstd[:, :n], ALU.mult),
             reads=[tm, L.rstd], writes=[tm])
        dt, dap = out_fn(k)
        P.op("act", lambda e, k=k, tm=tm, dap=dap: e.activation(out=dap, in_=tm[:, :n], func=AF.Identity,
                                                               bias=b[:, k:k + 1], scale=g[:, k:k + 1]),
             reads=[tm, g, b], writes=[dt])
        if after is not None:
            after(k, dt)


def blocks_of(n, w=512):
    out = []
    c = 0
    while c < n:
        out.append((c, min(w, n - c)))
        c += w
    return out


FB = 14
TGM = 1152


def ffn_phase(P, C, hin, hout, tok0, ntok, F, g_ap, b_ap, wgu_ap=None, wd_ap=None, moe=None, out_tok0=None):
    nc = P.nc
    if out_tok0 is None:
        out_tok0 = tok0
    NF = F // 128
    with ExitStack() as st:
        hTs = [P.sb(f"f_hT{i}", [128, KC, TGM], BF16, st) for i in range(2)]
        cur = {}
        bg = {"gen": None}

        def tick():
            if bg["gen"] is not None:
                try:
                    next(bg["gen"])
                except StopIteration:
                    bg["gen"] = None

        def flush():
            while bg["gen"] is not None:
                tick()
        yacc = [P.sb(f"f_yacc{k}", [128, TGM], F32, st) for k in range(KC)]
        act = [P.sb(f"f_act{c}", [128, TGM], BF16, st) for c in range(FB)]
        wgs = [P.sb(f"f_wg{i}", [128, KC, 512], BF16, st) for i in range(3)]
        wds = [P.sb(f"f_wd{i}", [128, FB, 256], BF16, st) for i in range(3)]
        stmp = [P.sb(f"f_st{i}", [128, 512], F32, st) for i in range(2)]
        psg = [P.ps(f"f_psg{i}", [128, 512], F32, st) for i in range(2)]
        psu = [P.ps(f"f_psu{i}", [128, 512], F32, st) for i in range(2)]
        psd = [P.ps(f"f_psd{i}", [128, 512], F32, st) for i in range(2)]
        L = LNBufs(P, st, 512)
        ostg = [P.sb(f"f_ostg{i}", [128, 512], F32, st) for i in range(3)]
        lng, lnb = ln_params(P, "f_ln", g_ap, b_ap, st)
        if moe is not None:
            wr = P.sb("f_wr", [128, KC, 8], F32, st)
            with nc.allow_non_contiguous_dma(reason="small router weight"):
                P.dma("sp", wr[:], moe["wr"].rearrange("(k p) e -> p k e", p=128), wr, writes=[wr])
            brb = P.sb("f_brb", [128, 8], F32, st)
            with nc.allow_non_contiguous_dma(reason="bias bcast"):
                P.dma("sp", brb[:], moe["br"].partition_broadcast(128), brb, writes=[brb])
            sel = P.sb("f_sel", [8, 8, 128], F32, st)
            P.dma("sp", sel[:], C.cin["sel"][:], sel, reads=[C.cin["sel"]], writes=[sel])
            gT = P.sb("f_gT", [8, TGM], F32, st)
            gbc = [P.sb(f"f_gbc{i}", [128, TGM], F32, st) for i in range(2)]
            gtmp = [P.sb(f"f_gtmp{i}", [128, 512], F32, st) for i in range(2)]
            rt = {n_: P.sb("f_rt_" + n_, [128, 8], F32, st) for n_ in ("lg", "v", "mk", "ex", "gt")}
            rden = P.sb("f_rden", [128, 1], F32, st)
            negv = P.sb("f_negv", [128, 1], F32, st)
        cnt = {"wg": 0, "wd": 0, "pg": 0, "pd": 0, "st": 0, "gt": 0, "os": 0, "gb": 0}

        groups = []
        t = 0
        first = ntok % 1024 if (ntok % 1024) else 0
        while t < ntok:
            n = min(1024 + (first if t == 0 else 0), ntok - t)
            groups.append((t, n))
            t += n

        def expert(wgu, wd, first_acc, gate_e):
            wgu_v = wgu.rearrange("(k p) c -> p k c", p=128)
            wd_v = wd.rearrange("(c p) d -> p c d", p=128)
            hT = cur["hT"]
            blks = cur["blks"]
            gb = None
            gbox = [None]

            def make_gb():
                gb_ = gbc[cnt["gb"] % 2]
                cnt["gb"] += 1
                for (c0, nb) in blks:
                    ps = L.ps_s
                    P.op("pe", lambda e, c0=c0, nb=nb: e.matmul(ps[:, :nb], sel[:, gate_e, :], gT[:, c0:c0 + nb], start=True, stop=True),
                         reads=[sel, gT], writes=[ps])
                    P.op("act", lambda e, c0=c0, nb=nb: e.copy(gb_[:, c0:c0 + nb], ps[:, :nb]), reads=[ps], writes=[gb_])
                return gb_
            for fb0 in range(0, NF, FB):
                nfb = min(FB, NF - fb0)
                for s0 in range(0, nfb, 2):
                    ns = min(2, nfb - s0)
                    ws = wgs[cnt["wg"] % 3]
                    cnt["wg"] += 1
                    cg = (fb0 + s0) * 128
                    P.dma("pool", ws[:, :, 0:ns * 128], wgu_v[:, :, cg:cg + ns * 128], ws, writes=[ws])
                    P.dma("pool", ws[:, :, 256:256 + ns * 128], wgu_v[:, :, F + cg:F + cg + ns * 128], ws, writes=[ws])
                    for ci in range(ns):
                        a = act[s0 + ci]
                        for (c0, nb) in blks:
                            pg = psg[cnt["pg"] % 2]
                            pu = psu[cnt["pg"] % 2]
                            cnt["pg"] += 1
                            for k in range(KC):
                                P.op("pe", lambda e, k=k, pg=pg, c0=c0, nb=nb, ci=ci, ws=ws: e.matmul(
                                    pg[:, :nb], ws[:, k, ci * 128:(ci + 1) * 128], hT[:, k, c0:c0 + nb],
                                    start=(k == 0), stop=(k == KC - 1)),
                                    reads=[ws, hT], writes=[pg], signal=(k == KC - 1))
                            for k in range(KC):
                                P.op("pe", lambda e, k=k, pu=pu, c0=c0, nb=nb, ci=ci, ws=ws: e.matmul(
                                    pu[:, :nb], ws[:, k, 256 + ci * 128:256 + (ci + 1) * 128], hT[:, k, c0:c0 + nb],
                                    start=(k == 0), stop=(k == KC - 1)),
                                    reads=[ws, hT], writes=[pu], signal=(k == KC - 1))
                            sm = stmp[cnt["st"] % 2]
                            cnt["st"] += 1
                            P.op("act", lambda e, pg=pg, sm=sm, nb=nb: e.activation(out=sm[:, :nb], in_=pg[:, :nb], func=AF.Silu),
                                 reads=[pg], writes=[sm])
                            P.op("dve", lambda e, pu=pu, sm=sm, a=a, c0=c0, nb=nb: e.tensor_tensor(
                                a[:, c0:c0 + nb], sm[:, :nb], pu[:, :nb], ALU.mult),
                                reads=[sm, pu], writes=[a])
                            tick()
                flush()
                if gate_e is not None and gbox[0] is None:
                    gbox[0] = make_gb()
                gb = gbox[0]
                for dp in range(4):
                    wdt = wds[cnt["wd"] % 3]
                    cnt["wd"] += 1
                    P.dma("pool", wdt[:, :nfb, :], wd_v[:, fb0:fb0 + nfb, dp * 256:(dp + 1) * 256], wdt, writes=[wdt])
                    for dc in range(2):
                        kk = dp * 2 + dc
                        for (c0, nb) in blks:
                            pd = psd[cnt["pd"] % 2]
                            cnt["pd"] += 1
                            for c in range(nfb):
                                P.op("pe", lambda e, c=c, pd=pd, c0=c0, nb=nb, dc=dc, wdt=wdt: e.matmul(
                                    pd[:, :nb], wdt[:, c, dc * 128:(dc + 1) * 128], act[c][:, c0:c0 + nb],
                                    start=(c == 0), stop=(c == nfb - 1)),
                                    reads=[wdt, act[c]], writes=[pd], signal=(c == nfb - 1))
                            y = yacc[kk]
                            if gb is None:
                                P.op("dve", lambda e, pd=pd, y=y, c0=c0, nb=nb: e.tensor_tensor(
                                    y[:, c0:c0 + nb], pd[:, :nb], y[:, c0:c0 + nb], ALU.add),
                                    reads=[pd, y], writes=[y])
                            else:
                                gt = gtmp[cnt["gt"] % 2]
                                cnt["gt"] += 1
                                P.op("dve", lambda e, pd=pd, gt=gt, c0=c0, nb=nb: e.tensor_tensor(
                                    gt[:, :nb], pd[:, :nb], gb[:, c0:c0 + nb], ALU.mult),
                                    reads=[pd, gb], writes=[gt])
                                P.op("dve", lambda e, gt=gt, y=y, c0=c0, nb=nb: e.tensor_tensor(
                                    y[:, c0:c0 + nb], gt[:, :nb], y[:, c0:c0 + nb], ALU.add),
                                    reads=[gt, y], writes=[y])

        def prologue(g0, n):
            strk = hin.trk(tok0 + g0, n)
            for k in range(KC):
                P.dma("sp", yacc[k][:, :n], hin.h[k, :, tok0 + g0:tok0 + g0 + n], yacc[k], reads=strk, writes=[yacc[k]])
            yield
            yield
            yield
            if moe is not None:
                for t0 in range(0, n, 128):
                    ps = L.ps_q
                    for k in range(KC):
                        P.op("pe", lambda e, k=k, t0=t0: e.matmul(ps[:, :8], yacc[k][:, t0:t0 + 128], wr[:, k, :],
                                                                 start=(k == 0), stop=(k == KC - 1)),
                             reads=[yacc[k], wr], writes=[ps], signal=(k == KC - 1))
                    lg, v, mk, ex, gt_ = rt["lg"], rt["v"], rt["mk"], rt["ex"], rt["gt"]
                    P.op("dve", lambda e: e.tensor_tensor(lg[:], ps[:, :8], brb[:], ALU.add), reads=[ps, brb], writes=[lg])
                    P.op("dve", lambda e: e.max(v[:], lg[:]), reads=[lg], writes=[v])
                    P.op("dve", lambda e: e.tensor_scalar(mk[:], lg[:], v[:, 1:2], None, ALU.is_ge), reads=[lg, v], writes=[mk])
                    P.op("dve", lambda e: e.tensor_scalar(negv[:], v[:, 0:1], -1.0, None, ALU.mult), reads=[v], writes=[negv])
                    P.op("act", lambda e: e.activation(out=ex[:], in_=lg[:], func=AF.Exp, bias=negv[:, 0:1]),
                         reads=[lg, negv], writes=[ex])
                    P.op("dve", lambda e: e.tensor_tensor(ex[:], ex[:], mk[:], ALU.mult), reads=[ex, mk], writes=[ex])
                    P.op("dve", lambda e: e.reduce_sum(rden[:], ex[:], AX.X), reads=[ex], writes=[rden])
                    P.op("dve", lambda e: e.reciprocal(rden[:], rden[:]), reads=[rden], writes=[rden])
                    P.op("dve", lambda e: e.tensor_scalar(gt_[:], ex[:], rden[:, 0:1], None, ALU.mult), reads=[ex, rden], writes=[gt_])
                    yield
                    pst = L.ps_s
                    P.op("pe", lambda e: e.matmul(pst[:8, :128], gt_[:], C.ident_f[:], start=True, stop=True),
                         reads=[gt_, C.ident_f], writes=[pst])
                    P.op("act", lambda e, t0=t0: e.copy(gT[:, t0:t0 + 128], pst[:8, :128]), reads=[pst], writes=[gT])
                    yield
            for k in range(KC):
                P.op("act", lambda e, k=k: e.mul(yacc[k][:, :n], yacc[k][:, :n], ALPHA),
                     reads=[yacc[k]], writes=[yacc[k]])
            yield

        def epilogue(g0, n):
            for (c0, nb) in blocks_of(n):
                def yk(k, c0=c0, nb=nb):
                    return yacc[k], yacc[k][:, c0:c0 + nb]

                def out_fn(k, nb=nb):
                    o = ostg[cnt["os"] % 3]
                    cnt["os"] += 1
                    return o, o[:, :nb]

                def after(k, o, c0=c0, nb=nb):
                    d0 = out_tok0 + g0 + c0
                    P.dma("sp", hout.h[k, :, d0:d0 + nb], o[:, :nb], o, reads=[o], writes=hout.trk(d0, nb))
                layernorm_fm(P, C, L, yk, nb, lng, lnb, out_fn, after)
                yield

        def chain(*gens):
            for g_ in gens:
                if g_ is not None:
                    yield from g_

        prev_ep = None
        for gi, (g0, n) in enumerate(groups):
            hT = hTs[gi % 2]
            cur["hT"] = hT
            cur["blks"] = blocks_of(n)
            P.dma("pool", hT[:, :, :n], hin.ap(tok0 + g0, n), hT, reads=hin.trk(tok0 + g0, n), writes=[hT])
            bg["gen"] = chain(prev_ep, prologue(g0, n))
            if moe is None:
                expert(wgu_ap, wd_ap, True, None)
            else:
                for ei in range(8):
                    expert(moe["wgu"][ei], moe["wd"][ei], ei == 0, ei)
            prev_ep = epilogue(g0, n)
        bg["gen"] = prev_ep
        flush()
        P.end_phase()


M_OQ, M_OK, M_OV, M_OG, M_OGT = 0, 512, 1024, 2048, 3072
M_IN = 3088


def mlstm_consts(P, C, st):
    ci = C.cin
    C.tri = P.sb("m_tri", [128, 128], F32, st)
    P.dma("sp", C.tri[:], ci["tri"][:], C.tri, reads=[ci["tri"]], writes=[C.tri])
    C.maskT = P.sb("m_maskT", [128, 64], F32, st)
    P.dma("sp", C.maskT[:], ci["maskT"][:], C.maskT, reads=[ci["maskT"]], writes=[C.maskT])
    C.ones_b = P.sb("m_ones_b", [128, 1], BF16, st)
    P.op("dve", lambda e: e.memset(C.ones_b[:], 1.0), writes=[C.ones_b])


def mlstm_phase(P, C, hin, hout, ntok, w_in, b_gate, ln_h, w_out, g_ap, b_ap, rowmask, st_in, st_out, prepass, par=None):
    nc = P.nc
    NT = ntok // 128
    with ExitStack() as st:
        mlstm_consts(P, C, st)
        w_in_v = w_in.rearrange("(k p) c -> p k c", p=128)
        ncol = (512 + 1024 + 16) if prepass else (512 + 1024 + 1024 + 16)
        win = P.sb("m_win", [128, KC, ncol], BF16, st)
        P.dma("pool", win[:, :, 0:512], w_in_v[:, :, M_OK:M_OK + 512], win, writes=[win])
        P.dma("pool", win[:, :, 512:1024], w_in_v[:, :, M_OV:M_OV + 512], win, writes=[win])
        P.dma("pool", win[:, :, 1024:1536], w_in_v[:, :, M_OV + 512:M_OV + 1024], win, writes=[win])
        if prepass:
            GOFF = 1536
        else:
            P.dma("pool", win[:, :, 1536:2048], w_in_v[:, :, M_OG:M_OG + 512], win, writes=[win])
            P.dma("pool", win[:, :, 2048:2560], w_in_v[:, :, M_OG + 512:M_OG + 1024], win, writes=[win])
            GOFF = 2560
        P.dma("pool", win[:, :, GOFF:GOFF + 16], w_in_v[:, :, M_OGT:M_OGT + 16], win, writes=[win])
        if not prepass:
            wq2 = P.sb("m_wq2", [128, KC, 8, 128], BF16, st)
            wk2 = P.sb("m_wk2", [128, KC, 8, 128], BF16, st)
            with nc.allow_non_contiguous_dma(reason="one-time dup weight layout"):
                for (wt, off) in ((wq2, M_OQ), (wk2, M_OK)):
                    for k in range(KC):
                        src = w_in_v[:, k, off:off + 512].rearrange("p (h d) -> p h d", h=8)
                        P.dma("pool", wt[:, k, :, 0:64], src, wt, writes=[wt])
                        P.dma("pool", wt[:, k, :, 64:128], src, wt, writes=[wt])
        bgb = P.sb("m_bgb", [128, 16], F32, st)
        with nc.allow_non_contiguous_dma(reason="bias bcast"):
            P.dma("sp", bgb[:], b_gate.partition_broadcast(128), bgb, writes=[bgb])
        rmask = P.sb("m_rmask", [128, 2], F32, st)
        P.dma("sp", rmask[:], rowmask[:], rmask, reads=[rowmask], writes=[rmask])
        Cst = P.sb("m_C", [128, 8, 128], F32, st)
        nst = P.sb("m_n", [128, 8], F32, st)
        mst = P.sb("m_m", [8, 1], F32, st)
        P.dma("sp", Cst[:], st_in["C"][:], Cst, reads=[st_in["C"]], writes=[Cst])
        P.dma("sp", nst[:], st_in["n"][:], nst, reads=[st_in["n"]], writes=[nst])
        with nc.allow_non_contiguous_dma(reason="8-element state vector"):
            P.dma("sp", mst[:], st_in["m"][:], mst, reads=[st_in["m"]], writes=[mst])
        if par is not None:
            parc = P.sb("m_par", [128, 1], F32, st)
            P.dma("sp", parc[:], par[:], parc, reads=[par], writes=[parc])
            P.op("dve", lambda e: e.tensor_scalar(Cst[:], Cst[:], parc[:, 0:1], None, ALU.mult), reads=[Cst, parc], writes=[Cst])
            P.op("dve", lambda e: e.tensor_scalar(nst[:], nst[:], parc[:, 0:1], None, ALU.mult), reads=[nst, parc], writes=[nst])
            P.op("dve", lambda e: e.tensor_scalar(mst[:], mst[:], parc[:8, 0:1], None, ALU.mult), reads=[mst, parc], writes=[mst])
        Cr = P.sb("m_Cr", [128, 8, 128], F32, st)
        Crb = P.sb("m_Crb", [128, 8, 128], BF16, st)
        nr = P.sb("m_nr", [128, 8], F32, st)
        nrb = P.sb("m_nrb", [128, 8], BF16, st)
        hT = [P.sb(f"m_hT{i}", [128, KC, 128], BF16, st) for i in range(2)]
        ke2 = P.sb("m_ke2", [128, 8, 128], BF16, st)
        vb = P.sb("m_vb", [128, 8, 128], BF16, st)
        sm = {n_: P.sb("m_s_" + n_, [128, 8], F32, st) for n_ in ("li", "lf", "b", "u", "d", "e", "lb", "t")}
        gsb = P.sb("m_gsb", [128, 16], F32, st)
        uT = P.sb("m_uT", [8, 128], F32, st)
        bT = P.sb("m_bT", [8, 128], F32, st)
        umax = P.sb("m_umax", [8, 2], F32, st)
        cc = P.sb("m_cc", [8, 2], F32, st)
        rr = P.sb("m_rr", [8, 2], F32, st)
        cw = P.sb("m_cw", [8, 128], F32, st)
        rw = P.sb("m_rw", [8, 2, 128], F32, st)
        zero8 = P.sb("m_zero8", [8, 128], F32, st)
        P.op("dve", lambda e: e.memset(zero8[:], 0.0), writes=[zero8])
        rbc = P.sb("m_rbc", [128, 2, 8], F32, st)
        pb = [P.ps(f"m_pb{i}", [128, 512], F32, st) for i in range(7)]
        ptb = P.ps("m_ptb", [128, 1024], BF16, st)
        import os
        STOP = os.environ.get("MSTOP", "Z")
        if not prepass:
            wout = P.sb("m_wout", [128, KC, 1024], BF16, st)
            w_out_v = w_out.rearrange("(k p) c -> p k c", p=128)
            for c0 in range(0, 1024, 512):
                P.dma("pool", wout[:, :, c0:c0 + 512], w_out_v[:, :, c0:c0 + 512], wout, writes=[wout])
            lnh = P.sb("m_lnh", [128, 1024], F32, st)
            with nc.allow_non_contiguous_dma(reason="ln_h bcast"):
                P.dma("sp", lnh[:], ln_h.partition_broadcast(128), lnh, writes=[lnh])
            lng, lnb = ln_params(P, "m_ln", g_ap, b_ap, st)
            qT2 = P.sb("m_qT2", [128, 8, 128], BF16, st)
            kT2 = P.sb("m_kT2", [128, 8, 128], BF16, st)
            sg = P.sb("m_sg", [128, 1024], F32, st)
            PT = P.sb("m_PT", [128, 8, 64], BF16, st)
            stmp = P.sb("m_stmp", [128, 8, 64], F32, st)
            hm = P.sb("m_hm", [128, 8, 128], F32, st)
            hc = P.sb("m_hc", [128, 8, 128], F32, st)
            gated2 = [P.sb(f"m_gated{i}", [128, 1024], BF16, st) for i in range(2)]
            gT = P.sb("m_gT", [128, KC, 128], BF16, st)
            hres = P.sb("m_hres", [128, KC, 128], F32, st)
            ypre = [P.sb(f"m_y{k}", [128, 128], F32, st) for k in range(KC)]
            hs = {n_: P.sb("m_h_" + n_, [128, 8], F32, st) for n_ in ("den", "ri", "mu", "var")}
            L = LNBufs.__new__(LNBufs)
            L.W = 128
            L.sq = [P.sb(f"mln_sq{i}", [128, 128], F32, st) for i in range(2)]
            L.ps_s, L.ps_q = pb[2], pb[3]
            L.mean = P.sb("mln_mean", [128, 128], F32, st)
            L.rstd = P.sb("mln_rstd", [128, 128], F32, st)
            L.tmp = [P.sb(f"mln_tmp{i}", [128, 128], F32, st) for i in range(2)]
            ostg = [P.sb(f"m_ostg{i}", [128, 128], F32, st) for i in range(3)]
        ocnt = [0]

        def proj_tok(ps, ht, c0, n_):
            for k in range(KC):
                P.op("pe", lambda e, k=k: e.matmul(ps[:, :n_], ht[:, k, :], win[:, k, c0:c0 + n_],
                                                   start=(k == 0), stop=(k == KC - 1)),
                     reads=[ht, win], writes=[ps], signal=(k == KC - 1))

        def proj_fm2(ps2, ht, wt):
            for h in range(8):
                ps = ps2[h // 4]
                for k in range(KC):
                    P.op("pe", lambda e, k=k, h=h, ps=ps: e.matmul(ps[:, (h % 4) * 128:(h % 4 + 1) * 128], wt[:, k, h, :], ht[:, k, :],
                                                                  start=(k == 0), stop=(k == KC - 1)),
                         reads=[ht, wt], writes=[ps], signal=(k == KC - 1 and h % 4 == 3))

        pending = [None]

        def post(t, gated):
            for k in range(KC):
                P.op("pe", lambda e, k=k: e.transpose(ptb[:, k * 128:(k + 1) * 128], gated[:, k * 128:(k + 1) * 128], C.ident_b[:]),
                     reads=[gated, C.ident_b], writes=[ptb], signal=(k == KC - 1))
            P.op("act", lambda e: e.copy(gT[:], ptb[:].rearrange("p (k t) -> p k t", k=KC)), reads=[ptb], writes=[gT])
            P.dma("sp", hres[:], hin.ap(t * 128, 128), hres, reads=hin.trk(t * 128, 128), writes=[hres])
            yield
            pM = (pb[4], pb[5])
            for dk in range(KC):
                pm = pM[dk // 4]
                for k in range(KC):
                    P.op("pe", lambda e, k=k, dk=dk, pm=pm: e.matmul(pm[:, (dk % 4) * 128:(dk % 4 + 1) * 128], wout[:, k, dk * 128:(dk + 1) * 128],
                                                                    gT[:, k, :], start=(k == 0), stop=(k == KC - 1)),
                         reads=[wout, gT], writes=[pm], signal=(k == KC - 1))
                P.op("dve", lambda e, dk=dk, pm=pm: e.scalar_tensor_tensor(ypre[dk][:], hres[:, dk, :], ALPHA,
                                                                          pm[:, (dk % 4) * 128:(dk % 4 + 1) * 128], ALU.mult, ALU.add),
                     reads=[hres, pm], writes=[ypre[dk]])
                if dk == 3:
                    yield

            def yk(k):
                return ypre[k], ypre[k][:]

            def out_fn(k):
                o = ostg[ocnt[0] % 3]
                ocnt[0] += 1
                return o, o[:]

            def after(k, o, t=t):
                P.dma("sp", hout.h[k, :, t * 128:(t + 1) * 128], o[:], o, reads=[o], writes=hout.trk(t * 128, 128))
            yield
            layernorm_fm(P, C, L, yk, 128, lng, lnb, out_fn, after)

        def adv():
            if pending[0] is not None:
                try:
                    next(pending[0])
                except StopIteration:
                    pending[0] = None

        def adv_all():
            while pending[0] is not None:
                adv()

        pg = pb[6]
        pD = pb[6]
        eP = [sm["e"], P.sb("m_s_e1", [128, 8], F32, st)]
        lbP = [sm["lb"], P.sb("m_s_lb1", [128, 8], F32, st)]
        rbcP = [rbc, P.sb("m_rbc1", [128, 2, 8], F32, st)]
        gG = [None]

        def tickG():
            if gG[0] is not None:
                try:
                    next(gG[0])
                except StopIteration:
                    gG[0] = None

        def flushG():
            while gG[0] is not None:
                tickG()

        def G(t):
            ht = hT[t % 2]
            P.dma("pool", ht[:], hin.ap(t * 128, 128), ht, reads=hin.trk(t * 128, 128), writes=[ht])
            yield
            proj_tok(pg, ht, GOFF, 16)
            yield
            P.op("dve", lambda e: e.tensor_tensor(gsb[:], pg[:, :16], bgb[:], ALU.add), reads=[pg, bgb], writes=[gsb])
            yield
            li, lf, b_, u_, d_, tt = (sm[n_] for n_ in ("li", "lf", "b", "u", "d", "t"))
            e_, lb, rbc = eP[t % 2], lbP[t % 2], rbcP[t % 2]
            P.op("act", lambda e: e.activation(out=tt[:], in_=gsb[:, 8:16], func=AF.Exp, scale=-1.0), reads=[gsb], writes=[tt])
            yield
            P.op("act", lambda e: e.activation(out=tt[:], in_=tt[:], func=AF.Ln, bias=1.0), reads=[tt], writes=[tt])
            yield
            if t == 0:
                P.op("dve", lambda e: e.tensor_scalar(lf[:], tt[:], -1.0, rmask[:, 0:1], ALU.mult, ALU.mult), reads=[tt, rmask], writes=[lf])
                yield
                P.op("dve", lambda e: e.tensor_scalar(li[:], gsb[:, 0:8], rmask[:, 0:1], rmask[:, 1:2], ALU.mult, ALU.add),
                     reads=[gsb, rmask], writes=[li])
                yield
            else:
                P.op("dve", lambda e: e.tensor_scalar(lf[:], tt[:], -1.0, None, ALU.mult), reads=[tt], writes=[lf])
                yield
                P.op("dve", lambda e: e.tensor_copy(li[:], gsb[:, 0:8]), reads=[gsb], writes=[li])
                yield
            P.op("pe", lambda e: e.matmul(pg[:, 16:24], C.tri[:], lf[:], start=True, stop=True), reads=[C.tri, lf], writes=[pg])
            yield
            P.op("dve", lambda e: e.tensor_copy(b_[:], pg[:, 16:24]), reads=[pg], writes=[b_])
            yield
            P.op("dve", lambda e: e.tensor_tensor(u_[:], li[:], b_[:], ALU.subtract), reads=[li, b_], writes=[u_])
            yield
            P.op("pe", lambda e: e.matmul(pg[:8, 32:160], u_[:], C.ident_f[:], start=True, stop=True), reads=[u_, C.ident_f], writes=[pg])
            yield
            P.op("act", lambda e: e.copy(uT[:], pg[:8, 32:160]), reads=[pg], writes=[uT])
            yield
            P.op("pe", lambda e: e.matmul(pg[:8, 160:288], b_[:], C.ident_f[:], start=True, stop=True), reads=[b_, C.ident_f], writes=[pg])
            yield
            P.op("act", lambda e: e.copy(bT[:], pg[:8, 160:288]), reads=[pg], writes=[bT])
            yield
            P.op("dve", lambda e: e.tensor_reduce(umax[:], uT[:].rearrange("p (c i) -> p c i", c=2), AX.X, ALU.max),
                 reads=[uT], writes=[umax])
            yield
            for ch in range(2):
                P.op("dve", lambda e, ch=ch: e.tensor_tensor(cc[:, ch:ch + 1], mst[:], umax[:, ch:ch + 1], ALU.max),
                     reads=[mst, umax], writes=[cc])
                yield
                P.op("dve", lambda e, ch=ch: e.tensor_tensor(rr[:, ch:ch + 1], mst[:], cc[:, ch:ch + 1], ALU.subtract),
                     reads=[mst, cc], writes=[rr])
                yield
                P.op("dve", lambda e, ch=ch: e.tensor_tensor(mst[:], cc[:, ch:ch + 1], bT[:, ch * 64 + 63:ch * 64 + 64], ALU.add),
                     reads=[cc, bT], writes=[mst])
                yield
            P.op("act", lambda e: e.activation(out=rr[:], in_=rr[:], func=AF.Exp), reads=[rr], writes=[rr])
            yield
            for ch in range(2):
                P.op("dve", lambda e, ch=ch: e.tensor_scalar(cw[:, ch * 64:(ch + 1) * 64], zero8[:, :64], cc[:, ch:ch + 1], None, ALU.add),
                     reads=[zero8, cc], writes=[cw])
                yield
                P.op("dve", lambda e, ch=ch: e.tensor_scalar(rw[:, ch, :], zero8[:], rr[:, ch:ch + 1], None, ALU.add),
                     reads=[zero8, rr], writes=[rw])
                yield
            P.op("pe", lambda e: e.matmul(pg[:, 24:32], cw[:], C.ident_f[:8, :8], start=True, stop=True), reads=[cw, C.ident_f], writes=[pg])
            yield
            P.op("dve", lambda e: e.tensor_tensor(d_[:], u_[:], pg[:, 24:32], ALU.subtract), reads=[u_, pg], writes=[d_])
            yield
            P.op("act", lambda e: e.activation(out=e_[:], in_=d_[:], func=AF.Exp), reads=[d_], writes=[e_])
            yield
            P.op("dve", lambda e: e.scalar_tensor_tensor(tt[:], b_[:], -1.0, pg[:, 24:32], ALU.mult, ALU.subtract), reads=[b_, pg], writes=[tt])
            yield
            P.op("act", lambda e: e.activation(out=lb[:], in_=tt[:], func=AF.Exp), reads=[tt], writes=[lb])
            yield
            for ch in range(2):
                P.op("pe", lambda e, ch=ch: e.matmul(pg[:, 288 + ch * 8:296 + ch * 8], rw[:, ch, :], C.ident_f[:8, :8], start=True, stop=True),
                     reads=[rw, C.ident_f], writes=[pg])
                yield
            P.op("dve", lambda e: e.tensor_copy(rbc[:].rearrange("p c h -> p (c h)"), pg[:, 288:304]), reads=[pg], writes=[rbc])
            yield


        gG[0] = G(0)
        flushG()
        for t in range(NT):
            ht = hT[t % 2]
            e_, lb, rbc = eP[t % 2], lbP[t % 2], rbcP[t % 2]
            adv()
            pk = pb[4]
            proj_tok(pk, ht, 0, 512)
            for dup in range(2):
                P.op("dve", lambda e, dup=dup: e.tensor_tensor(ke2[:, :, dup * 64:(dup + 1) * 64], pk[:].rearrange("p (h d) -> p h d", h=8),
                                                              e_[:].unsqueeze(2).to_broadcast([128, 8, 64]), ALU.mult),
                     reads=[pk, e_], writes=[ke2])
            for half in range(2):
                pv = pb[5] if half == 0 else pb[4]
                proj_tok(pv, ht, 512 + half * 512, 512)
                P.op("act", lambda e, half=half, pv=pv: e.copy(vb[:, half * 4:(half + 1) * 4, :], pv[:].rearrange("p (h d) -> p h d", h=4)),
                     reads=[pv], writes=[vb])
            adv()
            if not prepass:
                proj_fm2((pb[0], pb[1]), ht, wq2)
                for i_ in range(2):
                    P.op("act", lambda e, i_=i_: e.mul(qT2[:, i_ * 4:(i_ + 1) * 4, :], pb[i_][:].rearrange("p (c t) -> p c t", c=4), 0.125),
                         reads=[pb[i_]], writes=[qT2])
                adv()
                proj_fm2((pb[2], pb[3]), ht, wk2)
                for i_ in range(2):
                    P.op("dve", lambda e, i_=i_: e.tensor_copy(kT2[:, i_ * 4:(i_ + 1) * 4, :], pb[2 + i_][:].rearrange("p (c t) -> p c t", c=4)),
                         reads=[pb[2 + i_]], writes=[kT2])
                if STOP == "A0":
                    continue
                pS = pb[5]
                for ch in range(2):
                    rs = slice(ch * 64, (ch + 1) * 64)
                    for h in range(8):
                        P.op("pe", lambda e, rs=rs, h=h: e.matmul(pS[rs, h * 64:(h + 1) * 64], kT2[rs, h, rs], qT2[rs, h, rs],
                                                                 start=True, stop=True),
                             reads=[kT2, qT2], writes=[pS], rowgrp=ch)
                if STOP == "A1":
                    continue
                P.op("dve", lambda e: e.tensor_tensor(stmp[:], pS[:].rearrange("p (h i) -> p h i", h=8),
                                                      e_[:].unsqueeze(2).to_broadcast([128, 8, 64]), ALU.mult),
                     reads=[pS, e_], writes=[stmp])
                P.op("dve", lambda e: e.tensor_tensor(PT[:], stmp[:], C.maskT[:].unsqueeze(1).to_broadcast([128, 8, 64]), ALU.mult),
                     reads=[stmp, C.maskT], writes=[PT])
                if STOP == "A":
                    continue
                for half in range(2):
                    po = pb[4] if half == 0 else pb[5]
                    proj_tok(po, ht, 1536 + half * 512, 512)
                    P.op("act", lambda e, half=half, po=po: e.activation(out=sg[:, half * 512:(half + 1) * 512], in_=po[:], func=AF.Sigmoid),
                         reads=[po], writes=[sg])
            adv_all()
            if t + 1 < NT:
                gG[0] = G(t + 1)
            pA = (pb[0], pb[1])
            pC = (pb[2], pb[3])
            for ch in range(2):
                rs = slice(ch * 64, (ch + 1) * 64)
                P.op("dve", lambda e, ch=ch: e.tensor_tensor(Cr[:], Cst[:], rbc[:, ch, :].unsqueeze(2).to_broadcast([128, 8, 128]), ALU.mult),
                     reads=[Cst, rbc], writes=[Cr])
                P.op("dve", lambda e, ch=ch: e.tensor_tensor(nr[:], nst[:], rbc[:, ch, :], ALU.mult), reads=[nst, rbc], writes=[nr])
                tickG()
                if not prepass:
                    P.op("act", lambda e: e.copy(Crb[:], Cr[:]), reads=[Cr], writes=[Crb])
                    P.op("act", lambda e: e.copy(nrb[:], nr[:]), reads=[nr], writes=[nrb])
                    tickG()
                    for h in range(8):
                        pa = pA[h // 4]
                        hh = h % 4
                        P.op("pe", lambda e, h=h, pa=pa, hh=hh, rs=rs: e.matmul(pa[rs, hh * 128:(hh + 1) * 128], PT[rs, h, :], vb[rs, h, :],
                                                                               start=True, stop=False),
                             reads=[PT, vb], writes=[pa], rowgrp=ch)
                        P.op("pe", lambda e, h=h, pa=pa, hh=hh, rs=rs: e.matmul(pa[rs, hh * 128:(hh + 1) * 128], qT2[rs, h, rs], Crb[rs, h, :],
                                                                               start=False, stop=True),
                             reads=[qT2, Crb], writes=[pa], rowgrp=ch)
                        tickG()
                    for h in range(8):
                        P.op("pe", lambda e, h=h, rs=rs: e.matmul(pD[rs, 320 + h:321 + h], PT[rs, h, :], C.ones_b[rs, :], start=True, stop=False),
                             reads=[PT, C.ones_b], writes=[pD], rowgrp=ch)
                        P.op("pe", lambda e, h=h, rs=rs: e.matmul(pD[rs, 320 + h:321 + h], qT2[rs, h, rs], nrb[rs, h:h + 1], start=False, stop=True),
                             reads=[qT2, nrb], writes=[pD], rowgrp=ch)
                        tickG()
                for h in range(8):
                    pc = pC[h // 4]
                    hh = h % 4
                    P.op("pe", lambda e, h=h, pc=pc, hh=hh, rs=rs: e.matmul(pc[:, hh * 128:(hh + 1) * 128], ke2[rs, h, :], vb[rs, h, :],
                                                                           start=True, stop=True),
                         reads=[ke2, vb], writes=[pc], rowgrp=ch)
                    P.op("pe", lambda e, h=h, rs=rs: e.matmul(pD[:, 336 + ch * 8 + h:337 + ch * 8 + h], ke2[rs, h, :], C.ones_b[rs, :],
                                                             start=True, stop=True),
                         reads=[ke2, C.ones_b], writes=[pD], rowgrp=ch)
                    tickG()
                for i_ in range(2):
                    P.op("dve", lambda e, i_=i_: e.tensor_tensor(Cst[:, i_ * 4:(i_ + 1) * 4, :], Cr[:, i_ * 4:(i_ + 1) * 4, :],
                                                                pC[i_][:].rearrange("p (a d) -> p a d", a=4), ALU.add),
                         reads=[Cr, pC[i_]], writes=[Cst])
                P.op("dve", lambda e, ch=ch: e.tensor_tensor(nst[:], nr[:], pD[:, 336 + ch * 8:344 + ch * 8], ALU.add),
                     reads=[nr, pD], writes=[nst])
            if prepass:
                flushG()
                continue
            den, ri, mu, var = hs["den"], hs["ri"], hs["mu"], hs["var"]
            P.op("act", lambda e: e.activation(out=den[:], in_=pD[:, 320:328], func=AF.Abs), reads=[pD], writes=[den])
            P.op("dve", lambda e: e.tensor_tensor(den[:], den[:], lb[:], ALU.max), reads=[den, lb], writes=[den])
            P.op("dve", lambda e: e.reciprocal(ri[:], den[:]), reads=[den], writes=[ri])
            for half in range(2):
                pa = pA[half]
                P.op("dve", lambda e, half=half, pa=pa: e.tensor_tensor(
                    hm[:, half * 4:(half + 1) * 4, :], pa[:].rearrange("p (h d) -> p h d", h=4),
                    ri[:, half * 4:(half + 1) * 4].unsqueeze(2).to_broadcast([128, 4, 128]), ALU.mult),
                    reads=[pa, ri], writes=[hm])
            P.op("dve", lambda e: e.tensor_reduce(mu[:], hm[:], AX.X, ALU.add), reads=[hm], writes=[mu])
            P.op("dve", lambda e: e.tensor_scalar(mu[:], mu[:], 1.0 / 128, None, ALU.mult), reads=[mu], writes=[mu])
            P.op("dve", lambda e: e.tensor_tensor(hc[:], hm[:], mu[:].unsqueeze(2).to_broadcast([128, 8, 128]), ALU.subtract),
                 reads=[hm, mu], writes=[hc])
            P.op("act", lambda e: e.activation(out=hm[:], in_=hc[:], func=AF.Square), reads=[hc], writes=[hm])
            P.op("dve", lambda e: e.tensor_reduce(var[:], hm[:], AX.X, ALU.add), reads=[hm], writes=[var])
            P.op("dve", lambda e: e.tensor_scalar(var[:], var[:], 1.0 / 128, LN_EPS, ALU.mult, ALU.add), reads=[var], writes=[var])
            P.op("act", lambda e: e.activation(out=var[:], in_=var[:], func=AF.Sqrt), reads=[var], writes=[var])
            P.op("dve", lambda e: e.reciprocal(var[:], var[:]), reads=[var], writes=[var])
            P.op("dve", lambda e: e.tensor_tensor(hc[:], hc[:], var[:].unsqueeze(2).to_broadcast([128, 8, 128]), ALU.mult),
                 reads=[hc, var], writes=[hc])
            hcf = hc[:].rearrange("p h d -> p (h d)")
            P.op("dve", lambda e: e.tensor_tensor(hcf, hcf, lnh[:], ALU.mult), reads=[hc, lnh], writes=[hc])
            gated = gated2[t % 2]
            P.op("dve", lambda e: e.tensor_tensor(gated[:], hcf, sg[:], ALU.mult), reads=[hc, sg], writes=[gated])
            pending[0] = post(t, gated)
            flushG()
        adv_all()
        P.dma("sp", st_out["C"][:], Cst[:], Cst, reads=[Cst], writes=[st_out["C"]])
        P.dma("sp", st_out["n"][:], nst[:], nst, reads=[nst], writes=[st_out["n"]])
        with nc.allow_non_contiguous_dma(reason="8-element state vector"):
            P.dma("sp", st_out["m"][:], mst[:], mst, reads=[mst], writes=[st_out["m"]])
        P.end_phase()


def rope_inplace(P, x3, cs, sn, tmp, nh):
    xt, xa = x3
    c = cs[:].unsqueeze(1).to_broadcast([128, nh, 8])
    s = sn[:].unsqueeze(1).to_broadcast([128, nh, 8])
    x1 = xa[:, :, 0:8]
    x2 = xa[:, :, 8:16]
    t1, t2, t3, t4 = (tmp[:, i, :nh, :] for i in range(4))
    P.op("dve", lambda e: e.tensor_tensor(t1, x1, c, ALU.mult), reads=[xt, cs], writes=[tmp])
    P.op("dve", lambda e: e.tensor_tensor(t2, x2, s, ALU.mult), reads=[xt, sn], writes=[tmp])
    P.op("dve", lambda e: e.tensor_tensor(t3, x2, c, ALU.mult), reads=[xt, cs], writes=[tmp])
    P.op("dve", lambda e: e.tensor_tensor(t4, x1, s, ALU.mult), reads=[xt, sn], writes=[tmp])
    P.op("dve", lambda e: e.tensor_tensor(x1, t1, t2, ALU.subtract), reads=[tmp], writes=[xt])
    P.op("dve", lambda e: e.tensor_tensor(x2, t3, t4, ALU.add), reads=[tmp], writes=[xt])


def kv_phase(P, C, hin, ntok, w_kv, cos_d, sin_d, kt_out, v_out):
    NT = ntok // 128
    with ExitStack() as st:
        wkv = P.sb("k_wkv", [128, KC, 512], BF16, st)
        P.dma("pool", wkv[:], w_kv.rearrange("(k p) c -> p k c", p=128), wkv, writes=[wkv])
        hT = [P.sb(f"k_hT{i}", [128, KC, 128], BF16, st) for i in range(2)]
        cs = [P.sb(f"k_cs{i}", [128, 8], F32, st) for i in range(2)]
        sn = [P.sb(f"k_sn{i}", [128, 8], F32, st) for i in range(2)]
        kf = P.sb("k_kf", [128, 4, 64], F32, st)
        kb = P.sb("k_kb", [128, 256], BF16, st)
        vbt = [P.sb(f"k_vb{i}", [128, 256], BF16, st) for i in range(2)]
        ktb = [P.sb(f"k_ktb{i}", [64, 4, 128], BF16, st) for i in range(2)]
        tmp = P.sb("k_tmp", [128, 4, 16, 8], F32, st)
        ps = [P.ps(f"k_ps{i}", [128, 512], F32, st) for i in range(2)]
        pt = [P.ps(f"k_pt{i}", [128, 1024], BF16, st) for i in range(2)]
        for t in range(NT):
            ht, c_, s_ = hT[t % 2], cs[t % 2], sn[t % 2]
            P.dma("pool", ht[:], hin.ap(t * 128, 128), ht, reads=hin.trk(t * 128, 128), writes=[ht])
            P.dma("sp", c_[:], cos_d[t * 128:(t + 1) * 128, :], c_, reads=[cos_d], writes=[c_])
            P.dma("sp", s_[:], sin_d[t * 128:(t + 1) * 128, :], s_, reads=[sin_d], writes=[s_])
            p_ = ps[t % 2]
            for k in range(KC):
                P.op("pe", lambda e, k=k: e.matmul(p_[:], ht[:, k, :], wkv[:, k, :], start=(k == 0), stop=(k == KC - 1)),
                     reads=[ht, wkv], writes=[p_], signal=(k == KC - 1))
            vt = vbt[t % 2]
            P.op("act", lambda e: e.copy(vt[:], p_[:, 256:512]), reads=[p_], writes=[vt])
            P.op("act", lambda e: e.copy(kf[:], p_[:, 0:256].rearrange("p (h d) -> p h d", h=4)), reads=[p_], writes=[kf])
            rope_inplace(P, (kf, kf[:]), c_, s_, tmp, 4)
            P.op("act", lambda e: e.copy(kb[:], kf[:].rearrange("p h d -> p (h d)")), reads=[kf], writes=[kb])
            ptt = pt[t % 2]
            for g in range(4):
                P.op("pe", lambda e, g=g: e.transpose(ptt[:64, g * 128:(g + 1) * 128], kb[:, g * 64:(g + 1) * 64], C.ident_b[:]),
                     reads=[kb, C.ident_b], writes=[ptt], signal=(g == 3))
            kt = ktb[t % 2]
            P.op("dve", lambda e: e.tensor_copy(kt[:], ptt[:64, 0:512].rearrange("p (g t) -> p g t", g=4)), reads=[ptt], writes=[kt])
            P.dma("sp", kt_out.h[:, :, t * 128:(t + 1) * 128], kt[:], kt, reads=[kt], writes=[kt_out])
            P.dma("sp", v_out.h[t * 128:(t + 1) * 128, :], vt[:], vt, reads=[vt], writes=[v_out])
        P.end_phase()


def swa_phase(P, C, hin, hout, nblk, w_q, sinks, w_o, g_ap, b_ap, cos_d, sin_d, kt_d, v_d, halo, mask_d, mask0_d,
              in_tok0=128, out_tok0=128):
    nc = P.nc
    SCALE = 0.125
    with ExitStack() as st:
        wq = P.sb("a_wq", [128, KC, 1024], BF16, st)
        wo = P.sb("a_wo", [128, KC, 1024], BF16, st)
        for (wt, wsrc) in ((wq, w_q), (wo, w_o)):
            wv = wsrc.rearrange("(k p) c -> p k c", p=128)
            for c0 in range(0, 1024, 512):
                P.dma("pool", wt[:, :, c0:c0 + 512], wv[:, :, c0:c0 + 512], wt, writes=[wt])
        lng, lnb = ln_params(P, "a_ln", g_ap, b_ap, st)
        snk = P.sb("a_snk", [128, 16], F32, st)
        with nc.allow_non_contiguous_dma(reason="sink bcast"):
            P.dma("sp", snk[:], sinks.partition_broadcast(128), snk, writes=[snk])
        mask = P.sb("a_mask", [128, 272], F32, st)
        mask0 = P.sb("a_mask0", [128, 272], F32, st)
        P.dma("sp", mask[:], mask_d[:], mask, reads=[mask_d], writes=[mask])
        P.dma("sp", mask0[:], mask0_d[:], mask0, reads=[mask0_d], writes=[mask0])
        ktm = P.sb("a_ktm", [64, 4, 16], BF16, st)
        vm = P.sb("a_vm", [16, 256], BF16, st)
        P.dma("sp", ktm[:], halo["kt_meta"][:], ktm, reads=[halo["kt_meta"]], writes=[ktm])
        P.dma("sp", vm[:], halo["v_meta"][:], vm, reads=[halo["v_meta"]], writes=[vm])
        ktw = [P.sb(f"a_ktw{i}", [64, 4, 128], BF16, st) for i in range(3)]
        vw = [P.sb(f"a_vw{i}", [128, 256], BF16, st) for i in range(3)]
        P.dma("sp", ktw[0][:], halo["kt_prev"][:], ktw[0], reads=[halo["kt_prev"]], writes=[ktw[0]])
        P.dma("sp", vw[0][:], halo["v_prev"][:], vw[0], reads=[halo["v_prev"]], writes=[vw[0]])
        hT = [P.sb(f"a_hT{i}", [128, KC, 128], BF16, st) for i in range(2)]
        cs = [P.sb(f"a_cs{i}", [128, 8], F32, st) for i in range(2)]
        sn = [P.sb(f"a_sn{i}", [128, 8], F32, st) for i in range(2)]
        qf = P.sb("a_qf", [128, 16, 64], F32, st)
        qb = P.sb("a_qb", [128, 1024], BF16, st)
        qT = P.sb("a_qT", [64, 16, 128], BF16, st)
        tmp = P.sb("a_tmp", [128, 4, 16, 8], F32, st)
        smx = [P.sb(f"a_sm{i}", [128, 272], F32, st) for i in range(2)]
        pb_ = [P.sb(f"a_p{i}", [128, 272], BF16, st) for i in range(2)]
        pT = [P.sb(f"a_pT{i}", [128, 3, 128], BF16, st) for i in range(2)]
        sc = {n_: [P.sb(f"a_c_{n_}{i}", [128, 1], F32, st) for i in range(2)] for n_ in ("mx", "ng", "rs", "ex", "ri")}
        otok = P.sb("a_otok", [128, 1024], BF16, st)
        gT = P.sb("a_gT", [128, KC, 128], BF16, st)
        hres = P.sb("a_hres", [128, KC, 128], F32, st)
        ypre = [P.sb(f"a_y{k}", [128, 128], F32, st) for k in range(KC)]
        ostg = [P.sb(f"a_ostg{i}", [128, 128], F32, st) for i in range(3)]
        pq = [P.ps(f"a_pq{i}", [128, 512], F32, st) for i in range(2)]
        pS = [P.ps(f"a_pS{i}", [128, 512], F32, st) for i in range(2)]
        pV = [P.ps(f"a_pV{i}", [128, 512], F32, st) for i in range(2)]
        ptA = P.ps("a_ptA", [128, 1024], BF16, st)
        ptB = P.ps("a_ptB", [128, 1024], BF16, st)
        L = LNBufs.__new__(LNBufs)
        L.W = 128
        L.sq = [P.sb(f"aln_sq{i}", [128, 128], F32, st) for i in range(2)]
        L.ps_s, L.ps_q = pS[0], pS[1]
        L.mean = P.sb("aln_mean", [128, 128], F32, st)
        L.rstd = P.sb("aln_rstd", [128, 128], F32, st)
        L.tmp = [P.sb(f"aln_tmp{i}", [128, 128], F32, st) for i in range(2)]
        ocnt = [0]
        qf2 = [qf, P.sb("a_qf1", [128, 16, 64], F32, st)]
        qb2 = [qb, P.sb("a_qb1", [128, 1024], BF16, st)]
        qT2 = [qT, P.sb("a_qT1", [64, 16, 128], BF16, st)]
        smx.append(P.sb("a_sm2", [128, 272], F32, st))
        pb_.append(P.sb("a_p2", [128, 272], BF16, st))
        pT.append(P.sb("a_pT2", [128, 3, 128], BF16, st))
        for n_ in sc:
            for i_ in range(2, 8):
                sc[n_].append(P.sb(f"a_c_{n_}{i_}", [128, 1], F32, st))
        otok2 = [otok, P.sb("a_otok1", [128, 1024], BF16, st)]
        gT2 = [gT, P.sb("a_gT1", [128, KC, 128], BF16, st)]
        hres2 = [hres, P.sb("a_hres1", [128, KC, 128], F32, st)]
        ypre2 = [ypre, [P.sb(f"a_y1{k}", [128, 128], F32, st) for k in range(KC)]]
        L.ps_s, L.ps_q = pq[0], pq[1]

        def pre(n):
            t_in = in_tok0 + n * 128
            ht, c_, s_ = hT[n % 2], cs[n % 2], sn[n % 2]
            qf_, qb_, qT_ = qf2[n % 2], qb2[n % 2], qT2[n % 2]
            P.dma("pool", ht[:], hin.ap(t_in, 128), ht, reads=hin.trk(t_in, 128), writes=[ht])
            P.dma("sp", c_[:], cos_d[t_in:t_in + 128, :], c_, reads=[cos_d], writes=[c_])
            P.dma("sp", s_[:], sin_d[t_in:t_in + 128, :], s_, reads=[sin_d], writes=[s_])
            kown, vown = ktw[(n + 1) % 3], vw[(n + 1) % 3]
            P.dma("sp", kown[:], kt_d.h[:, :, t_in:t_in + 128], kown, reads=[kt_d], writes=[kown])
            P.dma("sp", vown[:], v_d.h[t_in:t_in + 128, :], vown, reads=[v_d], writes=[vown])
            for half in range(2):
                for k in range(KC):
                    P.op("pe", lambda e, k=k, half=half: e.matmul(pq[half][:], ht[:, k, :], wq[:, k, half * 512:(half + 1) * 512],
                                                                 start=(k == 0), stop=(k == KC - 1)),
                         reads=[ht, wq], writes=[pq[half]], signal=(k == KC - 1))
                P.op("act", lambda e, half=half: e.copy(qf_[:, half * 8:(half + 1) * 8, :], pq[half][:].rearrange("p (h d) -> p h d", h=8)),
                     reads=[pq[half]], writes=[qf_])
            rope_inplace(P, (qf_, qf_[:]), c_, s_, tmp, 16)
            P.op("act", lambda e: e.copy(qb_[:], qf_[:].rearrange("p h d -> p (h d)")), reads=[qf_], writes=[qb_])
            for half, ptx in enumerate((ptA, ptB)):
                for hh in range(8):
                    h = half * 8 + hh
                    P.op("pe", lambda e, h=h, hh=hh, ptx=ptx: e.transpose(ptx[:64, hh * 128:(hh + 1) * 128], qb_[:, h * 64:(h + 1) * 64], C.ident_b[:]),
                         reads=[qb_, C.ident_b], writes=[ptx], signal=(hh == 7))
                P.op("dve", lambda e, half=half, ptx=ptx: e.tensor_copy(qT_[:, half * 8:(half + 1) * 8, :], ptx[:64, :].rearrange("p (h t) -> p h t", h=8)),
                     reads=[ptx], writes=[qT_])

        def stage_fns(n):
            HB = n * 16
            kprev, vprev = ktw[n % 3], vw[n % 3]
            kown, vown = ktw[(n + 1) % 3], vw[(n + 1) % 3]
            qT_ = qT2[n % 2]
            otok_ = otok2[n % 2]
            mk = mask0 if n == 0 else mask

            def s1(h):
                g = h // 4
                ps_ = pS[h % 2]
                P.op("pe", lambda e: e.matmul(ps_[:, 0:128], qT_[:, h, :], kprev[:, g, :], start=True, stop=True),
                     reads=[qT_, kprev], writes=[ps_], signal=False)
                P.op("pe", lambda e: e.matmul(ps_[:, 128:256], qT_[:, h, :], kown[:, g, :], start=True, stop=True),
                     reads=[qT_, kown], writes=[ps_], signal=False)
                P.op("pe", lambda e: e.matmul(ps_[:, 256:272], qT_[:, h, :], ktm[:, g, :], start=True, stop=True),
                     reads=[qT_, ktm], writes=[ps_], signal=True)

            def s2a(h):
                i3 = (HB + h) % 3
                ps_, sm_ = pS[h % 2], smx[i3]
                mx, ng = sc["mx"][(HB + h) % 8], sc["ng"][(HB + h) % 8]
                P.op("dve", lambda e: e.scalar_tensor_tensor(sm_[:], ps_[:, 0:272], SCALE, mk[:], ALU.mult, ALU.add),
                     reads=[ps_, mk], writes=[sm_])
                P.op("dve", lambda e: e.reduce_max(mx[:], sm_[:], AX.X), reads=[sm_], writes=[mx])
                P.op("dve", lambda e: e.tensor_scalar(ng[:], mx[:], snk[:, h:h + 1], -1.0, ALU.max, ALU.mult),
                     reads=[mx, snk], writes=[ng])

            def s2b(h):
                i3 = (HB + h) % 3
                sm_, p_ = smx[i3], pb_[i3]
                ng, rs_, ex = (sc[n_][(HB + h) % 8] for n_ in ("ng", "rs", "ex"))
                P.op("act", lambda e: e.activation(out=p_[:], in_=sm_[:], func=AF.Exp, bias=ng[:, 0:1], accum_out=rs_[:]),
                     reads=[sm_, ng], writes=[p_, rs_])
                P.op("act", lambda e: e.activation(out=ex[:], in_=snk[:, h:h + 1], func=AF.Exp, bias=ng[:, 0:1]),
                     reads=[snk, ng], writes=[ex])

            def s2c(h):
                i3 = (HB + h) % 3
                rs_, ex, ri = (sc[n_][(HB + h) % 8] for n_ in ("rs", "ex", "ri"))
                P.op("dve", lambda e: e.tensor_tensor(ri[:], rs_[:], ex[:], ALU.add), reads=[rs_, ex], writes=[ri])
                P.op("dve", lambda e: e.reciprocal(ri[:], ri[:]), reads=[ri], writes=[ri])

            def s3a(h):
                p_ = pb_[(HB + h) % 3]
                ptx = ptA if h % 2 == 0 else ptB
                for j in range(2):
                    P.op("pe", lambda e, j=j: e.transpose(ptx[:, j * 128:(j + 1) * 128], p_[:, j * 128:(j + 1) * 128], C.ident_b[:]),
                         reads=[p_, C.ident_b], writes=[ptx], signal=False)
                P.op("pe", lambda e: e.transpose(ptx[:16, 256:384], p_[:, 256:272], C.ident_b[:]),
                     reads=[p_, C.ident_b], writes=[ptx], signal=True)

                pT_ = pT[(HB + h) % 3]
                P.op("act", lambda e: e.copy(pT_[:, 0:2, :], ptx[:, 0:256].rearrange("p (j t) -> p j t", j=2)),
                     reads=[ptx], writes=[pT_])
                P.op("act", lambda e: e.copy(pT_[:16, 2, :], ptx[:16, 256:384]), reads=[ptx], writes=[pT_])

            def s4a(h):
                g = h // 4
                pT_ = pT[(HB + h) % 3]
                pv = pV[h % 2]
                oc = (h // 2) * 64
                P.op("pe", lambda e: e.matmul(pv[:, oc:oc + 64], pT_[:, 0, :], vprev[:, g * 64:(g + 1) * 64], start=True, stop=False),
                     reads=[pT_, vprev], writes=[pv], signal=False)
                P.op("pe", lambda e: e.matmul(pv[:, oc:oc + 64], pT_[:, 1, :], vown[:, g * 64:(g + 1) * 64], start=False, stop=False),
                     reads=[pT_, vown], writes=[pv], signal=False)
                P.op("pe", lambda e: e.matmul(pv[:, oc:oc + 64], pT_[:16, 2, :], vm[:, g * 64:(g + 1) * 64], start=False, stop=True),
                     reads=[pT_, vm], writes=[pv], signal=True)

            def s4b(h):
                ri = sc["ri"][(HB + h) % 8]
                pv = pV[h % 2]
                oc = (h // 2) * 64
                P.op("dve", lambda e: e.tensor_scalar(otok_[:, h * 64:(h + 1) * 64], pv[:, oc:oc + 64], ri[:, 0:1], None, ALU.mult),
                     reads=[pv, ri], writes=[otok_])

            return (s1, s2a, s2b, s2c, s3a, s4a, s4b)

        def post(n):
            t_in = in_tok0 + n * 128
            otok_, gT_, hres_, ypre_ = otok2[n % 2], gT2[n % 2], hres2[n % 2], ypre2[n % 2]
            for k in range(KC):
                P.op("pe", lambda e, k=k: e.transpose(ptA[:, k * 128:(k + 1) * 128], otok_[:, k * 128:(k + 1) * 128], C.ident_b[:]),
                     reads=[otok_, C.ident_b], writes=[ptA], signal=(k == KC - 1))
            P.op("act", lambda e: e.copy(gT_[:], ptA[:].rearrange("p (k t) -> p k t", k=KC)), reads=[ptA], writes=[gT_])
            P.dma("sp", hres_[:], hin.ap(t_in, 128), hres_, reads=hin.trk(t_in, 128), writes=[hres_])
            yield
            for dk in range(KC):
                pm = pq[dk // 4]
                for k in range(KC):
                    P.op("pe", lambda e, k=k, dk=dk, pm=pm: e.matmul(pm[:, (dk % 4) * 128:(dk % 4 + 1) * 128], wo[:, k, dk * 128:(dk + 1) * 128],
                                                                    gT_[:, k, :], start=(k == 0), stop=(k == KC - 1)),
                         reads=[wo, gT_], writes=[pm], signal=(k == KC - 1))
                P.op("dve", lambda e, dk=dk, pm=pm: e.scalar_tensor_tensor(ypre_[dk][:], hres_[:, dk, :], ALPHA,
                                                                          pm[:, (dk % 4) * 128:(dk % 4 + 1) * 128], ALU.mult, ALU.add),
                     reads=[hres_, pm], writes=[ypre_[dk]])
                if dk == 3:
                    yield

            def yk(k):
                return ypre_[k], ypre_[k][:]

            def out_fn(k):
                o = ostg[ocnt[0] % 3]
                ocnt[0] += 1
                return o, o[:]

            def after(k, o, n=n):
                d0 = out_tok0 + n * 128
                P.dma("sp", hout.h[k, :, d0:d0 + 128], o[:], o, reads=[o], writes=hout.trk(d0, 128))
            yield
            layernorm_fm(P, C, L, yk, 128, lng, lnb, out_fn, after)

        NST = 7
        fns = {}
        pre(0)
        posts = []
        total = nblk * 16
        for step in range(total + NST - 1):
            for j in range(NST):
                H = step - j
                if 0 <= H < total:
                    n, h = divmod(H, 16)
                    if n not in fns:
                        fns[n] = stage_fns(n)
                    fns[n][j](h)
                    if j == NST - 1 and h == 15:
                        posts.append(post(n))
                        fns.pop(n - 1, None)
            n_s1, h_s1 = divmod(min(step, total - 1), 16)
            if step < total and h_s1 == 6 and n_s1 + 1 < nblk:
                pre(n_s1 + 1)
            if posts:
                try:
                    next(posts[0])
                except StopIteration:
                    posts.pop(0)
        for g_ in posts:
            for _ in g_:
                pass
        P.end_phase()


NBLK = 16
BLK = 1024
NSLOT = NBLK * BLK
BIGI = 1 << 28


class MoeScratch:
    def __init__(self, P, ntok):
        self.ntok = ntok
        self.htok = P.dram("ms_htok", [ntok, 1024], F32)
        self.xs = P.dram("ms_xs", [NSLOT, 1024], BF16)
        self.aux = P.dram("ms_aux", [NSLOT, 16], F32)
        self.y = [P.dram(f"ms_y{r}", [ntok, 1024], F32) for r in range(2)]
        self.inited = False


def moe_sparse_phase(P, C, S, hin, hout, tok0, ntok, F, g_ap, b_ap, moe, out_tok0=None):
    nc = P.nc
    g = nc.gpsimd
    if out_tok0 is None:
        out_tok0 = tok0
    NT = ntok // 128
    NF = F // 128
    with ExitStack() as st:
        pb = [P.ps(f"x_pb{i}", [128, 512], F32, st) for i in range(7)]
        ptb = P.ps("x_ptb", [128, 1024], BF16, st)
        lng, lnb = ln_params(P, "x_ln", g_ap, b_ap, st)
        wr = P.sb("x_wr", [128, KC, 8], F32, st)
        with nc.allow_non_contiguous_dma(reason="small router weight"):
            P.dma("sp", wr[:], moe["wr"].rearrange("(k p) e -> p k e", p=128), wr, writes=[wr])
        brb = P.sb("x_brb", [128, 8], F32, st)
        with nc.allow_non_contiguous_dma(reason="bias bcast"):
            P.dma("sp", brb[:], moe["br"].partition_broadcast(128), brb, writes=[brb])
        tris = P.sb("x_tris", [128, 128], F32, st)
        P.dma("sp", tris[:], C.cin["tris"][:], tris, reads=[C.cin["tris"]], writes=[tris])
        iot = P.sb("x_iota", [128, 1], F32, st)
        P.dma("sp", iot[:], C.cin["iota"][:], iot, reads=[C.cin["iota"]], writes=[iot])
        M1 = P.sb("x_M1", [128, NT, 8], F32, st)
        M2 = P.sb("x_M2", [128, NT, 8], F32, st)
        POS = P.sb("x_POS", [128, NT, 8], F32, st)
        GG = P.sb("x_GG", [128, NT, 2], F32, st)
        CNT = P.sb("x_CNT", [128, 8], F32, st)
        P.op("dve", lambda e: e.memset(CNT[:], 0.0), writes=[CNT])
        auxinit = P.sb("x_auxinit", [128, 16, 16], F32, st)
        auxinit_i = auxinit[:].bitcast(I32)
        P.op("dve", lambda e: e.memset(auxinit[:], 0.0), writes=[auxinit])
        P.op("dve", lambda e: e.memset(auxinit_i[:, :, 1:3], BIGI), reads=[auxinit], writes=[auxinit])
        aux_v = S.aux.h[:, :].rearrange("(a p s) c -> a p s c", p=128, s=16)
        for a in range(NSLOT // (128 * 16)):
            P.dma("sp", aux_v[a], auxinit[:], auxinit, reads=[auxinit], writes=[S.aux])
        if not S.inited:
            zrow = P.sb("x_zrow", [128, 4, 1024], BF16, st)
            P.op("dve", lambda e: e.memset(zrow[:], 0.0), writes=[zrow])
            xs_v = S.xs.h[:, :].rearrange("(a p s) c -> a p s c", p=128, s=4)
            for a in range(NSLOT // 512):
                P.dma("sp", xs_v[a], zrow[:], zrow, reads=[zrow], writes=[S.xs])
            S.inited = True
        hT = P.sb("x_hT", [128, KC, BLK], BF16, st)
        xrow = P.sb("x_xrow", [128, 8, 1024], BF16, st)
        auxb = P.sb("x_auxb", [128, 8, 16], F32, st)
        act = [P.sb(f"x_act{c}", [128, BLK], BF16, st) for c in range(FB)]
        ytok = [P.sb(f"x_ytok{s}", [128, 1024], F32, st) for s in range(8)]
        wgs = [P.sb(f"x_wg{i}", [128, KC, 512], BF16, st) for i in range(3)]
        wds = [P.sb(f"x_wd{i}", [128, FB, 1024], BF16, st) for i in range(2)]
        stmp = [P.sb(f"x_st{i}", [128, 512], F32, st) for i in range(2)]
        class KV_:
            def __init__(self, t):
                self.t = t
        hfm = [ytok[0], ytok[1]]
        htk = [ytok[2], ytok[3]]
        rt = {n_: P.sb("x_rt_" + n_, [128, 8], F32, st) for n_ in ("lg", "v", "mk")}
        c1 = {n_: P.sb("x_c_" + n_, [128, 1], F32, st) for n_ in ("d", "ex", "g1")}
        for t in range(NT):
            hf, hk = hfm[t % 2], htk[t % 2]
            hfv = hf[:].rearrange("p (k t) -> p k t", k=KC)
            P.dma("sp", hfv, hin.ap(tok0 + t * 128, 128), hf, reads=hin.trk(tok0 + t * 128, 128), writes=[hf])
            ps = pb[0]
            for k in range(KC):
                P.op("pe", lambda e, k=k: e.matmul(ps[:, :8], hfv[:, k, :], wr[:, k, :], start=(k == 0), stop=(k == KC - 1)),
                     reads=[hf, wr], writes=[ps], signal=(k == KC - 1))
            lg, v, mk = rt["lg"], rt["v"], rt["mk"]
            P.op("dve", lambda e: e.tensor_tensor(lg[:], ps[:, :8], brb[:], ALU.add), reads=[ps, brb], writes=[lg])
            P.op("dve", lambda e: e.max(v[:], lg[:]), reads=[lg], writes=[v])
            P.op("dve", lambda e, t=t: e.tensor_scalar(M1[:, t, :], lg[:], v[:, 0:1], None, ALU.is_equal), reads=[lg, v], writes=[M1])
            P.op("dve", lambda e, t=t: e.tensor_scalar(M2[:, t, :], lg[:], v[:, 1:2], None, ALU.is_equal), reads=[lg, v], writes=[M2])
            P.op("dve", lambda e, t=t: e.tensor_tensor(mk[:], M1[:, t, :], M2[:, t, :], ALU.add), reads=[M1, M2], writes=[mk])
            d_, ex, g1 = c1["d"], c1["ex"], c1["g1"]
            P.op("dve", lambda e: e.tensor_tensor(d_[:], v[:, 1:2], v[:, 0:1], ALU.subtract), reads=[v], writes=[d_])
            P.op("act", lambda e: e.activation(out=ex[:], in_=d_[:], func=AF.Exp), reads=[d_], writes=[ex])
            P.op("dve", lambda e: e.tensor_scalar(g1[:], ex[:], 1.0, None, ALU.add), reads=[ex], writes=[g1])
            P.op("dve", lambda e, t=t: e.reciprocal(GG[:, t, 0:1], g1[:]), reads=[g1], writes=[GG])
            P.op("dve", lambda e, t=t: e.tensor_tensor(GG[:, t, 1:2], GG[:, t, 0:1], ex[:], ALU.mult), reads=[GG, ex], writes=[GG])
            P.op("pe", lambda e: e.matmul(ps[:, 8:16], tris[:], mk[:], start=True, stop=True), reads=[tris, mk], writes=[ps])
            P.op("pe", lambda e: e.matmul(ps[:, 16:24], C.ones_f[:], mk[:], start=True, stop=True), reads=[C.ones_f, mk], writes=[ps])
            P.op("dve", lambda e, t=t: e.tensor_tensor(POS[:, t, :], ps[:, 8:16], CNT[:], ALU.add), reads=[ps, CNT], writes=[POS])
            P.op("dve", lambda e: e.tensor_tensor(CNT[:], CNT[:], ps[:, 16:24], ALU.add), reads=[ps, CNT], writes=[CNT])
            for half in range(2):
                pt = pb[1 + half]
                for kk in range(4):
                    k = half * 4 + kk
                    P.op("pe", lambda e, k=k, kk=kk, pt=pt: e.matmul(pt[:, kk * 128:(kk + 1) * 128], hfv[:, k, :], C.ident_f[:], start=True, stop=True),
                         reads=[hf, C.ident_f], writes=[pt], signal=(kk == 3))
                P.op("act", lambda e, half=half, pt=pt: e.copy(hk[:, half * 512:(half + 1) * 512], pt[:]), reads=[pt], writes=[hk])
            P.dma("sp", S.htok.h[tok0 + t * 128:tok0 + (t + 1) * 128, :], hk[:], hk, reads=[hk], writes=[S.htok])
        NB = P.sb("x_NB", [128, 8], F32, st)
        tmp8 = P.sb("x_tmp8", [128, 8], F32, st)
        P.op("dve", lambda e: e.tensor_scalar(NB[:], CNT[:], 0.0, None, ALU.is_gt), reads=[CNT], writes=[NB])
        for kq in range(1, 5):
            P.op("dve", lambda e, kq=kq: e.tensor_scalar(tmp8[:], CNT[:], float(kq * BLK), None, ALU.is_gt), reads=[CNT], writes=[tmp8])
            P.op("dve", lambda e: e.tensor_tensor(NB[:], NB[:], tmp8[:], ALU.add), reads=[NB, tmp8], writes=[NB])
        PST = P.sb("x_PST", [128, 8], F32, st)
        PEND = P.sb("x_PEND", [128, 8], F32, st)
        P.op("dve", lambda e: e.memset(PST[:], 0.0), writes=[PST])
        for e_ in range(8):
            P.op("dve", lambda e, e_=e_: e.scalar_tensor_tensor(PEND[:, e_:e_ + 1], NB[:, e_:e_ + 1], float(BLK), PST[:, e_:e_ + 1], ALU.mult, ALU.add),
                 reads=[NB, PST], writes=[PEND])
            if e_ < 7:
                P.op("dve", lambda e, e_=e_: e.tensor_copy(PST[:, e_ + 1:e_ + 2], PEND[:, e_:e_ + 1]), reads=[PEND], writes=[PST])
        BE = P.sb("x_BE", [128, NBLK], F32, st)
        BEI = P.sb("x_BEI", [128, NBLK], I32, st)
        for b in range(NBLK):
            P.op("dve", lambda e, b=b: e.tensor_scalar(tmp8[:], PEND[:], float(b * BLK), None, ALU.is_le), reads=[PEND], writes=[tmp8])
            P.op("dve", lambda e, b=b: e.reduce_sum(BE[:, b:b + 1], tmp8[:], AX.X), reads=[tmp8], writes=[BE])
        P.op("dve", lambda e: e.tensor_scalar(BE[:], BE[:], 7.0, None, ALU.min), reads=[BE], writes=[BE])
        P.op("dve", lambda e: e.tensor_copy(BEI[:], BE[:]), reads=[BE], writes=[BEI])
        xb = [act[0], act[1]]
        dd = [P.sb(f"x_dd{i}", [128, 2], F32, st) for i in range(2)]
        ddi = [P.sb(f"x_ddi{i}", [128, 2], I32, st) for i in range(2)]
        axr = [P.sb(f"x_axr{i}", [128, 2, 16], F32, st) for i in range(2)]
        tidf = P.sb("x_tidf", [128, 1], F32, st)
        tidi = P.sb("x_tidi", [128, 1], I32, st)

        def ind_scatter(dst_h, idx_ap, src_ap, src_t, idx_t, dst_t, bound):
            for (h, v) in P._waits("pool", [src_t, idx_t], [dst_t]):
                g.wait_ge(h, v)
            if src_t.dsem is None or src_t.dcnt + 16 > SEM_LIMIT:
                P.dma_sem(src_t)
            src_t.dcnt += 16
            g.indirect_dma_start(out=dst_h, out_offset=bass.IndirectOffsetOnAxis(ap=idx_ap, axis=0), in_=src_ap, in_offset=None,
                                 bounds_check=bound, oob_is_err=False).then_inc(src_t.dsem, 16)
            tok = (src_t.dkey, src_t.dsem, src_t.dcnt)
            P._commit(tok, [src_t, idx_t], [dst_t])

        for t in range(NT):
            x_, d2, d2i, ax = xb[t % 2], dd[t % 2], ddi[t % 2], axr[t % 2]
            P.dma("pool", x_[:], S.htok.h[tok0 + t * 128:tok0 + (t + 1) * 128, :], x_, reads=[S.htok], writes=[x_])
            P.op("dve", lambda e, t=t: e.tensor_tensor(tmp8[:], POS[:, t, :], PST[:], ALU.add), reads=[POS, PST], writes=[tmp8])
            for r, Mr in enumerate((M1, M2)):
                P.op("dve", lambda e, t=t, Mr=Mr: e.tensor_tensor(rt["lg"][:], tmp8[:], Mr[:, t, :], ALU.mult), reads=[tmp8, Mr], writes=[rt["lg"]])
                P.op("dve", lambda e, r=r: e.reduce_sum(d2[:, r:r + 1], rt["lg"][:], AX.X), reads=[rt["lg"]], writes=[d2])
            P.op("dve", lambda e: e.tensor_copy(d2i[:], d2[:]), reads=[d2], writes=[d2i])
            P.op("dve", lambda e, t=t: e.tensor_scalar(tidf[:], iot[:], float(tok0 + t * 128), None, ALU.add), reads=[iot], writes=[tidf])
            P.op("dve", lambda e: e.tensor_copy(tidi[:], tidf[:]), reads=[tidf], writes=[tidi])
            axi = ax[:].bitcast(I32)
            P.op("dve", lambda e: e.memset(ax[:], 0.0), writes=[ax])
            P.op("dve", lambda e: e.memset(axi[:, :, 1:3], BIGI), reads=[ax], writes=[ax])
            for r in range(2):
                P.op("dve", lambda e, r=r, t=t: e.tensor_copy(ax[:, r, 0:1], GG[:, t, r:r + 1]), reads=[GG, ax], writes=[ax])
                P.op("dve", lambda e, r=r: e.tensor_copy(axi[:, r, 1 + r:2 + r], tidi[:]), reads=[tidi, ax], writes=[ax])
            for r in range(2):
                ind_scatter(S.xs.h[:, :], d2i[:, r:r + 1], x_[:, :], x_, d2i, S.xs, NSLOT - 1)
                ind_scatter(S.aux.h[:, :], d2i[:, r:r + 1], ax[:, r, :], ax, d2i, S.aux, NSLOT - 1)
        psg = (pb[0], pb[1])
        psu = (pb[2], pb[3])
        psd = (pb[4], pb[5])
        cnt = {"wg": 0, "wd": 0, "pg": 0, "pd": 0, "st": 0}
        wgu0 = moe["wgu"][0].rearrange("(k p) c -> p k c", p=128)
        wd0 = moe["wd"][0].rearrange("(c p) d -> p c d", p=128)
        ESTR_GU = 1024 * 2 * F
        ESTR_D = F * 1024

        def dyn_rows(out2d, tensor, pattern, ebreg, ebase, mult, add, big, col, tile):
            P.nkey += 1
            with g.register(f"x_eb{P.nkey}") as eb, g.register(f"x_ad{P.nkey}") as ad:
                g.reg_load(eb, ebreg)
                g.reg_add(ad, eb, ebase)
                g.reg_mul(ad, ad, mult)
                g.reg_add(ad, ad, add)
                g.reg_mul(ad, ad, big)
                g.reg_add(ad, ad, col)
                v = g.snap(ad, donate=True, min_val=0, max_val=(1 << 28))
                P.dma("pool", out2d, bass.AP(tensor, v, pattern), tile, writes=[tile])

        def tens_off(x):
            return (x.tensor, x.offset) if hasattr(x, "tensor") else (x, 0)
        wgu_t, off_gu = tens_off(moe["wgu"])
        wd_t, off_d = tens_off(moe["wd"])
        eb_gu = off_gu // ESTR_GU
        eb_d = off_d // ESTR_D
        assert off_gu % ESTR_GU == 0 and off_d % ESTR_D == 0

        for b in range(NBLK):
            for (h, v) in P._waits("pool", [BEI], []):
                g.wait_ge(h, v)
            ereg = BEI[0:1, b:b + 1]
            P.dma("sp", xrow[:], S.xs.h[b * BLK:(b + 1) * BLK, :].rearrange("(s p) c -> p s c", p=128), xrow, reads=[S.xs], writes=[xrow])
            P.dma("sp", auxb[:], S.aux.h[b * BLK:(b + 1) * BLK, :].rearrange("(s p) c -> p s c", p=128), auxb, reads=[S.aux], writes=[auxb])
            for k in range(KC):
                for s_ in range(8):
                    P.op("pe", lambda e, k=k, s_=s_: e.transpose(ptb[:, s_ * 128:(s_ + 1) * 128], xrow[:, s_, k * 128:(k + 1) * 128], C.ident_b[:]),
                         reads=[xrow, C.ident_b], writes=[ptb], signal=(s_ == 7))
                eng = "act" if k % 2 == 0 else "dve"
                if eng == "act":
                    P.op("act", lambda e, k=k: e.copy(hT[:, k, :], ptb[:]), reads=[ptb], writes=[hT])
                else:
                    P.op("dve", lambda e, k=k: e.tensor_copy(hT[:, k, :], ptb[:]), reads=[ptb], writes=[hT])
            for fb0 in range(0, NF, FB):
                nfb = min(FB, NF - fb0)
                for s0 in range(0, nfb, 2):
                    ns = min(2, nfb - s0)
                    ws = wgs[cnt["wg"] % 3]
                    cnt["wg"] += 1
                    cg = (fb0 + s0) * 128
                    for k in range(KC):
                        dyn_rows(ws[:, k, 0:ns * 128], wgu_t, [[2 * F, 128], [1, ns * 128]], ereg, eb_gu, KC, k, 128 * 2 * F, cg, ws)
                        dyn_rows(ws[:, k, 256:256 + ns * 128], wgu_t, [[2 * F, 128], [1, ns * 128]], ereg, eb_gu, KC, k, 128 * 2 * F, F + cg, ws)
                    for ci in range(ns):
                        a = act[s0 + ci]
                        for c0 in (0, 512):
                            pg = psg[cnt["pg"] % 2]
                            pu = psu[cnt["pg"] % 2]
                            cnt["pg"] += 1
                            for k in range(KC):
                                P.op("pe", lambda e, k=k, pg=pg, c0=c0, ci=ci, ws=ws: e.matmul(
                                    pg[:], ws[:, k, ci * 128:(ci + 1) * 128], hT[:, k, c0:c0 + 512], start=(k == 0), stop=(k == KC - 1)),
                                    reads=[ws, hT], writes=[pg], signal=(k == KC - 1))
                            for k in range(KC):
                                P.op("pe", lambda e, k=k, pu=pu, c0=c0, ci=ci, ws=ws: e.matmul(
                                    pu[:], ws[:, k, 256 + ci * 128:256 + (ci + 1) * 128], hT[:, k, c0:c0 + 512], start=(k == 0), stop=(k == KC - 1)),
                                    reads=[ws, hT], writes=[pu], signal=(k == KC - 1))
                            sm = stmp[cnt["st"] % 2]
                            cnt["st"] += 1
                            P.op("act", lambda e, pg=pg, sm=sm: e.activation(out=sm[:], in_=pg[:], func=AF.Silu), reads=[pg], writes=[sm])
                            P.op("dve", lambda e, pu=pu, sm=sm, a=a, c0=c0: e.tensor_tensor(a[:, c0:c0 + 512], sm[:], pu[:], ALU.mult),
                                 reads=[sm, pu], writes=[a])
                wdt = wds[cnt["wd"] % 2]
                cnt["wd"] += 1
                for dh in range(2):
                    for c in range(nfb):
                        dyn_rows(wdt[:, c, dh * 512:(dh + 1) * 512], wd_t, [[1024, 128], [1, 512]], ereg, eb_d, NF, fb0 + c, 128 * 1024, dh * 512, wdt)
                for s_ in range(8):
                    for dh in range(2):
                        pd = psd[cnt["pd"] % 2]
                        cnt["pd"] += 1
                        for c in range(nfb):
                            P.op("pe", lambda e, c=c, pd=pd, s_=s_, dh=dh, wdt=wdt: e.matmul(
                                pd[:], act[c][:, s_ * 128:(s_ + 1) * 128], wdt[:, c, dh * 512:(dh + 1) * 512], start=(c == 0), stop=(c == nfb - 1)),
                                reads=[wdt, act[c]], writes=[pd], signal=(c == nfb - 1))
                        y = ytok[s_]
                        if fb0 == 0:
                            P.op("dve", lambda e, pd=pd, y=y, s_=s_, dh=dh: e.tensor_scalar(y[:, dh * 512:(dh + 1) * 512], pd[:], auxb[:, s_, 0:1], None, ALU.mult),
                                 reads=[pd, auxb], writes=[y])
                        else:
                            P.op("dve", lambda e, pd=pd, y=y, s_=s_, dh=dh: e.scalar_tensor_tensor(
                                y[:, dh * 512:(dh + 1) * 512], pd[:], auxb[:, s_, 0:1], y[:, dh * 512:(dh + 1) * 512], ALU.mult, ALU.add),
                                reads=[pd, auxb, y], writes=[y])
            auxbi = auxb[:].bitcast(I32)
            for s_ in range(8):
                for r in range(2):
                    ind_scatter(S.y[r].h[:, :], auxbi[:, s_, 1 + r:2 + r], ytok[s_][:, :], ytok[s_], auxb, S.y[r], S.ntok - 1)
        ya = [ytok[0], ytok[1]]
        yb = [ytok[2], ytok[3]]
        hh = [ytok[4], ytok[5]]
        sq = ytok[6]
        e1 = {n_: P.sb("x_e_" + n_, [128, 1], F32, st) for n_ in ("s", "q", "mu", "m2", "rs")}
        ostg = [ytok[7], P.sb("x_ostg1", [128, 1024], F32, st)]
        for t in range(NT):
            a_, b_, h_ = ya[t % 2], yb[t % 2], hh[t % 2]
            r0 = tok0 + t * 128
            P.dma("sp", a_[:], S.y[0].h[r0:r0 + 128, :], a_, reads=[S.y[0]], writes=[a_])
            P.dma("sp", b_[:], S.y[1].h[r0:r0 + 128, :], b_, reads=[S.y[1]], writes=[b_])
            P.dma("sp", h_[:], S.htok.h[r0:r0 + 128, :], h_, reads=[S.htok], writes=[h_])
            P.op("dve", lambda e: e.tensor_tensor(a_[:], a_[:], b_[:], ALU.add), reads=[a_, b_], writes=[a_])
            P.op("dve", lambda e: e.scalar_tensor_tensor(a_[:], h_[:], ALPHA, a_[:], ALU.mult, ALU.add), reads=[h_, a_], writes=[a_])
            s_, q_, mu, m2, rs = (e1[n_] for n_ in ("s", "q", "mu", "m2", "rs"))
            P.op("dve", lambda e: e.reduce_sum(s_[:], a_[:], AX.X), reads=[a_], writes=[s_])
            P.op("act", lambda e: e.activation(out=sq[:], in_=a_[:], func=AF.Square, accum_out=q_[:]), reads=[a_], writes=[sq, q_])
            P.op("dve", lambda e: e.tensor_scalar(mu[:], s_[:], 1.0 / D, None, ALU.mult), reads=[s_], writes=[mu])
            P.op("dve", lambda e: e.tensor_tensor(m2[:], mu[:], mu[:], ALU.mult), reads=[mu], writes=[m2])
            P.op("dve", lambda e: e.scalar_tensor_tensor(rs[:], q_[:], 1.0 / D, m2[:], ALU.mult, ALU.subtract), reads=[q_, m2], writes=[rs])
            P.op("act", lambda e: e.activation(out=rs[:], in_=rs[:], func=AF.Sqrt, bias=C.eps_col[:, 0:1]), reads=[rs, C.eps_col], writes=[rs])
            P.op("dve", lambda e: e.reciprocal(rs[:], rs[:]), reads=[rs], writes=[rs])
            P.op("dve", lambda e: e.tensor_scalar(b_[:], a_[:], mu[:, 0:1], rs[:, 0:1], ALU.subtract, ALU.mult), reads=[a_, mu, rs], writes=[b_])
            o = ostg[t % 2]
            ov = o[:].rearrange("p (k t) -> p k t", k=KC)
            for half in range(2):
                pt = pb[half]
                for kk in range(4):
                    k = half * 4 + kk
                    P.op("pe", lambda e, k=k, kk=kk, pt=pt: e.matmul(pt[:, kk * 128:(kk + 1) * 128], b_[:, k * 128:(k + 1) * 128], C.ident_f[:], start=True, stop=True),
                         reads=[b_, C.ident_f], writes=[pt], signal=(kk == 3))
                for kk in range(4):
                    k = half * 4 + kk
                    P.op("act", lambda e, k=k, kk=kk, pt=pt: e.activation(out=ov[:, k, :], in_=pt[:, kk * 128:(kk + 1) * 128], func=AF.Identity,
                                                                         bias=lnb[:, k:k + 1], scale=lng[:, k:k + 1]),
                         reads=[pt, lng, lnb], writes=[o])
            d0 = out_tok0 + t * 128
            P.dma("sp", hout.ap(d0, 128), ov, o, reads=[o], writes=hout.trk(d0, 128))
        P.end_phase()


import ml_dtypes

N_CORES = 8
SEQ = 8192
HALF = 4096
NTOK_A = 128 + HALF
NEGM = -30000.0
D_FF = 2816
D_FF_EXP = 3584


def host_consts():
    sel = np.zeros((8, 8, 128), np.float32)
    for e in range(8):
        sel[e, e, :] = 1
    j = np.arange(128)[:, None]
    i = np.arange(128)[None, :]
    tri = ((j // 64 == i // 64) & (j <= i)).astype(np.float32)
    maskT = ((np.arange(128)[:, None] % 64) <= np.arange(64)[None, :]).astype(np.float32)
    qi = np.arange(128)[:, None]
    kj = np.arange(256)[None, :]
    rel = qi + 128 - kj
    band = (rel >= 0) & (rel < 128)
    mask = np.zeros((128, 272), np.float32)
    mask[:, :256] = np.where(band, 0.0, NEGM)
    tris = (j < i).astype(np.float32)
    iota = np.arange(128, dtype=np.float32).reshape(128, 1)
    return {"ones": np.ones((128, 128), np.float32), "ident": np.eye(128, dtype=np.float32), "sel": sel,
            "tri": tri, "maskT": maskT, "mask": mask, "tris": tris, "iota": iota}


CONST_SHAPES = {"ones": [128, 128], "ident": [128, 128], "sel": [8, 8, 128], "tri": [128, 128],
                "maskT": [128, 64], "mask": [128, 272], "tris": [128, 128], "iota": [128, 1]}


def per_core_layout(x, meta):
    cores = []
    half_idx = np.arange(8, dtype=np.float32)
    inv_freq = (np.float32(500000.0) ** (-half_idx * np.float32(2.0) / np.float32(16))).astype(np.float32)
    base_mask = host_consts()["mask"]
    for c in range(N_CORES):
        b, hf = c // 2, c % 2
        tok = np.zeros((NTOK_A, 1024), np.float32)
        pos = np.zeros((NTOK_A,), np.float32)
        rm = np.zeros((128, 2), np.float32)
        if hf == 0:
            tok[112:128] = meta
            pos[112:128] = np.arange(16, dtype=np.float32)
            rm[112:128, 0] = 1.0
        rm[:, 1] = (1.0 - rm[:, 0]) * np.float32(-1e30)
        tok[128:] = x[b, hf * HALF:(hf + 1) * HALF]
        pos[128:] = 16 + hf * HALF + np.arange(HALF, dtype=np.float32)
        ang = (pos[:, None] * inv_freq[None, :]).astype(np.float32)
        m0 = base_mask.copy()
        if hf == 0:
            m0[:, :128] = NEGM
        cores.append({
            "xin": np.ascontiguousarray(tok.T.reshape(8, 128, NTOK_A)),
            "rowmask": rm, "cos": np.cos(ang).astype(np.float32), "sin": np.sin(ang).astype(np.float32),
            "mask0": m0,
        })
    return cores


class Builder:
    def __init__(self):
        self.nc = bass.Bass("TRN2", target_bir_lowering=False)
        self.stack = ExitStack()
        self.P = Prog(self.nc, self.stack)
        self.C = Consts()
        self.ins = {}
        cin = {k: self.inp(k, s) for k, s in CONST_SHAPES.items()}
        load_consts(self.P, self.C, cin)

    def inp(self, name, shape, dt=F32):
        t = self.P.dram(name, shape, dt, kind="ExternalInput")
        self.ins[name] = t
        return t

    def out(self, name, shape, dt=F32):
        return self.P.dram(name, shape, dt, kind="ExternalOutput")

    def act_in(self, name, ntok):
        a = Act(self.P, name, ntok, kind="ExternalInput")
        return a

    def act_out(self, name, ntok):
        return Act(self.P, name, ntok, kind="ExternalOutput")

    def act_tmp(self, name, ntok):
        return Act(self.P, name, ntok)

    def finish(self):
        self.P.finish()
        self.stack.close()
        return self.nc


def state_io(B, prefix, out):
    mk = B.out if out else B.inp
    return {"C": mk(prefix + "C", [128, 8, 128]), "n": mk(prefix + "n", [128, 8]), "m": mk(prefix + "m", [8, 1])}


def run(nc, in_maps):
    res = run_bass_kernel_spmd(nc, in_maps, core_ids=list(range(N_CORES)))
    return res.results


STW = 1040


def state_views(ap2d):
    return {"C": View(ap2d[:, 0:1024].rearrange("p (h d) -> p h d", h=8)),
            "n": View(ap2d[:, 1024:1032]),
            "m": View(ap2d[0:8, 1032:1033])}


def build_fused():
    B = Builder()
    P, C = B.P, B.C
    nc = B.nc
    PAIRS = [[0, 1], [2, 3], [4, 5], [6, 7]]
    xin = B.act_in("xin", NTOK_A)
    rmk = B.inp("rowmask", [128, 2])
    par = B.inp("par", [128, 1])
    cos_d = B.inp("cos", [NTOK_A, 8])
    sin_d = B.inp("sin_t", [NTOK_A, 8])
    m0 = B.inp("mask0", [128, 272])
    zst = B.inp("zstate", [128, STW])
    w_in_a = B.inp("w_in_a", [2, 1024, 3088])
    b_gate_a = B.inp("b_gate_a", [2, 16])
    ln_h_a = B.inp("ln_h_a", [2, 1024])
    w_out_a = B.inp("w_out_a", [2, 1024, 1024])
    w_kv = B.inp("w_kv", [1024, 512])
    w_q_b = B.inp("w_q_b", [2, 1024, 1024])
    sinks_b = B.inp("sinks_b", [2, 16])
    w_o_b = B.inp("w_o_b", [2, 1024, 1024])
    w_gu_d = B.inp("w_gu_d", [2, 1024, 2 * D_FF])
    w_down_d = B.inp("w_down_d", [2, D_FF, 1024])
    w_router = B.inp("w_router", [2, 1024, 8])
    b_router = B.inp("b_router", [2, 8])
    w_gu_e = B.inp("w_gu_e", [2, 8, 1024, 2 * D_FF_EXP])
    w_down_e = B.inp("w_down_e", [2, 8, D_FF_EXP, 1024])
    ln_g = B.inp("ln_g", [4, 2, 1024])
    ln_b = B.inp("ln_b", [4, 2, 1024])
    out = B.act_out("out", HALF)
    hA = B.act_tmp("hA", NTOK_A)
    hB = B.act_tmp("hB", NTOK_A)
    hmid = B.act_tmp("hmid", NTOK_A)
    st_loc = P.dram("st_loc", [128, STW], F32)
    st_all = P.dram("st_all", [256, STW], F32)
    st_dump = P.dram("st_dump", [128, STW], F32)
    kt_d = P.dram("kt_d", [64, 4, NTOK_A], BF16)
    v_d = P.dram("v_d", [NTOK_A, 256], BF16)
    HW = 64 * 4 * 144 + 144 * 256
    halo_loc = P.dram("halo_loc", [1, HW], BF16)
    halo_all = P.dram("halo_all", [2, HW], BF16)

    with ExitStack() as st0:
        zt = P.sb("zpad", [128, 8], F32, st0)
        P.op("dve", lambda e: e.memset(zt[:], 0.0), writes=[zt])
        with nc.allow_non_contiguous_dma(reason="8-column pad of the packed state row"):
            P.dma("sp", st_loc.h[:, 1032:1040], zt[:], zt, reads=[zt], writes=[st_loc])
            P.dma("sp", st_dump.h[:, 1032:1040], zt[:], zt, reads=[zt], writes=[st_dump])
        P.end_phase()

    def ffn(layer, hin, hout, tok0, ntok, out_tok0=None):
        i = layer // 2
        if layer % 2 == 0:
            ffn_phase(P, C, hin, hout, tok0, ntok, D_FF, ln_g.h[layer, 1], ln_b.h[layer, 1],
                      wgu_ap=w_gu_d.h[i], wd_ap=w_down_d.h[i], out_tok0=out_tok0)
        else:
            ffn_phase(P, C, hin, hout, tok0, ntok, D_FF_EXP, ln_g.h[layer, 1], ln_b.h[layer, 1],
                      moe=dict(wr=w_router.h[i], br=b_router.h[i], wgu=w_gu_e.h[i], wd=w_down_e.h[i]), out_tok0=out_tok0)

    h_cur, h_nxt = xin, hA
    for layer in range(2):
        mlstm_phase(P, C, h_cur, None, NTOK_A, w_in_a.h[layer], b_gate_a.h[layer], None, None, None, None, rmk,
                    state_views(zst.h[:, :]), state_views(st_loc.h[:, :]), True)
        P.collective_allgather(st_loc.h.ap().opt(), st_all.h.ap().opt(), PAIRS)
        mlstm_phase(P, C, h_cur, hmid, NTOK_A, w_in_a.h[layer], b_gate_a.h[layer], ln_h_a.h[layer], w_out_a.h[layer],
                    ln_g.h[layer, 0], ln_b.h[layer, 0], rmk, state_views(st_all.h[0:128, :]), state_views(st_dump.h[:, :]),
                    False, par=par)
        ffn(layer, hmid, h_nxt, 0, NTOK_A)
        h_cur, h_nxt = h_nxt, (hB if h_nxt is hA else hA)
    kv_phase(P, C, h_cur, NTOK_A, w_kv[:], cos_d, sin_d, kt_d, v_d)
    hk = halo_loc.h[0, 0:64 * 4 * 144].rearrange("(d g t) -> d g t", d=64, g=4)
    hv = halo_loc.h[0, 64 * 4 * 144:HW].rearrange("(t c) -> t c", c=256)
    with ExitStack() as st:
        kbuf = P.sb("hx_k", [64, 4, 144], BF16, st)
        vbuf = P.sb("hx_v", [128, 2, 256], BF16, st)
        P.dma("sp", kbuf[:, :, 0:16], kt_d.h[:, :, 112:128], kbuf, writes=[kbuf])
        P.dma("sp", kbuf[:, :, 16:144], kt_d.h[:, :, NTOK_A - 128:NTOK_A], kbuf, writes=[kbuf])
        P.dma("sp", hk, kbuf[:], kbuf, reads=[kbuf], writes=[halo_loc])
        P.dma("sp", vbuf[:16, 0, :], v_d.h[112:128, :], vbuf, writes=[vbuf])
        P.dma("sp", vbuf[:, 1, :], v_d.h[NTOK_A - 128:NTOK_A, :], vbuf, writes=[vbuf])
        P.dma("sp", hv[0:16, :], vbuf[:16, 0, :], vbuf, reads=[vbuf], writes=[halo_loc])
        P.dma("sp", hv[16:144, :], vbuf[:, 1, :], vbuf, reads=[vbuf], writes=[halo_loc])
        P.end_phase()
    P.collective_allgather(halo_loc.h.ap().opt(), halo_all.h.ap().opt(), PAIRS)
    rk = halo_all.h[0, 0:64 * 4 * 144].rearrange("(d g t) -> d g t", d=64, g=4)
    rv = halo_all.h[0, 64 * 4 * 144:HW].rearrange("(t c) -> t c", c=256)
    halo = {"kt_meta": View(rk[:, :, 0:16]), "v_meta": View(rv[0:16, :]),
            "kt_prev": View(rk[:, :, 16:144]), "v_prev": View(rv[16:144, :])}
    for layer in range(2, 4):
        j = layer - 2
        swa_phase(P, C, h_cur, hmid, HALF // 128, w_q_b.h[j], sinks_b.h[j], w_o_b.h[j], ln_g.h[layer, 0], ln_b.h[layer, 0],
                  cos_d, sin_d, kt_d, v_d, halo, C.cin["mask"], m0)
        if layer == 3:
            ffn(layer, hmid, out, 128, HALF, out_tok0=0)
        else:
            ffn(layer, hmid, h_nxt, 128, HALF)
            h_cur, h_nxt = h_nxt, (hB if h_nxt is hA else hA)
    return B.finish()


def kernel(x, meta, w_in_a, b_gate_a, ln_h_a, w_out_a, w_kv, w_q_b, sinks_b, w_o_b,
           w_gu_d, w_down_d, w_router, b_router, w_gu_e, w_down_e, ln_g, ln_b):
    f = lambda a: np.ascontiguousarray(np.asarray(a, dtype=np.float32))
    x, meta = f(x), f(meta)
    consts = host_consts()
    lay = per_core_layout(x, meta)
    shared = {"w_in_a": f(w_in_a), "b_gate_a": f(b_gate_a), "ln_h_a": f(ln_h_a), "w_out_a": f(w_out_a), "w_kv": f(w_kv),
              "w_q_b": f(w_q_b), "sinks_b": f(sinks_b), "w_o_b": f(w_o_b), "w_gu_d": f(w_gu_d), "w_down_d": f(w_down_d),
              "w_router": f(w_router), "b_router": f(b_router), "w_gu_e": f(w_gu_e), "w_down_e": f(w_down_e),
              "ln_g": f(ln_g), "ln_b": f(ln_b), "zstate": np.zeros((128, STW), np.float32)}
    nc = build_fused()
    in_maps = []
    for c in range(N_CORES):
        m = dict(consts)
        m.update(shared)
        m.update({"xin": lay[c]["xin"], "rowmask": lay[c]["rowmask"], "cos": lay[c]["cos"], "sin_t": lay[c]["sin"],
                  "mask0": lay[c]["mask0"], "par": np.full((128, 1), float(c % 2), np.float32)})
        in_maps.append(m)
    res = run_bass_kernel_spmd(nc, in_maps, core_ids=list(range(N_CORES))).results
    out = np.empty((4, SEQ, 1024), np.float32)
    for c in range(N_CORES):
        b, hf = c // 2, c % 2
        out[b, hf * HALF:(hf + 1) * HALF] = res[c]["out"].reshape(1024, HALF).T
    return out
```
